# Optimizing a Trainium2 kernel written in Bass

```python
import math
import jax
import jax.numpy as jnp
from jax import lax
import numpy as np

D_MODEL = 1024
BATCH = 4
SEQ = 4096
DEPTH = 1

DN_HEADS = 4
DN_HEAD_DIM = 128
DN_WIDTH = DN_HEADS * DN_HEAD_DIM
CONV_WIDTH = 4
CHUNK = 64
SWA_HEADS = 8
SWA_KV_HEADS = 2
SWA_GROUP = SWA_HEADS // SWA_KV_HEADS
SWA_HEAD_DIM = 64
SWA_WIDTH = SWA_HEADS * SWA_HEAD_DIM
SWA_KV_WIDTH = SWA_KV_HEADS * SWA_HEAD_DIM
WINDOW = 128
MIX_WIDTH = DN_WIDTH + SWA_WIDTH
IN_SIZES = (3 * DN_WIDTH, DN_WIDTH, DN_HEADS, DN_HEADS, SWA_WIDTH, SWA_KV_WIDTH, SWA_KV_WIDTH)
IN_COLS = sum(IN_SIZES)
N_EXPERTS = 32
TOP_K = 4
D_FF = D_MODEL
SWIGLU_ALPHA = 1.702
SWIGLU_LIMIT = 7.0
MOE_BLOCK = 128
EPS = 1e-6
NEG = -1e30

kernel_name = 'hybrid_deltanet_swa_moe_adaln'


def _rmsnorm(x, w):
    xf = x.astype(jnp.float32)
    y = xf * lax.rsqrt(jnp.mean(xf * xf, axis=-1, keepdims=True) + EPS)
    return (y * w.astype(jnp.float32)).astype(x.dtype)


def _l2norm(x):
    xf = x.astype(jnp.float32)
    return xf * lax.rsqrt(jnp.sum(xf * xf, axis=-1, keepdims=True) + EPS)


def _causal_conv_silu(u, w):
    n_ch = u.shape[-1]
    y = lax.conv_general_dilated(u, w[:, None, :].astype(u.dtype), window_strides=(1,),
                                 padding=[(CONV_WIDTH - 1, 0)],
                                 dimension_numbers=('NWC', 'WIO', 'NWC'),
                                 feature_group_count=n_ch)
    return jax.nn.silu(y)


def _chunk_gated_delta_rule(q, k, v, g, beta):
    Bn, Sn, H, Dk = q.shape
    Dv = v.shape[-1]
    N = Sn // CHUNK

    def to_chunks(t):
        t = t.astype(jnp.float32).reshape(Bn, N, CHUNK, *t.shape[2:])
        return jnp.moveaxis(t, 3, 2)

    qc = to_chunks(q) * (Dk ** -0.5)
    kc, vc, gc, bc = to_chunks(k), to_chunks(v), to_chunks(g), to_chunks(beta)
    gcum = jnp.cumsum(gc, axis=-1)
    incl = jnp.tril(jnp.ones((CHUNK, CHUNK), bool))
    strict = jnp.tril(jnp.ones((CHUNK, CHUNK), bool), -1)
    diff = gcum[..., :, None] - gcum[..., None, :]
    decay = jnp.where(incl, jnp.exp(jnp.where(incl, diff, 0.0)), 0.0)
    k_beta = kc * bc[..., None]
    a_kk = jnp.where(strict, jnp.einsum('bnhcd,bnhsd->bnhcs', k_beta, kc) * decay, 0.0)
    t_mat = a_kk + jnp.eye(CHUNK, dtype=jnp.float32)
    rhs = jnp.concatenate([vc * bc[..., None], k_beta * jnp.exp(gcum)[..., None]], axis=-1)
    sol = jax.lax.linalg.triangular_solve(t_mat, rhs, left_side=True, lower=True,
                                          unit_diagonal=True)
    u, w = sol[..., :Dv], sol[..., Dv:]
    a_qk = jnp.where(incl, jnp.einsum('bnhcd,bnhsd->bnhcs', qc, kc) * decay, 0.0)
    q_dec = qc * jnp.exp(gcum)[..., None]
    g_last = gcum[..., -1]
    k_dec = kc * jnp.exp(g_last[..., None] - gcum)[..., None]
    chunk_decay = jnp.exp(g_last)
    xs = tuple(jnp.moveaxis(t, 1, 0) for t in (q_dec, k_dec, u, w, a_qk, chunk_decay))

    def step(state, inp):
        q_i, k_i, u_i, w_i, a_i, d_i = inp
        v_new = u_i - jnp.einsum('bhcd,bhde->bhce', w_i, state)
        o_i = (jnp.einsum('bhcd,bhde->bhce', q_i, state)
               + jnp.einsum('bhcs,bhse->bhce', a_i, v_new))
        state = state * d_i[..., None, None] + jnp.einsum('bhcd,bhce->bhde', k_i, v_new)
        return state, o_i

    state0 = jnp.zeros((Bn, H, Dk, Dv), jnp.float32)
    _, o = lax.scan(step, state0, xs)
    return jnp.transpose(o, (1, 0, 3, 2, 4)).reshape(Bn, Sn, H, Dv)


def _sliding_window_attention(q, k, v, q_norm_w, k_norm_w, sinks):
    Bn, Sn, _ = q.shape
    NB = Sn // WINDOW
    q = _rmsnorm(q.reshape(Bn, Sn, SWA_HEADS, SWA_HEAD_DIM), q_norm_w).astype(jnp.float32)
    k = _rmsnorm(k.reshape(Bn, Sn, SWA_KV_HEADS, SWA_HEAD_DIM), k_norm_w).astype(jnp.float32)
    v = v.reshape(Bn, Sn, SWA_KV_HEADS, SWA_HEAD_DIM).astype(jnp.float32)
    pad = jnp.zeros((Bn, WINDOW, SWA_KV_HEADS, SWA_HEAD_DIM), jnp.float32)

    def band(t):
        tp = jnp.concatenate([pad, t], axis=1).reshape(Bn, NB + 1, WINDOW, SWA_KV_HEADS, SWA_HEAD_DIM)
        return jnp.concatenate([tp[:, :-1], tp[:, 1:]], axis=2)

    kw, vw = band(k), band(v)
    qb = q.reshape(Bn, NB, WINDOW, SWA_KV_HEADS, SWA_GROUP, SWA_HEAD_DIM)
    s = jnp.einsum('bnqkgd,bnskd->bnkgqs', qb, kw) * (SWA_HEAD_DIM ** -0.5)
    qi = jnp.arange(WINDOW)[:, None]
    kj = jnp.arange(2 * WINDOW)[None, :]
    rel = (qi + WINDOW - kj).astype(jnp.float32)
    key_pos = jnp.arange(NB)[:, None, None] * WINDOW - WINDOW + kj[None]
    valid = (rel[None] >= 0) & (rel[None] < WINDOW) & (key_pos >= 0)
    slopes = (2.0 ** (-8.0 * jnp.arange(1, SWA_HEADS + 1, dtype=jnp.float32) / SWA_HEADS))
    slopes = slopes.reshape(SWA_KV_HEADS, SWA_GROUP)
    logits = s - slopes[None, None, :, :, None, None] * rel
    logits = jnp.where(valid[None, :, None, None], logits, NEG)
    sink = sinks.astype(jnp.float32).reshape(SWA_KV_HEADS, SWA_GROUP)[None, None, :, :, None]
    m = jnp.maximum(jnp.max(logits, axis=-1), sink)
    p = jnp.exp(logits - m[..., None])
    denom = jnp.sum(p, axis=-1) + jnp.exp(sink - m)
    o = jnp.einsum('bnkgqs,bnskd->bnqkgd', p / denom[..., None], vw)
    return o.reshape(Bn, Sn, SWA_WIDTH)


def _mixer(h, w_in, conv_w, a_log, dt_bias, dn_norm_w, q_norm_w, k_norm_w, sinks, w_out):
    Bn, Sn, _ = h.shape
    proj = h @ w_in
    split_idx = np.cumsum(IN_SIZES)[:-1].tolist()
    dn_qkv, dn_z, dn_a, dn_b, sw_q, sw_k, sw_v = jnp.split(proj, split_idx, axis=-1)
    qkv = _causal_conv_silu(dn_qkv, conv_w)
    q, k, v = jnp.split(qkv, 3, axis=-1)
    q = _l2norm(q.reshape(Bn, Sn, DN_HEADS, DN_HEAD_DIM))
    k = _l2norm(k.reshape(Bn, Sn, DN_HEADS, DN_HEAD_DIM))
    v = v.reshape(Bn, Sn, DN_HEADS, DN_HEAD_DIM)
    beta = jax.nn.sigmoid(dn_b.astype(jnp.float32))
    g = -jnp.exp(a_log.astype(jnp.float32)) * jax.nn.softplus(
        dn_a.astype(jnp.float32) + dt_bias.astype(jnp.float32))
    o_dn = _chunk_gated_delta_rule(q, k, v, g, beta)
    o_dn = _rmsnorm(o_dn, dn_norm_w) * jax.nn.silu(
        dn_z.reshape(Bn, Sn, DN_HEADS, DN_HEAD_DIM).astype(jnp.float32))
    o_dn = o_dn.reshape(Bn, Sn, DN_WIDTH)
    o_sw = _sliding_window_attention(sw_q, sw_k, sw_v, q_norm_w, k_norm_w, sinks)
    mixed = jnp.concatenate([o_dn.astype(h.dtype), o_sw.astype(h.dtype)], axis=-1)
    return mixed @ w_out


def _moe(h, w_router, b_router, w_up, b_up, w_down, b_down):
    Bn, Sn, D = h.shape
    T = Bn * Sn
    xf = h.reshape(T, D)
    logits = (xf @ w_router).astype(jnp.float32) + b_router.astype(jnp.float32)
    top_val, top_idx = lax.top_k(logits, TOP_K)
    gates = jax.nn.softmax(top_val, axis=-1)
    n_assign = T * TOP_K
    flat_e = top_idx.reshape(-1)
    flat_tok = jnp.arange(n_assign, dtype=jnp.int32) // TOP_K
    flat_gate = gates.reshape(-1)
    order = jnp.argsort(flat_e)
    se, stok, sgate = flat_e[order], flat_tok[order], flat_gate[order]
    counts = jnp.bincount(flat_e, length=N_EXPERTS)
    padded = (counts + MOE_BLOCK - 1) // MOE_BLOCK * MOE_BLOCK
    start = jnp.cumsum(counts) - counts
    pend = jnp.cumsum(padded)
    pstart = pend - padded
    dest = pstart[se] + (jnp.arange(n_assign) - start[se])
    n_pad = n_assign + N_EXPERTS * MOE_BLOCK
    n_blocks = n_pad // MOE_BLOCK
    row_tok = jnp.zeros((n_pad,), jnp.int32).at[dest].set(stok)
    row_gate = jnp.zeros((n_pad,), jnp.float32).at[dest].set(sgate)
    block_e = jnp.minimum(jnp.searchsorted(pend, jnp.arange(n_blocks) * MOE_BLOCK, side='right'),
                          N_EXPERTS - 1)
    xs = xf[row_tok].reshape(n_blocks, MOE_BLOCK, D)

    def expert_block(args):
        xb, e = args
        hu = xb @ w_up[e] + b_up[e]
        x_glu = jnp.minimum(hu[:, :D_FF], SWIGLU_LIMIT)
        x_lin = jnp.clip(hu[:, D_FF:], -SWIGLU_LIMIT, SWIGLU_LIMIT)
        act = x_glu * jax.nn.sigmoid(SWIGLU_ALPHA * x_glu) * (x_lin + 1.0)
        return act @ w_down[e] + b_down[e]

    ys = lax.map(expert_block, (xs, block_e)).reshape(n_pad, D)
    ys = ys.astype(jnp.float32) * row_gate[:, None]
    out = jax.ops.segment_sum(ys, row_tok, num_segments=T)
    return out.reshape(Bn, Sn, D).astype(h.dtype)


def setup_inputs(seed: int = 0) -> dict:
    key = jax.random.key(seed)
    ks = jax.random.split(key, 24)
    L = DEPTH

    def nrm(k, shape, scale):
        return jax.random.normal(k, shape, jnp.float32) * scale

    dt = jnp.exp(jax.random.uniform(ks[8], (L, DN_HEADS), jnp.float32,
                                    math.log(1e-3), math.log(1e-1)))
    return {
        'x': nrm(ks[0], (BATCH, SEQ, D_MODEL), 1.0),
        'c': nrm(ks[1], (BATCH, D_MODEL), 1.0),
        'w_ada': nrm(ks[2], (L, D_MODEL, 6 * D_MODEL), 0.5 * D_MODEL ** -0.5),
        'b_ada': nrm(ks[3], (L, 6 * D_MODEL), 0.02),
        'norm1_w': 1.0 + nrm(ks[4], (L, D_MODEL), 0.02),
        'w_in': nrm(ks[5], (L, D_MODEL, IN_COLS), D_MODEL ** -0.5),
        'conv_w': nrm(ks[6], (L, CONV_WIDTH, 3 * DN_WIDTH), CONV_WIDTH ** -0.5),
        'a_log': jnp.log(jax.random.uniform(ks[7], (L, DN_HEADS), jnp.float32, 1.0, 16.0)),
        'dt_bias': dt + jnp.log(-jnp.expm1(-dt)),
        'dn_norm_w': 1.0 + nrm(ks[9], (L, DN_HEAD_DIM), 0.02),
        'q_norm_w': 1.0 + nrm(ks[10], (L, SWA_HEAD_DIM), 0.02),
        'k_norm_w': 1.0 + nrm(ks[11], (L, SWA_HEAD_DIM), 0.02),
        'sinks': nrm(ks[12], (L, SWA_HEADS), 1.0),
        'w_out': nrm(ks[13], (L, MIX_WIDTH, D_MODEL), MIX_WIDTH ** -0.5),
        'norm2_w': 1.0 + nrm(ks[14], (L, D_MODEL), 0.02),
        'w_router': nrm(ks[15], (L, D_MODEL, N_EXPERTS), D_MODEL ** -0.5),
        'b_router': nrm(ks[16], (L, N_EXPERTS), 0.01),
        'w_up': nrm(ks[17], (L, N_EXPERTS, D_MODEL, 2 * D_FF), D_MODEL ** -0.5),
        'b_up': nrm(ks[18], (L, N_EXPERTS, 2 * D_FF), 0.01),
        'w_down': nrm(ks[19], (L, N_EXPERTS, D_FF, D_MODEL), D_FF ** -0.5),
        'b_down': nrm(ks[20], (L, N_EXPERTS, D_MODEL), 0.01),
    }


def reference(x, c, w_ada, b_ada, norm1_w, w_in, conv_w, a_log, dt_bias, dn_norm_w,
              q_norm_w, k_norm_w, sinks, w_out, norm2_w, w_router, b_router,
              w_up, b_up, w_down, b_down):
    out_dtype = x.dtype
    c_act = jax.nn.silu(c)
    for l in range(DEPTH):
        mod = (c_act @ w_ada[l] + b_ada[l])[:, None, :]
        shift1, scale1, gate1, shift2, scale2, gate2 = jnp.split(mod, 6, axis=-1)
        h = _rmsnorm(x, norm1_w[l]) * (1.0 + scale1) + shift1
        y = _mixer(h, w_in[l], conv_w[l], a_log[l], dt_bias[l], dn_norm_w[l],
                   q_norm_w[l], k_norm_w[l], sinks[l], w_out[l])
        x = x + gate1 * y
        h = _rmsnorm(x, norm2_w[l]) * (1.0 + scale2) + shift2
        x = x + gate2 * _moe(h, w_router[l], b_router[l], w_up[l], b_up[l], w_down[l], b_down[l])
    return x.astype(out_dtype)
```

```python
import numpy as np
import ml_dtypes
import concourse.bass as bass
import concourse.mybir as mybir
from concourse.bass_utils import run_bass_kernel_spmd
from contextlib import ExitStack

F32 = mybir.dt.float32
BF16 = mybir.dt.bfloat16
I32 = mybir.dt.int32
U32 = mybir.dt.uint32
AF = mybir.ActivationFunctionType
ALU = mybir.AluOpType
AX = mybir.AxisListType

EPS = 1e-6
NEGBIG = -30000.0


class Buf:
    __slots__ = ("name", "w", "rs", "excl")

    def __init__(self, name, excl=False):
        self.name = name
        self.w = None
        self.rs = []
        self.excl = excl


class Stager:
    def __init__(self, S):
        self.S = S
        self.st = {}

    def op(self, k, eng, fn, reads=(), writes=()):
        self.st.setdefault(k, []).append((eng, fn, reads, writes))

    def run(self):
        for k in sorted(self.st):
            for eng, fn, r, w in self.st[k]:
                self.S.op(eng, fn, reads=r, writes=w)
        self.st = {}


class Sched:
    ENG = ("pe", "act", "dve", "pool", "sp")

    def __init__(self, nc, stack):
        self.nc = nc
        self.stack = stack
        self.prog = {e: [] for e in self.ENG}
        self.esem = {e: stack.enter_context(nc.semaphore("es_" + e)) for e in self.ENG}
        self.tick = {e: 0 for e in self.ENG}
        self.waited = {e: {} for e in self.ENG}
        self.dsems = {}
        self.nbuf = 0

    def buf(self, name=None, excl=False):
        self.nbuf += 1
        return Buf((name or "b") + str(self.nbuf), excl)

    def _collect(self, eng, reads, writes):
        deps = {}

        def add(d):
            if d is None:
                return
            s, v = d
            if eng == "pe" and s is self.esem["pe"]:
                return
            k = id(s)
            if k not in deps or deps[k][1] < v:
                deps[k] = (s, v)
        for b in reads:
            add(b.w)
            if b.excl:
                for r in b.rs:
                    add(r)
        for b in writes:
            add(b.w)
            for r in b.rs:
                add(r)
        out = []
        wd = self.waited[eng]
        for k, (s, v) in deps.items():
            if wd.get(k, 0) >= v:
                continue
            wd[k] = v
            out.append((s, v))
        return out

    def _commit(self, reads, writes, done):
        for b in writes:
            b.w = done
            b.rs = []
        for b in reads:
            if b.excl:
                b.w = done
                b.rs = []
            elif b not in writes:
                b.rs.append(done)
                if len(b.rs) > 24:
                    m = {}
                    for s, v in b.rs:
                        if id(s) not in m or m[id(s)][1] < v:
                            m[id(s)] = (s, v)
                    b.rs = list(m.values())

    stopped = False
    stop_at = None

    def mark(self, label):
        if self.stop_at is not None and label == self.stop_at:
            self.stopped = True

    def op(self, eng, fn, reads=(), writes=()):
        if self.stopped:
            return
        waits = self._collect(eng, reads, writes)
        self.tick[eng] += 1
        done = (self.esem[eng], self.tick[eng])
        self.prog[eng].append((waits, fn, self.esem[eng], 1))
        self._commit(reads, writes, done)

    def dma(self, eng, fn, reads=(), writes=(), key=None, force=False):
        if self.stopped and not force:
            return
        waits = self._collect(eng, reads, writes)
        kb = key if key is not None else (writes[0] if writes else reads[0])
        if kb not in self.dsems:
            self.dsems[kb] = [self.stack.enter_context(self.nc.semaphore("ds_" + kb.name)), 0]
        d = self.dsems[kb]
        d[1] += 16
        done = (d[0], d[1])
        self.prog[eng].append((waits, fn, d[0], 16))
        self._commit(reads, writes, done)

    def barrier(self):
        allw = [(self.esem[e], self.tick[e]) for e in self.ENG if self.tick[e] > 0]
        allw += [(d[0], d[1]) for d in self.dsems.values()]
        for e in self.ENG:
            wd = self.waited[e]
            waits = []
            for s_, v in allw:
                if s_ is self.esem[e]:
                    continue
                if wd.get(id(s_), 0) >= v:
                    continue
                wd[id(s_)] = v
                waits.append((s_, v))
            self.prog[e].append((waits, None, None, 0))

    def final_wait(self, eng, bufs):
        waits = self._collect(eng, bufs, bufs)
        self.prog[eng].append((waits, None, None, 0))

    def flush(self):
        with self.nc.Block() as block:
            self.emit(block)
        self.prog = {e: [] for e in self.ENG}

    def emit(self, block):
        m = {"pe": block.tensor, "act": block.scalar, "dve": block.vector,
             "pool": block.gpsimd, "sp": block.sync}
        for e in self.ENG:
            plist = self.prog[e]

            def body(engobj, plist=plist):
                for waits, fn, sem, inc in plist:
                    for (s, v) in waits:
                        engobj.wait_ge(s, v)
                    if fn is not None:
                        fn(engobj).then_inc(sem, inc)
            m[e](body)


C_ID, C_UI, C_SL, C_NEG, C_OFFD, C_ONE, C_BD, C_SWB = 0, 128, 256, 384, 512, 640, 768, 896
NCST = 896 + 2 * 8 * 128


def make_consts():
    c = np.zeros((128, NCST), np.float32)
    i = np.arange(128)
    c[:, C_ID:C_ID + 128] = np.eye(128)
    c[:, C_UI:C_UI + 128] = (i[:, None] <= i[None, :])
    c[:, C_SL:C_SL + 128] = (i[:, None] > i[None, :])
    c[:, C_NEG:C_NEG + 128] = np.where(i[:, None] > i[None, :], NEGBIG, 0.0)
    c[:, C_OFFD:C_OFFD + 128] = 1.0 - np.eye(128)
    c[:, C_ONE:C_ONE + 128] = 1.0
    c[:, C_BD:C_BD + 128] = ((i[:, None] // 64) == (i[None, :] // 64))
    s = i[:, None].astype(np.float32)
    q = i[None, :].astype(np.float32)
    for h in range(8):
        slope = 2.0 ** (-(h + 1))
        prev = np.where(s > q, -slope * (q + 128.0 - s), NEGBIG)
        own = np.where(s <= q, -slope * (q - s), NEGBIG)
        c[:, C_SWB + (0 * 8 + h) * 128: C_SWB + (0 * 8 + h) * 128 + 128] = prev
        c[:, C_SWB + (1 * 8 + h) * 128: C_SWB + (1 * 8 + h) * 128 + 128] = own
    return c


P_FLAG, P_HALO, P_C, P_ALOG, P_DTB, P_SINK, P_DNW, P_QNW, P_KNW, P_N1, P_BADA, P_CONV = \
    0, 1, 2, 10, 14, 18, 26, 154, 155, 156, 164, 212
NPRM = 212 + 48 + 1
P_QNW_HI = 212 + 48

NFM = 18 * 128
NTM = 512 + 8 + 128


SPARSE = True
NBLK = 96


def build(stage=9, stop_at=None, dbg=False):
    nc = bass.Bass("TRN2", target_bir_lowering=False)
    dbg_d = nc.dram_tensor("dbg", [128, 4096], F32, kind="ExternalOutput").ap() if dbg else None

    def din(name, shape, dt=F32):
        return nc.dram_tensor(name, shape, dt, kind="ExternalInput").ap()
    xp_d = din("xp", [2048, 1024])
    xo_d = din("xo", [2048, 1024])
    cst_d = din("cst", [128, NCST])
    prm_d = din("prm", [128, NPRM])
    wada_d = din("w_ada", [1024, 6144])
    bada_d = din("b_ada", [1, 6144])
    wfm_d = din("w_fm", [1024, NFM])
    wtm_d = din("w_tm", [1024, NTM])
    wout_d = din("w_out", [1024, 1024])
    n2_d = din("n2bc", [128, 1024])
    cstm_d = din("cstm", [128, 384], BF16)
    wr_d = din("w_router", [1024, 32])
    prm2_d = din("prm2", [128, 544])
    bdn_d = din("b_down", [32, 1024])
    if not SPARSE:
        wup_d = din("w_up", [32 * 1024, 2048])
        wdn_d = din("w_down", [32 * 1024, 1024])
    if SPARSE:
        prm3_d = din("prm3", [128, 160])
        bupg_d = din("b_upg", [4096, 16])
        wupg_d = nc.dram_tensor("w_upg", [4096, 8, 2048], F32, kind="ExternalInput").ap()
        wdng_d = nc.dram_tensor("w_dng", [4096, 8, 1024], F32, kind="ExternalInput").ap()
        wub_d = nc.dram_tensor("wub", [4096, 16384], BF16, kind="Internal").ap()
        wdb_d = nc.dram_tensor("wdb", [4096, 8192], BF16, kind="Internal").ap()
    out_d = nc.dram_tensor("out", [2048, 1024], F32, kind="ExternalOutput").ap()

    with ExitStack() as st:
        S = Sched(nc, st)
        G = Stager(S)
        S.stop_at = stop_at

        def sb(name, shape, dt=F32):
            return st.enter_context(nc.sbuf_tensor("s_" + name, shape, dt))

        def ps(name, shape, dt=F32):
            return st.enter_context(nc.psum_tensor("p_" + name, shape, dt))

        cst = sb("cst", [128, NCST]); Bcst = S.buf("cst")
        cstb = sb("cstb", [128, 896], BF16); Bcstb = S.buf("cstb")
        prm = sb("prm", [128, NPRM]); Bprm = S.buf("prm")

        cstm = sb("cstm", [128, 384], BF16); Bcstm = S.buf("cstm")
        S.dma("sp", lambda e: e.dma_start(out=cstm[:], in_=cstm_d), writes=[Bcstm])

        def cs(off, n=128):
            return cst[:, off:off + n]

        def csb(off, n=128):
            return cstb[:, off:off + n]

        S.dma("sp", lambda e: e.dma_start(out=cst[:], in_=cst_d), writes=[Bcst])
        S.dma("sp", lambda e: e.dma_start(out=prm[:], in_=prm_d), writes=[Bprm])
        S.op("dve", lambda e: e.tensor_copy(out=cstb[:], in_=cst[:, 0:896]), reads=[Bcst], writes=[Bcstb])

        PB = [ps(f"pb{i}", [128, 512]) for i in range(7)]
        BPB = [S.buf(f"pb{i}", excl=True) for i in range(7)]
        PT = ps("ptr", [128, 1024], BF16)
        BPTb = S.buf("ptr", excl=True)
        BPT = [BPTb for i in range(8)]

        cact = sb("cact", [128, 8]); Bcact = S.buf("cact")
        S.op("act", lambda e: e.activation(out=cact[:], in_=prm[:, P_C:P_C + 8], func=AF.Silu), reads=[Bprm], writes=[Bcact])
        scr_d = nc.dram_tensor("scr", [4, 1024], F32, kind="Internal").ap()
        Bscr = S.buf("scr")
        gate1 = sb("gate1", [128, 1024]); Bmodbc = S.buf("modbc")
        modT = sb("modT", [128, 16]); BmodT = S.buf("modT")
        A1 = sb("A1", [128, 8]); B1 = sb("B1", [128, 8]); BA1 = S.buf("A1")
        st_root = st
        st = ExitStack()
        wfm = sb("wfm", [128, 8, NFM], BF16); Bwfm = S.buf("wfm")
        wtm = sb("wtm", [128, 8, NTM], BF16); Bwtm = S.buf("wtm")
        wout = sb("wout", [128, 8, 1024], BF16); Bwout = S.buf("wout")
        for hh in range(2):
            S.dma("pool", lambda e, hh=hh: e.dma_start(out=wfm[:, :, hh * 1152:(hh + 1) * 1152],
                                                     in_=wfm_d.rearrange("(k p) n -> p k n", p=128)[:, :, hh * 1152:(hh + 1) * 1152]), writes=[Bwfm])
        S.dma("pool", lambda e: e.dma_start(out=wtm[:], in_=wtm_d.rearrange("(k p) n -> p k n", p=128)), writes=[Bwtm])
        S.dma("pool", lambda e: e.dma_start(out=wout[:], in_=wout_d.rearrange("(k p) n -> p k n", p=128)), writes=[Bwout])
        st_outer = st
        st = ExitStack()
        n2bc = sb("n2bc", [128, 1024]); Bn2 = S.buf("n2")
        S.dma("sp", lambda e: e.dma_start(out=n2bc[:], in_=n2_d), writes=[Bn2])
        wa = [sb(f"wa{i}", [128, 8, 512]) for i in range(2)]
        Bwa = [S.buf(f"wa{i}") for i in range(2)]
        wav = wada_d.rearrange("(k p) n -> p k n", p=128)
        modrow = sb("modrow", [1, 4096]); Bmodrow = S.buf("modrow")
        badarow = sb("badarow", [1, 4096]); Bbadarow = S.buf("badarow")
        S.dma("sp", lambda e: e.dma_start(out=badarow[:], in_=bada_d[:, 2048:6144]), writes=[Bbadarow])
        for grp in range(12):
            w_ = wa[grp % 2]; Bw_ = Bwa[grp % 2]
            S.dma("sp", lambda e, w_=w_, grp=grp: e.dma_start(out=w_[:], in_=wav[:, :, grp * 512:(grp + 1) * 512]), writes=[Bw_])
            if grp < 4:
                for j in range(4):
                    col = grp * 4 + j
                    for k in range(8):
                        S.op("pe", lambda e, w_=w_, j=j, k=k, col=col: e.matmul(
                            PB[0][:, col:col + 1], lhsT=w_[:, k, j * 128:(j + 1) * 128], rhs=cact[:, k:k + 1],
                            start=(k == 0), stop=(k == 7)), reads=[Bw_, Bcact], writes=[BPB[0]])
                if grp == 3:
                    S.op("dve", lambda e: e.tensor_tensor(out=modT[:], in0=PB[0][:, 0:16], in1=prm[:, P_BADA:P_BADA + 16], op=ALU.add),
                         reads=[BPB[0], Bprm], writes=[BmodT])
                    S.op("dve", lambda e: e.scalar_tensor_tensor(out=A1[:], in0=modT[:, 8:16], scalar=1.0, in1=prm[:, P_N1:P_N1 + 8],
                                                               op0=ALU.add, op1=ALU.mult), reads=[BmodT, Bprm], writes=[BA1])
                    S.op("dve", lambda e: e.tensor_copy(out=B1[:], in_=modT[:, 0:8]), reads=[BmodT], writes=[BA1])
            else:
                g2 = grp - 4
                pb = PB[1 + (g2 % 2)]; Bpb = BPB[1 + (g2 % 2)]
                for k in range(8):
                    S.op("pe", lambda e, w_=w_, k=k, pb=pb: e.matmul(pb[0:1, :], lhsT=cact[:, k:k + 1], rhs=w_[:, k, :],
                                                                     start=(k == 0), stop=(k == 7)), reads=[Bw_, Bcact], writes=[Bpb])
                S.op("dve", lambda e, pb=pb, g2=g2: e.tensor_tensor(out=modrow[0:1, g2 * 512:(g2 + 1) * 512], in0=pb[0:1, :],
                                                                    in1=badarow[0:1, g2 * 512:(g2 + 1) * 512], op=ALU.add),
                     reads=[Bpb, Bbadarow], writes=[Bmodrow])
        w2row = sb("w2row", [1, 1024]); Bw2row = S.buf("w2row")
        S.op("dve", lambda e: e.scalar_tensor_tensor(out=w2row[0:1, :], in0=modrow[0:1, 2048:3072], scalar=1.0, in1=n2bc[0:1, :], op0=ALU.add, op1=ALU.mult),
             reads=[Bmodrow, Bn2], writes=[Bw2row])
        S.dma("sp", lambda e: e.dma_start(out=scr_d[0:1, :], in_=modrow[0:1, 1024:2048]), reads=[Bmodrow], writes=[Bscr])
        S.dma("sp", lambda e: e.dma_start(out=scr_d[1:2, :], in_=w2row[0:1, :]), reads=[Bw2row], writes=[Bscr])
        S.dma("sp", lambda e: e.dma_start(out=scr_d[2:3, :], in_=modrow[0:1, 3072:4096]), reads=[Bmodrow], writes=[Bscr])
        for v, dst in enumerate((gate1,)):
            for hh in range(2):
                pb = PB[1 + hh]; Bpb = BPB[1 + hh]
                S.op("pe", lambda e, pb=pb, v=v, hh=hh: e.matmul(pb[:, :], lhsT=cst[0:1, C_ONE:C_ONE + 128],
                                                               rhs=modrow[0:1, v * 1024 + hh * 512: v * 1024 + (hh + 1) * 512],
                                                               start=True, stop=True), reads=[Bmodrow, Bcst], writes=[Bpb])
                if True:
                    S.op("act", lambda e, pb=pb, hh=hh, dst=dst: e.activation(out=dst[:, hh * 512:(hh + 1) * 512], in_=pb[:, :], func=AF.Identity),
                         reads=[Bpb], writes=[Bmodbc])

        S.mark("adaln")
        S.barrier()
        S.flush(); st.close()
        st = st_outer
        nA = sb("nA", [128, 4]); esink = sb("esink", [128, 8]); Bder = S.buf("der")
        S.op("act", lambda e: e.activation(out=nA[:], in_=prm[:, P_ALOG:P_ALOG + 4], func=AF.Exp), reads=[Bprm], writes=[Bder])
        S.op("dve", lambda e: e.tensor_scalar(out=nA[:], in0=nA[:], scalar1=-1.0, scalar2=None, op0=ALU.mult), reads=[Bder], writes=[Bder])
        S.op("act", lambda e: e.activation(out=esink[:], in_=prm[:, P_SINK:P_SINK + 8], func=AF.Exp), reads=[Bprm], writes=[Bder])

        S.mark("m0")
        xt = [sb(f"xt{i}", [128, 1024]) for i in range(4)]; Bxt = [S.buf(f"xt{i}") for i in range(4)]
        stat = [sb(f"stat{i}", [128, 2]) for i in range(2)]; Bstat = [S.buf(f"stat{i}") for i in range(2)]
        xn = [sb(f"xn{i}", [128, 1024], BF16) for i in range(2)]; Bxn = [S.buf(f"xn{i}") for i in range(2)]
        hT = sb("hT", [128, 8, 512], BF16); BhT = S.buf("hT")
        Ub = [sb(f"Ub{i}", [128, 516]) for i in range(2)]; BUb = [S.buf(f"Ub{i}") for i in range(2)]
        halo = sb("halo", [128, 12, 4]); Bhalo = [S.buf(f"halo{c}") for c in range(12)]
        ctmp = [sb(f"ctmp{i}", [128, 512]) for i in range(2)]; Bctmp = [S.buf(f"ctmp{i}") for i in range(2)]
        cs2 = [sb(f"csil{i}", [128, 512]) for i in range(2)]; Bcs2 = [S.buf(f"csil{i}") for i in range(2)]
        ctmp2 = None; Bctmp2 = None
        sqb2 = [sb(f"sqb{i}", [128, 512], BF16) for i in range(2)]; Bsqb2 = [S.buf(f"sqb{i}") for i in range(2)]
        rst = sb("rst", [128, 512]); Brst = S.buf("rst")
        fm = sb("fm", [128, 12, 512], BF16); Bfm = [S.buf(f"fm{c}") for c in range(12)]
        swq = sb("swq", [128, 8, 512], BF16); Bswq = [S.buf(f"swq{c}") for c in range(4)]
        swk = sb("swk", [128, 2, 640], BF16); Bswk = [S.buf(f"swk{c}") for c in range(2)]
        vv = sb("vv", [128, 5, 2, 128], BF16); Bvv = [S.buf(f"vv{t}") for t in range(5)]
        siluz = sb("siluz", [128, 4, 512], BF16); Bsz = [S.buf(f"sz{t}") for t in range(4)]
        tmf = sb("tmf", [128, 4, 32]); Btmf = [S.buf(f"tmf{t}") for t in range(4)]
        S32 = sb("S32", [128, 4, 128]); Sbf = sb("Sbf", [128, 4, 128], BF16); BS = [S.buf(f"S{h}") for h in range(4)]
        mixT = sb("mixT", [128, 8, 512], BF16); Bmix = [S.buf(f"mix{t}") for t in range(4)]
        Kdec = sb("Kdec", [128, 2, 4, 128], BF16); Vtm = sb("Vtm", [128, 2, 4, 128], BF16)
        Aqk = sb("Aqk", [128, 2, 4, 128], BF16); Minv = sb("Minv", [128, 2, 4, 128], BF16)
        Bdn = [[S.buf(f"dn{t}_{h}") for h in range(4)] for t in range(2)]
        Bfull = sb("Bfull", [128, 4, 2, 128], BF16); BBf = [S.buf(f"Bf{h}") for h in range(4)]
        WT = sb("WT", [128, 4, 2, 128], BF16); BWT = [S.buf(f"WT{h}") for h in range(4)]
        T1s = sb("T1s", [128, 4, 128], BF16); BT1 = [S.buf(f"T1{h}") for h in range(4)]
        D2T = sb("D2T", [128, 4, 128], BF16); BD2T = [S.buf(f"D2T{h}") for h in range(4)]
        lg = sb("lg", [128, 4, 128]); Blg = [S.buf(f"lg{h}") for h in range(4)]
        DT = sb("DT", [128, 4, 128]); BDT = [S.buf(f"DT{h}") for h in range(4)]
        ZPR = sb("ZPR", [128, 4, 3, 128], BF16); BZPR = [S.buf(f"ZPR{h}") for h in range(4)]
        PTt = sb("PTt", [128, 4, 128], BF16); BPTt = [S.buf(f"PTt{h}") for h in range(4)]
        Rm = sb("Rm", [128, 4, 128], BF16); BRm = [S.buf(f"Rm{h}") for h in range(4)]
        vnew = sb("vnew", [128, 4, 128], BF16); Bvn = [S.buf(f"vn{h}") for h in range(4)]
        QSs = sb("QSs", [128, 4, 128]); BQSs = [S.buf(f"QSs{h}") for h in range(4)]
        ot = sb("ot", [128, 4, 128]); Bot = [S.buf(f"ot{h}") for h in range(4)]
        om = sb("om", [128, 4, 128], BF16); Bom = [S.buf(f"om{h}") for h in range(4)]
        ost = sb("ost", [128, 4, 2]); Bost = [S.buf(f"ost{h}") for h in range(4)]
        sc = [sb(f"sc{i}", [128, 512]) for i in range(2)]; Bsc = [S.buf(f"sc{i}") for i in range(2)]
        pTt = sb("pTt", [128, 2, 512], BF16); BpT = [S.buf(f"pT{i}") for i in range(2)]
        den = sb("den", [128, 512]); Bden = S.buf("den")
        x1t = [sb(f"x1t{i}", [128, 1024]) for i in range(1)] * 2; Bx1t = [S.buf(f"x1t{i}") for i in range(1)] * 2
        Bout = S.buf("out")
        BP4 = [BPB[4] for h in range(4)]

        S.op("pool", lambda e: e.memset(halo[:], 0.0), writes=Bhalo)
        S.op("pool", lambda e: e.memset(S32[:], 0.0), writes=BS)
        S.op("pool", lambda e: e.memset(Sbf[:], 0.0), writes=BS)
        S.op("pool", lambda e: e.memset(ZPR[:], 0.0), writes=BZPR)
        S.op("pool", lambda e: e.memset(swk[:], 0.0), writes=Bswk)
        S.op("pool", lambda e: e.memset(vv[:], 0.0), writes=Bvv)

        S.mark("m1")
        Bwcv = S.buf("wcv")
        conv_q = []
        if SPARSE and stage >= 2:
            for c in range(64):
                conv_q.append(lambda e, c=c: e.dma_start(out=wub_d[c * 64:(c + 1) * 64, :].rearrange("r (k n) -> r k n", k=8), in_=wupg_d[c * 64:(c + 1) * 64, :, :]))
                conv_q.append(lambda e, c=c: e.dma_start(out=wdb_d[c * 64:(c + 1) * 64, :].rearrange("r (k n) -> r k n", k=8), in_=wdng_d[c * 64:(c + 1) * 64, :, :]))

        def conv_step(n=1):
            for _ in range(n):
                if conv_q:
                    S.dma("pool", conv_q.pop(0), writes=[Bwcv])
        ident_b = csb(C_ID)
        ones_b = csb(C_ONE)
        bd_b = csb(C_BD)
        rr = {"fmps": 0, "eng": 0}

        def evac_eng():
            rr["eng"] += 1
            return "act" if rr["eng"] % 2 else "dve"

        def supertile(phase, sti):
            own = (phase == 1)
            xsrc = xo_d if own else xp_d
            for tl in range(4):
                gt = sti * 4 + tl
                i2 = gt % 2
                S.dma("sp", lambda e, gt=gt, tl=tl: e.dma_start(out=xt[tl][:], in_=xsrc[gt * 128:(gt + 1) * 128, :]), writes=[Bxt[tl]])
                S.mark("m2")
                S.op("act", lambda e, i2=i2, tl=tl: e.activation(out=xn[i2][:], in_=xt[tl][:], func=AF.Square, accum_out=stat[i2][:, 0:1]),
                     reads=[Bxt[tl]], writes=[Bxn[i2], Bstat[i2]])
                S.mark("a1")
                S.op("act", lambda e, i2=i2: e.activation(out=stat[i2][:, 1:2], in_=stat[i2][:, 0:1], func=AF.Sqrt, scale=1.0 / 1024, bias=EPS),
                     reads=[Bstat[i2]], writes=[Bstat[i2]])
                S.op("dve", lambda e, i2=i2: e.reciprocal(out=stat[i2][:, 1:2], in_=stat[i2][:, 1:2]), reads=[Bstat[i2]], writes=[Bstat[i2]])
                S.op("dve", lambda e, i2=i2, tl=tl: e.tensor_scalar(out=xn[i2][:], in0=xt[tl][:], scalar1=stat[i2][:, 1:2], scalar2=None, op0=ALU.mult),
                     reads=[Bxt[tl], Bstat[i2]], writes=[Bxn[i2]])
                S.mark("a2")
                for k in range(8):
                    S.op("pe", lambda e, i2=i2, k=k: e.transpose(PT[:, k * 128:(k + 1) * 128], xn[i2][:, k * 128:(k + 1) * 128], ident_b),
                         reads=[Bxn[i2], Bcstb], writes=[BPT[k]])
                    S.mark("a3")
                    eng = evac_eng()
                    if eng == "act":
                        S.op("act", lambda e, k=k, tl=tl: e.activation(out=hT[:, k, tl * 128:(tl + 1) * 128], in_=PT[:, k * 128:(k + 1) * 128],
                                                                      func=AF.Identity, scale=A1[:, k:k + 1], bias=B1[:, k:k + 1]),
                             reads=[BPT[k], BA1], writes=[BhT])
                        S.mark(f"ea{gt}_{k}")
                    else:
                        S.op("dve", lambda e, k=k, tl=tl: e.tensor_scalar(out=hT[:, k, tl * 128:(tl + 1) * 128], in0=PT[:, k * 128:(k + 1) * 128],
                                                                         scalar1=A1[:, k:k + 1], scalar2=B1[:, k:k + 1], op0=ALU.mult, op1=ALU.add),
                             reads=[BPT[k], BA1], writes=[BhT])
                        S.mark(f"ed{gt}_{k}")
            S.mark(f"p{phase}s{sti}a")
            chunks = list(range(18)) if own else ((list(range(0, 12)) if sti == 3 else list(range(4, 12))) + [16, 17])
            for ci, c in enumerate(chunks):
                conv_step(1)
                SH = 2 * ci
                ST = 2 * ci + 3
                cs_ = cs2[ci % 2]; Bcs = Bcs2[ci % 2]; sqb = sqb2[ci % 2]; Bsqb = Bsqb2[ci % 2]
                bi = rr["fmps"] % 2
                rr["fmps"] += 1
                pb = PB[bi]; Bpb = BPB[bi]
                for k in range(8):
                    G.op(SH, "pe", lambda e, cs_=cs_, sqb=sqb, pb=pb, c=c, k=k: e.matmul(pb[:, :], lhsT=wfm[:, k, c * 128:(c + 1) * 128], rhs=hT[:, k, :],
                                                                   start=(k == 0), stop=(k == 7)), reads=[Bwfm, BhT], writes=[Bpb])
                if c < 12:
                    ci = c % 2
                    U_ = Ub[ci]; BU_ = BUb[ci]
                    G.op(SH, "pool", lambda e, cs_=cs_, sqb=sqb, c=c, U_=U_: e.tensor_copy(out=U_[:, 1:4], in_=halo[:, c, 0:3]), reads=[Bhalo[c]], writes=[BU_])
                    G.op(SH, "act", lambda e, cs_=cs_, sqb=sqb, pb=pb, U_=U_: e.activation(out=U_[:, 4:516], in_=pb[:, :], func=AF.Identity), reads=[Bpb], writes=[BU_])
                    G.op(SH, "pool", lambda e, cs_=cs_, sqb=sqb, c=c, U_=U_: e.tensor_copy(out=halo[:, c, 0:3], in_=U_[:, 513:516]), reads=[BU_], writes=[Bhalo[c]])
                    ce = "dve"
                    cw = P_CONV + c * 4
                    G.op(SH, ce, lambda e, cs_=cs_, sqb=sqb, U_=U_, ci=ci, cw=cw: e.tensor_scalar(out=ctmp[ci][:], in0=U_[:, 1:513], scalar1=prm[:, cw:cw + 1], scalar2=None, op0=ALU.mult),
                         reads=[BU_, Bprm], writes=[Bctmp[ci]])
                    for j in range(1, 4):
                        if ce == "pool":
                            G.op(SH, ce, lambda e, cs_=cs_, sqb=sqb, U_=U_, cw=cw, j=j: e.tensor_scalar(out=ctmp2[:], in0=U_[:, 1 + j:513 + j], scalar1=prm[:, cw + j:cw + j + 1], scalar2=None, op0=ALU.mult),
                                 reads=[BU_, Bprm], writes=[Bctmp2])
                            G.op(SH, ce, lambda e, cs_=cs_, sqb=sqb, ci=ci: e.tensor_tensor(out=ctmp[ci][:], in0=ctmp[ci][:], in1=ctmp2[:], op=ALU.add), reads=[Bctmp[ci], Bctmp2], writes=[Bctmp[ci]])
                            continue
                        G.op(SH, ce, lambda e, cs_=cs_, sqb=sqb, U_=U_, ci=ci, cw=cw, j=j: e.scalar_tensor_tensor(out=ctmp[ci][:], in0=U_[:, 1 + j:513 + j], scalar=prm[:, cw + j:cw + j + 1],
                                                                                         in1=ctmp[ci][:], op0=ALU.mult, op1=ALU.add),
                             reads=[BU_, Bprm, Bctmp[ci]], writes=[Bctmp[ci]])
                    if c >= 8:
                        G.op(SH, "act", lambda e, cs_=cs_, sqb=sqb, c=c, ci=ci: e.activation(out=fm[:, c, :], in_=ctmp[ci][:], func=AF.Silu), reads=[Bctmp[ci]], writes=[Bfm[c]])
                    else:
                        G.op(SH, "act", lambda e, cs_=cs_, sqb=sqb, ci=ci: e.activation(out=cs_[:], in_=ctmp[ci][:], func=AF.Silu), reads=[Bctmp[ci]], writes=[Bcs])
                        G.op(SH, "pool", lambda e, cs_=cs_, sqb=sqb: e.tensor_tensor(out=sqb[:], in0=cs_[:], in1=cs_[:], op=ALU.mult), reads=[Bcs], writes=[Bsqb])
                        G.op(ST, "pe", lambda e, cs_=cs_, sqb=sqb: e.matmul(PB[2][:, :], lhsT=ones_b, rhs=sqb[:], start=True, stop=True), reads=[Bsqb, Bcstb], writes=[BPB[2]])
                        G.op(ST, "act", lambda e, cs_=cs_, sqb=sqb: e.activation(out=rst[:], in_=PB[2][:, :], func=AF.Sqrt, bias=EPS, scale=1.0), reads=[BPB[2]], writes=[Brst])
                        G.op(ST, "dve", lambda e, cs_=cs_, sqb=sqb: e.reciprocal(out=rst[:], in_=rst[:]), reads=[Brst], writes=[Brst])
                        qs = (128.0 ** -0.5) if c < 4 else 1.0
                        G.op(ST, "dve", lambda e, cs_=cs_, sqb=sqb, c=c, qs=qs: e.scalar_tensor_tensor(out=fm[:, c, :], in0=cs_[:], scalar=qs, in1=rst[:], op0=ALU.mult, op1=ALU.mult),
                             reads=[Bcs, Brst], writes=[Bfm[c]])
                else:
                    G.op(SH, "act", lambda e, cs_=cs_, sqb=sqb, pb=pb: e.activation(out=cs_[:], in_=pb[:, :], func=AF.Identity), reads=[Bpb], writes=[Bcs])
                    G.op(SH, "pool", lambda e, cs_=cs_, sqb=sqb: e.tensor_tensor(out=sqb[:], in0=cs_[:], in1=cs_[:], op=ALU.mult), reads=[Bcs], writes=[Bsqb])
                    G.op(ST, "pe", lambda e, cs_=cs_, sqb=sqb: e.matmul(PB[2][:, :], lhsT=bd_b, rhs=sqb[:], start=True, stop=True), reads=[Bsqb, Bcstb], writes=[BPB[2]])
                    G.op(ST, "act", lambda e, cs_=cs_, sqb=sqb: e.activation(out=rst[:], in_=PB[2][:, :], func=AF.Sqrt, bias=EPS, scale=1.0 / 64), reads=[BPB[2]], writes=[Brst])
                    G.op(ST, "dve", lambda e, cs_=cs_, sqb=sqb: e.reciprocal(out=rst[:], in_=rst[:]), reads=[Brst], writes=[Brst])
                    if c < 16:
                        for par in range(2):
                            G.op(ST, "dve", lambda e, cs_=cs_, sqb=sqb, c=c, par=par: e.scalar_tensor_tensor(out=swq[:, 2 * (c - 12) + par, :], in0=cs_[:], scalar=prm[:, (P_QNW if par == 0 else P_QNW_HI):(P_QNW if par == 0 else P_QNW_HI) + 1], in1=rst[:],
                                                                                      op0=ALU.mult, op1=ALU.mult), reads=[Bcs, Brst, Bprm], writes=[Bswq[c - 12]])
                    else:
                        j = c - 16
                        G.op(ST, "pool", lambda e, cs_=cs_, sqb=sqb, j=j: e.tensor_copy(out=swk[:, j, 0:128], in_=swk[:, j, 512:640]), reads=[Bswk[j]], writes=[Bswk[j]])
                        G.op(ST, "dve", lambda e, cs_=cs_, sqb=sqb, j=j: e.scalar_tensor_tensor(out=swk[:, j, 128:640], in0=cs_[:], scalar=prm[:, P_KNW:P_KNW + 1], in1=rst[:],
                                                                         op0=ALU.mult, op1=ALU.mult), reads=[Bcs, Brst, Bprm], writes=[Bswk[j]])
            G.run()
            S.mark(f"p{phase}s{sti}b")
            S.op("pool", lambda e: e.tensor_copy(out=vv[:, 0, :, :], in_=vv[:, 4, :, :]), reads=[Bvv[4]], writes=[Bvv[0]])
            for tl in range(4):
                tsl = slice(tl * 128, (tl + 1) * 128)
                if own:
                    for k in range(8):
                        S.op("pe", lambda e, k=k, tsl=tsl: e.matmul(PB[3][:, :], lhsT=hT[:, k, tsl], rhs=wtm[:, k, 0:512], start=(k == 0), stop=(k == 7)),
                             reads=[BhT, Bwtm], writes=[BPB[3]])
                    S.op("act", lambda e, tl=tl: e.activation(out=siluz[:, tl, :], in_=PB[3][:, :], func=AF.Silu), reads=[BPB[3]], writes=[Bsz[tl]])
                for k in range(8):
                    S.op("pe", lambda e, k=k, tsl=tsl: e.matmul(PB[4][:, 0:136], lhsT=hT[:, k, tsl], rhs=wtm[:, k, 512:648], start=(k == 0), stop=(k == 7)),
                         reads=[BhT, Bwtm], writes=[BPB[4]])
                T = tmf[:, tl, :]
                Bt = Btmf[tl]
                S.op("dve", lambda e, T=T: e.tensor_tensor(out=T[:, 28:32], in0=PB[4][:, 0:4], in1=prm[:, P_DTB:P_DTB + 4], op=ALU.add), reads=[BPB[4], Bprm], writes=[Bt])
                S.op("act", lambda e, T=T: e.activation(out=T[:, 28:32], in_=T[:, 28:32], func=AF.Exp), reads=[Bt], writes=[Bt])
                S.op("act", lambda e, T=T: e.activation(out=T[:, 28:32], in_=T[:, 28:32], func=AF.Ln, bias=1.0, scale=1.0), reads=[Bt], writes=[Bt])
                S.op("dve", lambda e, T=T: e.tensor_tensor(out=T[:, 0:4], in0=T[:, 28:32], in1=nA[:], op=ALU.mult), reads=[Bt, Bder], writes=[Bt])
                S.op("act", lambda e, T=T: e.activation(out=T[:, 4:8], in_=PB[4][:, 4:8], func=AF.Sigmoid), reads=[BPB[4]], writes=[Bt])
                S.op("dve", lambda e, T=T: e.tensor_scalar(out=T[:, 24:28], in0=T[:, 4:8], scalar1=-1.0, scalar2=None, op0=ALU.mult), reads=[Bt], writes=[Bt])
                for j in range(2):
                    for d in range(2):
                        S.op("act" if d == 0 else "dve",
                             (lambda e, tl=tl, j=j, d=d: e.activation(out=vv[:, 1 + tl, j, d * 64:(d + 1) * 64], in_=PB[4][:, 8 + j * 64: 8 + (j + 1) * 64], func=AF.Identity))
                             if d == 0 else
                             (lambda e, tl=tl, j=j, d=d: e.tensor_copy(out=vv[:, 1 + tl, j, d * 64:(d + 1) * 64], in_=PB[4][:, 8 + j * 64: 8 + (j + 1) * 64])),
                             reads=[BPB[4]], writes=[Bvv[1 + tl]])
                S.op("pe", lambda e, T=T: e.matmul(PB[5][:, 0:4], lhsT=cs(C_UI), rhs=T[:, 0:4], start=True, stop=True), reads=[Bt, Bcst], writes=[BPB[5]])
                S.op("pe", lambda e, T=T: e.matmul(PB[5][:, 4:8], lhsT=cs(C_ONE), rhs=T[:, 0:4], start=True, stop=True), reads=[Bt, Bcst], writes=[BPB[5]])
                S.op("act", lambda e, T=T: e.activation(out=T[:, 8:12], in_=PB[5][:, 0:4], func=AF.Exp), reads=[BPB[5]], writes=[Bt])
                S.op("dve", lambda e, T=T: e.tensor_scalar(out=T[:, 12:16], in0=T[:, 8:12], scalar1=-1.0, scalar2=None, op0=ALU.mult), reads=[Bt], writes=[Bt])
                S.op("act", lambda e, T=T: e.activation(out=T[:, 28:32], in_=PB[5][:, 0:4], func=AF.Identity), reads=[BPB[5]], writes=[Bt])
                S.op("dve", lambda e, T=T: e.tensor_tensor(out=T[:, 28:32], in0=PB[5][:, 4:8], in1=T[:, 28:32], op=ALU.subtract), reads=[BPB[5], Bt], writes=[Bt])
                S.op("act", lambda e, T=T: e.activation(out=T[:, 16:20], in_=T[:, 28:32], func=AF.Exp), reads=[Bt], writes=[Bt])
                S.op("act", lambda e, T=T: e.activation(out=T[:, 20:24], in_=PB[5][:, 4:8], func=AF.Exp), reads=[BPB[5]], writes=[Bt])
            S.mark(f"p{phase}s{sti}b2")
            if dbg and phase == 0 and sti == 0:
                Bdbg = S.buf("dbg")
                S.dma("pool", lambda e: e.dma_start(out=dbg_d[:, 0:1536].rearrange("p (c t) -> p c t", c=12), in_=fm[:, :, 0:128]), reads=Bfm, writes=[Bdbg])
                S.dma("sp", lambda e: e.dma_start(out=dbg_d[:, 1536:1568], in_=tmf[:, 0, :]), reads=Btmf, writes=[Bdbg])
                S.dma("sp", lambda e: e.dma_start(out=dbg_d[:, 1600:1608], in_=A1[:, :]), reads=[BA1], writes=[Bdbg])
                S.dma("sp", lambda e: e.dma_start(out=dbg_d[:, 1608:1616], in_=B1[:, :]), reads=[BA1], writes=[Bdbg])
                S.dma("pool", lambda e: e.dma_start(out=dbg_d[:, 2048:3072].rearrange("p (c t) -> p c t", c=8), in_=hT[:, :, 0:128]), reads=[BhT], writes=[Bdbg])
            def dn_pre(tl):
                tsl = slice(tl * 128, (tl + 1) * 128)
                tb = tl % 2
                T = tmf[:, tl, :]
                Bt = Btmf[tl]
                for h in range(4):
                    Bd = Bdn[tb][h]
                    qT = fm[:, h, tsl]; kT = fm[:, 4 + h, tsl]; vT = fm[:, 8 + h, tsl]
                    Bq, Bk, Bv = Bfm[h], Bfm[4 + h], Bfm[8 + h]
                    G.op(0, "pe", lambda e, kT=kT, h=h: e.transpose(PT[:, h * 128:(h + 1) * 128], kT, ident_b), reads=[Bk, Bcstb], writes=[BPT[h]])
                    G.op(1, "act", lambda e, tl=tl, tb=tb, h=h, T=T: e.activation(out=Kdec[:, tb, h, :], in_=PT[:, h * 128:(h + 1) * 128], func=AF.Identity, scale=T[:, 16 + h:17 + h]),
                         reads=[BPT[h], Bt], writes=[Bd])
                    G.op(0, "pe", lambda e, vT=vT, h=h: e.transpose(PT[:, (4 + h) * 128:(5 + h) * 128], vT, ident_b), reads=[Bv, Bcstb], writes=[BPT[4 + h]])
                    G.op(1, "dve", lambda e, tl=tl, tb=tb, h=h: e.tensor_copy(out=Vtm[:, tb, h, :], in_=PT[:, (4 + h) * 128:(5 + h) * 128]), reads=[BPT[4 + h]], writes=[Bd])
                    pb = PB[h]; Bpb = BPB[h]
                    G.op(2, "pe", lambda e, pb=pb, kT=kT: e.matmul(pb[:, 0:128], lhsT=kT, rhs=kT, start=True, stop=True), reads=[Bk], writes=[Bpb])
                    if own:
                        G.op(2, "pe", lambda e, pb=pb, kT=kT, qT=qT: e.matmul(pb[:, 128:256], lhsT=kT, rhs=qT, start=True, stop=True), reads=[Bk, Bq], writes=[Bpb])
                    G.op(2, "pool", lambda e, h=h, T=T: e.tensor_scalar(out=lg[:, h, :], in0=cs(C_SL), scalar1=T[:, h:h + 1], scalar2=None, op0=ALU.mult),
                         reads=[Bcst, Bt], writes=[Blg[h]])
                    G.op(3, "pe", lambda e, pb=pb, h=h: e.matmul(pb[:, 256:384], lhsT=lg[:, h, :], rhs=cs(C_UI), start=True, stop=False), reads=[Blg[h], Bcst], writes=[Bpb])
                    G.op(3, "pe", lambda e, pb=pb: e.matmul(pb[:, 256:384], lhsT=cs(C_ID), rhs=cs(C_NEG), start=False, stop=True), reads=[Bcst], writes=[Bpb])
                    G.op(4, "act", lambda e, pb=pb, h=h: e.activation(out=DT[:, h, :], in_=pb[:, 256:384], func=AF.Exp), reads=[Bpb], writes=[BDT[h]])
                    if own:
                        G.op(5, "dve", lambda e, pb=pb, h=h, tl=tl, tb=tb: e.tensor_tensor(out=Aqk[:, tb, h, :], in0=pb[:, 128:256], in1=DT[:, h, :], op=ALU.mult),
                             reads=[Bpb, BDT[h]], writes=[Bd])
                    G.op(5, "dve", lambda e, pb=pb, h=h, T=T: e.scalar_tensor_tensor(out=lg[:, h, :], in0=pb[:, 0:128], scalar=T[:, 24 + h:25 + h], in1=DT[:, h, :],
                                                                                 op0=ALU.mult, op1=ALU.mult), reads=[Bpb, Bt, BDT[h], Blg[h]], writes=[Blg[h]])
                    G.op(6, "pool", lambda e, h=h: e.tensor_tensor(out=Bfull[:, h, 0, :], in0=lg[:, h, :], in1=cs(C_OFFD), op=ALU.mult), reads=[Blg[h], Bcst], writes=[BBf[h]])
                    G.op(7, "pe", lambda e, h=h: e.transpose(PT[:, h * 128:(h + 1) * 128], Bfull[:, h, 0, :], ident_b), reads=[BBf[h], Bcstb], writes=[BPT[h]])
                    G.op(8, "act", lambda e, h=h: e.activation(out=Bfull[:, h, 1, :], in_=PT[:, h * 128:(h + 1) * 128], func=AF.Identity), reads=[BPT[h]], writes=[BBf[h]])
                    G.op(9, "pool", lambda e, h=h: e.tensor_tensor(out=ZPR[:, h, 1, :], in0=Bfull[:, h, 0, :], in1=cstm[:, 0:128], op=ALU.mult), reads=[BBf[h], Bcstm], writes=[BZPR[h]])
                    G.op(9, "pool", lambda e, h=h: e.tensor_copy(out=ZPR[:, h, 2, :], in_=cs(C_ID)), reads=[Bcst], writes=[BZPR[h]])
                    G.op(9, "pool", lambda e, h=h: e.tensor_tensor(out=PTt[:, h, :], in0=Bfull[:, h, 1, :], in1=cstm[:, 0:128], op=ALU.mult), reads=[BBf[h], Bcstm], writes=[BPTt[h]])
                    G.op(9, "pool", lambda e, h=h: e.tensor_tensor(out=WT[:, h, 0, :], in0=Bfull[:, h, 1, :], in1=cstm[:, 128:256], op=ALU.mult), reads=[BBf[h], Bcstm], writes=[BWT[h]])
                    G.op(9, "pool", lambda e, h=h: e.tensor_tensor(out=WT[:, h, 1, :], in0=Bfull[:, h, 1, :], in1=cstm[:, 256:384], op=ALU.mult), reads=[BBf[h], Bcstm], writes=[BWT[h]])
                G.run()
                for lvl in range(5):
                    for h in range(4):
                        pb = PB[h]; Bpb = BPB[h]
                        if lvl < 4:
                            S.op("pe", lambda e, pb=pb, h=h: e.matmul(pb[:, 0:256], lhsT=PTt[:, h, :], rhs=ZPR[:, h, 1:3, :], start=True, stop=True),
                                 reads=[BPTt[h], BZPR[h]], writes=[Bpb])
                            S.op("pe", lambda e, pb=pb, h=h: e.matmul(pb[:, 256:384], lhsT=ZPR[:, h, 1, :], rhs=PTt[:, h, :], start=True, stop=True),
                                 reads=[BPTt[h], BZPR[h]], writes=[Bpb])
                            S.op("dve", lambda e, pb=pb, h=h: e.tensor_tensor(out=ZPR[:, h, 1:3, :], in0=pb[:, 0:256].rearrange("p (a b) -> p a b", a=2),
                                                                             in1=ZPR[:, h, 0:3:2, :], op=ALU.add), reads=[Bpb, BZPR[h]], writes=[BZPR[h]])
                            S.op("act", lambda e, pb=pb, h=h: e.activation(out=PTt[:, h, :], in_=pb[:, 256:384], func=AF.Identity), reads=[Bpb], writes=[BPTt[h]])
                        else:
                            S.op("pe", lambda e, pb=pb, h=h: e.matmul(pb[:, 0:128], lhsT=PTt[:, h, :], rhs=ZPR[:, h, 2, :], start=True, stop=True),
                                 reads=[BPTt[h], BZPR[h]], writes=[Bpb])
                            S.op("dve", lambda e, pb=pb, h=h: e.tensor_tensor(out=ZPR[:, h, 1, :], in0=pb[:, 0:128], in1=ZPR[:, h, 2, :], op=ALU.add),
                                 reads=[Bpb, BZPR[h]], writes=[BZPR[h]])
                for h in range(4):
                    S.op("pe", lambda e, h=h: e.transpose(PT[:, h * 128:(h + 1) * 128], ZPR[:, h, 1, :], ident_b), reads=[BZPR[h], Bcstb], writes=[BPT[h]])
                    S.op("act", lambda e, h=h: e.activation(out=PTt[:, h, :], in_=PT[:, h * 128:(h + 1) * 128], func=AF.Identity), reads=[BPT[h]], writes=[BPTt[h]])
                for h in range(4):
                    pb = PB[h]; Bpb = BPB[h]
                    S.op("pe", lambda e, pb=pb, h=h: e.matmul(pb[:, 0:128], lhsT=WT[:, h, 0, :], rhs=ZPR[:, h, 1, :], start=True, stop=True), reads=[BWT[h], BZPR[h]], writes=[Bpb])
                    S.op("act", lambda e, pb=pb, h=h: e.activation(out=T1s[:, h, :], in_=pb[:, 0:128], func=AF.Identity), reads=[Bpb], writes=[BT1[h]])
                for h in range(4):
                    pb = PB[h]; Bpb = BPB[h]
                    S.op("pe", lambda e, pb=pb, h=h: e.matmul(pb[:, 0:128], lhsT=PTt[:, h, :], rhs=T1s[:, h, :], start=True, stop=True), reads=[BPTt[h], BT1[h]], writes=[Bpb])
                    S.op("pe", lambda e, pb=pb, h=h: e.matmul(pb[:, 128:256], lhsT=T1s[:, h, :], rhs=PTt[:, h, :], start=True, stop=True), reads=[BPTt[h], BT1[h]], writes=[Bpb])
                    S.op("dve", lambda e, pb=pb, h=h: e.tensor_tensor(out=ZPR[:, h, 2, :], in0=pb[:, 0:128], in1=ZPR[:, h, 1, :], op=ALU.add), reads=[Bpb, BZPR[h]], writes=[BZPR[h]])
                    S.op("dve", lambda e, pb=pb, h=h: e.tensor_tensor(out=D2T[:, h, :], in0=pb[:, 128:256], in1=PTt[:, h, :], op=ALU.add), reads=[Bpb, BPTt[h]], writes=[BD2T[h]])
                for h in range(4):
                    pb = PB[h]; Bpb = BPB[h]
                    S.op("pe", lambda e, pb=pb, h=h: e.matmul(pb[:, 0:128], lhsT=WT[:, h, 1, :], rhs=ZPR[:, h, 2, :], start=True, stop=True), reads=[BWT[h], BZPR[h]], writes=[Bpb])
                    S.op("act", lambda e, pb=pb, h=h: e.activation(out=T1s[:, h, :], in_=pb[:, 0:128], func=AF.Identity), reads=[Bpb], writes=[BT1[h]])
                for h in range(4):
                    pb = PB[h]; Bpb = BPB[h]
                    S.op("pe", lambda e, pb=pb, h=h: e.matmul(pb[:, 0:128], lhsT=D2T[:, h, :], rhs=T1s[:, h, :], start=True, stop=True), reads=[BD2T[h], BT1[h]], writes=[Bpb])
                    S.op("dve", lambda e, pb=pb, h=h, tb=tb: e.tensor_tensor(out=Minv[:, tb, h, :], in0=pb[:, 0:128], in1=ZPR[:, h, 2, :], op=ALU.add),
                         reads=[Bpb, BZPR[h]], writes=[Bdn[tb][h]])
            def dn_scan(tl):
                tsl = slice(tl * 128, (tl + 1) * 128)
                tb = tl % 2
                T = tmf[:, tl, :]
                Bt = Btmf[tl]
                for h in range(4):
                    Bd = Bdn[tb][h]
                    pb = PB[h]; Bpb = BPB[h]
                    qT = fm[:, h, tsl]; kT = fm[:, 4 + h, tsl]
                    G.op(0, "pe", lambda e, pb=pb, kT=kT, h=h: e.matmul(pb[:, 0:128], lhsT=kT, rhs=Sbf[:, h, :], start=True, stop=True), reads=[Bfm[4 + h], BS[h]], writes=[Bpb])
                    if own:
                        G.op(0, "pe", lambda e, pb=pb, qT=qT, h=h: e.matmul(pb[:, 128:256], lhsT=qT, rhs=Sbf[:, h, :], start=True, stop=True), reads=[Bfm[h], BS[h]], writes=[Bpb])
                    G.op(1, "dve", lambda e, pb=pb, h=h, tl=tl, tb=tb, T=T: e.scalar_tensor_tensor(out=Rm[:, h, :], in0=pb[:, 0:128], scalar=T[:, 12 + h:13 + h], in1=Vtm[:, tb, h, :],
                                                                                        op0=ALU.mult, op1=ALU.add), reads=[Bpb, Bt, Bd], writes=[BRm[h]])
                    if own:
                        G.op(1, "act", lambda e, pb=pb, h=h, T=T: e.activation(out=QSs[:, h, :], in_=pb[:, 128:256], func=AF.Identity, scale=T[:, 8 + h:9 + h]),
                             reads=[Bpb, Bt], writes=[BQSs[h]])
                    G.op(2, "pe", lambda e, pb=pb, h=h, tl=tl, tb=tb: e.matmul(pb[:, 256:384], lhsT=Minv[:, tb, h, :], rhs=Rm[:, h, :], start=True, stop=True), reads=[Bd, BRm[h]], writes=[Bpb])
                    G.op(3, "act", lambda e, pb=pb, h=h, T=T: e.activation(out=vnew[:, h, :], in_=pb[:, 256:384], func=AF.Identity, scale=T[:, 4 + h:5 + h]),
                         reads=[Bpb, Bt], writes=[Bvn[h]])
                    p4 = PB[4][:, h * 128:(h + 1) * 128]
                    G.op(4, "pe", lambda e, p4=p4, h=h, tl=tl, tb=tb: e.matmul(p4, lhsT=Kdec[:, tb, h, :], rhs=vnew[:, h, :], start=True, stop=True), reads=[Bd, Bvn[h]], writes=[BP4[h]])
                    if own:
                        G.op(4, "pe", lambda e, pb=pb, h=h, tl=tl, tb=tb: e.matmul(pb[:, 384:512], lhsT=Aqk[:, tb, h, :], rhs=vnew[:, h, :], start=True, stop=True), reads=[Bd, Bvn[h]], writes=[Bpb])
                    G.op(5, "dve", lambda e, p4=p4, h=h, T=T: e.scalar_tensor_tensor(out=S32[:, h, :], in0=S32[:, h, :], scalar=T[:, 20 + h:21 + h], in1=p4, op0=ALU.mult, op1=ALU.add),
                         reads=[BP4[h], Bt, BS[h]], writes=[BS[h]])
                    G.op(6, "act", lambda e, h=h: e.activation(out=Sbf[:, h, :], in_=S32[:, h, :], func=AF.Identity), reads=[BS[h]], writes=[BS[h]])
                    if own:
                        G.op(5, "dve", lambda e, pb=pb, h=h: e.tensor_tensor(out=ot[:, h, :], in0=pb[:, 384:512], in1=QSs[:, h, :], op=ALU.add), reads=[Bpb, BQSs[h]], writes=[Bot[h]])
                        G.op(7, "act", lambda e, h=h: e.activation(out=QSs[:, h, :], in_=ot[:, h, :], func=AF.Square, accum_out=ost[:, h, 0:1]), reads=[Bot[h]], writes=[BQSs[h], Bost[h]])
                        G.op(8, "act", lambda e, h=h: e.activation(out=ost[:, h, 1:2], in_=ost[:, h, 0:1], func=AF.Sqrt, scale=1.0 / 128, bias=EPS), reads=[Bost[h]], writes=[Bost[h]])
                        G.op(9, "dve", lambda e, h=h: e.reciprocal(out=ost[:, h, 1:2], in_=ost[:, h, 1:2]), reads=[Bost[h]], writes=[Bost[h]])
                        G.op(10, "dve", lambda e, h=h: e.scalar_tensor_tensor(out=ot[:, h, :], in0=ot[:, h, :], scalar=ost[:, h, 1:2], in1=prm[:, P_DNW:P_DNW + 128], op0=ALU.mult, op1=ALU.mult),
                             reads=[Bot[h], Bost[h], Bprm], writes=[Bot[h]])
                        G.op(11, "pool", lambda e, h=h, tl=tl, tb=tb: e.tensor_tensor(out=om[:, h, :], in0=ot[:, h, :], in1=siluz[:, tl, h * 128:(h + 1) * 128], op=ALU.mult),
                             reads=[Bot[h], Bsz[tl]], writes=[Bom[h]])
                        G.op(12, "pe", lambda e, h=h: e.transpose(PT[:, h * 128:(h + 1) * 128], om[:, h, :], ident_b), reads=[Bom[h], Bcstb], writes=[BPT[h]])
                        G.op(13, "act", lambda e, h=h, tsl=tsl: e.activation(out=mixT[:, h, tsl], in_=PT[:, h * 128:(h + 1) * 128], func=AF.Identity), reads=[BPT[h]], writes=[Bmix[tl]])
                G.run()
            dn_pre(0)
            if dbg and phase == 0 and sti == 0:
                for i_, src in enumerate((Kdec, Vtm, Minv)):
                    S.dma("pool", lambda e, i_=i_, src=src: e.dma_start(out=dbg_d[:, 2048 + i_ * 512: 2048 + (i_ + 1) * 512].rearrange("p (h t) -> p h t", h=4), in_=src[:, 0, :, :]),
                          reads=Bdn[0] + [Bdbg], writes=[Bdbg])
            S.mark(f"p{phase}s{sti}c")
            dn_pre(1)
            dn_scan(0)
            dn_pre(2)
            dn_scan(1)
            dn_pre(3)
            dn_scan(2)
            dn_scan(3)
            S.mark(f"p{phase}s{sti}d")
            if not own:
                return
            for tl in range(4):
                gt = sti * 4 + tl
                for j in range(2):
                    for kb in range(2):
                        k0 = tl * 128 + kb * 128
                        pb = PB[kb]; Bpb = BPB[kb]
                        for g in range(4):
                            h = j * 4 + g
                            pr = (h % 2) * 64
                            S.op("pe", lambda e, pb=pb, j=j, k0=k0, pr=pr, h=h, g=g, tl=tl: e.matmul(
                                pb[:, g * 128:(g + 1) * 128], lhsT=swk[:, j, k0:k0 + 128], rhs=swq[:, h, tl * 128:(tl + 1) * 128],
                                start=True, stop=True), reads=[Bswk[j], Bswq[h // 2]], writes=[Bpb])
                        bofs = C_SWB + (kb * 8 + j * 4) * 128
                        S.op("dve", lambda e, pb=pb, kb=kb, bofs=bofs: e.scalar_tensor_tensor(out=sc[kb][:], in0=pb[:, :], scalar=0.125, in1=cst[:, bofs:bofs + 512],
                                                                                            op0=ALU.mult, op1=ALU.add), reads=[Bpb, Bcst], writes=[Bsc[kb]])
                        if kb == 0 and gt == 0:
                            S.op("act", lambda e, kb=kb: e.activation(out=pTt[:, kb, :], in_=sc[kb][:], func=AF.Exp, bias=prm[:, P_HALO:P_HALO + 1], scale=1.0),
                                 reads=[Bsc[kb], Bprm], writes=[BpT[kb]])
                        else:
                            S.op("act", lambda e, kb=kb: e.activation(out=pTt[:, kb, :], in_=sc[kb][:], func=AF.Exp), reads=[Bsc[kb]], writes=[BpT[kb]])
                    for kb in range(2):
                        S.op("pe", lambda e, kb=kb, tl=tl, j=j: e.matmul(PB[2][:, :], lhsT=vv[:, tl + kb, j, :], rhs=pTt[:, kb, :], start=(kb == 0), stop=(kb == 1)),
                             reads=[Bvv[tl + kb], BpT[kb]], writes=[BPB[2]])
                    for kb in range(2):
                        S.op("pe", lambda e, kb=kb: e.matmul(PB[3][:, :], lhsT=ones_b, rhs=pTt[:, kb, :], start=(kb == 0), stop=(kb == 1)),
                             reads=[Bcstb, BpT[kb]], writes=[BPB[3]])
                    for g in range(4):
                        h = j * 4 + g
                        S.op("dve", lambda e, g=g, h=h: e.tensor_scalar(out=den[:, g * 128:(g + 1) * 128], in0=PB[3][:, g * 128:(g + 1) * 128], scalar1=esink[:, h:h + 1],
                                                                       scalar2=None, op0=ALU.add), reads=[BPB[3], Bder], writes=[Bden])
                    S.op("dve", lambda e: e.reciprocal(out=den[:], in_=den[:]), reads=[Bden], writes=[Bden])
                    for g in range(4):
                        h = j * 4 + g
                        pr = (h % 2) * 64
                        S.op("dve", lambda e, g=g, h=h, pr=pr, tl=tl: e.tensor_tensor(out=mixT[pr:pr + 64, 4 + h // 2, tl * 128:(tl + 1) * 128],
                                                                                     in0=PB[2][pr:pr + 64, g * 128:(g + 1) * 128], in1=den[pr:pr + 64, g * 128:(g + 1) * 128], op=ALU.mult),
                             reads=[BPB[2], Bden], writes=[Bmix[tl]])
            S.mark(f"p{phase}s{sti}e")
            for tl in range(4):
                gt = sti * 4 + tl
                i2 = gt % 2
                for hh in range(2):
                    pb = PB[5 + hh]; Bpb = BPB[5 + hh]
                    for k in range(8):
                        S.op("pe", lambda e, pb=pb, k=k, tl=tl, hh=hh: e.matmul(pb[:, :], lhsT=mixT[:, k, tl * 128:(tl + 1) * 128], rhs=wout[:, k, hh * 512:(hh + 1) * 512],
                                                                               start=(k == 0), stop=(k == 7)), reads=[Bmix[tl], Bwout], writes=[Bpb])
                    S.op("dve", lambda e, pb=pb, i2=i2, hh=hh: e.tensor_tensor(out=x1t[i2][:, hh * 512:(hh + 1) * 512], in0=pb[:, :], in1=gate1[:, hh * 512:(hh + 1) * 512], op=ALU.mult),
                         reads=[Bpb, Bmodbc], writes=[Bx1t[i2]])
                    S.op("pool", lambda e, tl=tl, hh=hh, i2=i2: e.tensor_tensor(out=x1t[i2][:, hh * 512:(hh + 1) * 512], in0=x1t[i2][:, hh * 512:(hh + 1) * 512],
                                                                               in1=xt[tl][:, hh * 512:(hh + 1) * 512], op=ALU.add), reads=[Bx1t[i2], Bxt[tl]], writes=[Bx1t[i2]])
                S.dma("sp", lambda e, gt=gt, i2=i2: e.dma_start(out=out_d[gt * 128:(gt + 1) * 128, :], in_=x1t[i2][:]), reads=[Bx1t[i2]], writes=[Bout])

        for phase in range(2):
            for sti in range(4):
                supertile(phase, sti)
            if phase == 0:
                for h in range(4):
                    S.op("dve", lambda e, h=h: e.tensor_scalar(out=S32[:, h, :], in0=S32[:, h, :], scalar1=prm[:, P_FLAG:P_FLAG + 1], scalar2=None, op0=ALU.mult),
                         reads=[BS[h], Bprm], writes=[BS[h]])
                    S.op("act", lambda e, h=h: e.activation(out=Sbf[:, h, :], in_=S32[:, h, :], func=AF.Identity), reads=[BS[h]], writes=[BS[h]])
                for c in range(12):
                    S.op("dve", lambda e, c=c: e.tensor_scalar(out=halo[:, c, :], in0=halo[:, c, :], scalar1=prm[:, P_FLAG:P_FLAG + 1], scalar2=None, op0=ALU.mult),
                         reads=[Bhalo[c], Bprm], writes=[Bhalo[c]])


        conv_step(1000)
        S.mark("mixer_done")
        S.barrier()
        S.flush(); st.close()
        st = ExitStack()

        if stage >= 2 and SPARSE and not S.stopped:
            xs_d = nc.dram_tensor("xs", [NBLK * 128, 1024], BF16, kind="Internal").ap(); Bxs = S.buf("xs")
            ys_d = nc.dram_tensor("ysc", [NBLK * 128, 1024], F32, kind="Internal").ap(); Bys = S.buf("ys")
            h2s_d = nc.dram_tensor("h2s", [2048, 1024], BF16, kind="Internal").ap(); Bh2s = S.buf("h2s")
            sh2 = sb("sh2", [128, 1024]); w2s = sb("w2s", [128, 1024]); g2 = sb("g2", [128, 1024]); Bmb = S.buf("mb")
            for i_, dst in enumerate((sh2, w2s, g2)):
                S.dma("sp", lambda e, i_=i_, dst=dst: e.dma_start(out=dst[:], in_=scr_d[i_:i_ + 1, :].to_broadcast([128, 1024])), reads=[Bscr], writes=[Bmb])
            bdn = sb("bdn", [32, 1024]); Bbdn = S.buf("bdn")
            S.dma("sp", lambda e: e.dma_start(out=bdn[:], in_=bdn_d), writes=[Bbdn])
            prm2 = sb("prm2", [128, 32]); Bprm2 = S.buf("prm2")
            S.dma("sp", lambda e: e.dma_start(out=prm2[:], in_=prm2_d[:, 0:32]), writes=[Bprm2])
            prm3 = sb("prm3", [128, 160]); Bprm3 = S.buf("prm3")
            S.dma("sp", lambda e: e.dma_start(out=prm3[:], in_=prm3_d), writes=[Bprm3])
            Rk = sb("Rk", [128, 16, 32]); I4 = sb("I4", [128, 16, 4]); GK = sb("GK", [128, 16, 4]); Gt = sb("Gt", [128, 16, 32])
            DIf = sb("DIf", [128, 16, 4]); DI = sb("DI", [128, 64], I32)
            Brt = [S.buf(f"rt{t}") for t in range(16)]; BDI = [S.buf(f"DI{t}") for t in range(16)]
            cntbc = sb("cntbc", [128, 32]); Bcnt = S.buf("cnt")
            rtg = sb("rtg", [128, 4, 32]); Brtg = S.buf("rtg")
            ebf = sb("ebf", [128, 96]); sam = sb("sam", [128, 96]); idf = sb("idf", [128, 2, 96]); idxW = sb("idxW", [128, 192], I32); Beb = S.buf("eb")
            GT = sb("GT", [32, 128]); BGT = S.buf("GT")
            S.op("pool", lambda e: e.memset(cntbc[:], 0.0), writes=[Bcnt])
            st_moe = st
            st = ExitStack()
            wr = sb("wr", [128, 8, 32]); Bwr = S.buf("wr")
            S.dma("sp", lambda e: e.dma_start(out=wr[:], in_=wr_d.rearrange("(k p) n -> p k n", p=128)), writes=[Bwr])
            xb = sb("xb", [128, 1024]); Bxb = S.buf("xb")
            h2f = sb("h2f", [128, 1024]); Bh2f = S.buf("h2f")
            h2b = sb("h2b", [128, 1024], BF16); Bh2b = S.buf("h2b")
            h2Tf = sb("h2Tf", [128, 8, 128]); Bh2Tf = S.buf("h2Tf")
            SUb = sb("SUb", [128, 128], BF16); BSUb = S.buf("SUb")
            Mb = sb("Mb", [128, 32], BF16); BMb = S.buf("Mb")
            sm = sb("sm", [128, 64]); Bsm = S.buf("sm")
            i8 = sb("i8", [128, 8], U32); Bi8 = S.buf("i8")
            ex = sb("ex", [128, 32]); Bex = S.buf("ex")
            dtm = sb("dtm", [128, 2, 32]); Bdtm = S.buf("dtm")
            S.op("dve", lambda e: e.tensor_tensor(out=SUb[:], in0=cs(C_UI), in1=cs(C_OFFD), op=ALU.mult), reads=[Bcst], writes=[BSUb])
            for t in range(16):
                S.dma("sp", lambda e, t=t: e.dma_start(out=xb[:], in_=out_d[t * 128:(t + 1) * 128, :]), reads=[Bout], writes=[Bxb])
                S.op("act", lambda e: e.activation(out=h2f[:], in_=xb[:], func=AF.Square, accum_out=sm[:, 11:12]), reads=[Bxb], writes=[Bh2f, Bsm])
                S.op("act", lambda e: e.activation(out=sm[:, 12:13], in_=sm[:, 11:12], func=AF.Sqrt, scale=1.0 / 1024, bias=EPS), reads=[Bsm], writes=[Bsm])
                S.op("dve", lambda e: e.reciprocal(out=sm[:, 12:13], in_=sm[:, 12:13]), reads=[Bsm], writes=[Bsm])
                S.op("dve", lambda e: e.scalar_tensor_tensor(out=h2f[:], in0=xb[:], scalar=sm[:, 12:13], in1=w2s[:], op0=ALU.mult, op1=ALU.mult), reads=[Bxb, Bsm, Bmb, Bh2f], writes=[Bh2f])
                S.op("dve", lambda e: e.tensor_tensor(out=h2f[:], in0=h2f[:], in1=sh2[:], op=ALU.add), reads=[Bh2f, Bmb], writes=[Bh2f])
                S.op("act", lambda e: e.activation(out=h2b[:], in_=h2f[:], func=AF.Identity), reads=[Bh2f], writes=[Bh2b])
                S.dma("sp", lambda e, t=t: e.dma_start(out=h2s_d[t * 128:(t + 1) * 128, :], in_=h2b[:]), reads=[Bh2b], writes=[Bh2s])
                for b2 in range(2):
                    for kk in range(4):
                        k = b2 * 4 + kk
                        S.op("pe", lambda e, b2=b2, kk=kk, k=k: e.transpose(PB[b2][:, kk * 128:(kk + 1) * 128], h2f[:, k * 128:(k + 1) * 128], cs(C_ID)), reads=[Bh2f, Bcst], writes=[BPB[b2]])
                    S.op("dve", lambda e, b2=b2: e.tensor_copy(out=h2Tf[:, b2 * 4:(b2 + 1) * 4, :], in_=PB[b2][:, :].rearrange("p (a b) -> p a b", a=4)), reads=[BPB[b2]], writes=[Bh2Tf])
                for k in range(8):
                    S.op("pe", lambda e, k=k: e.matmul(PB[6][:, 0:32], lhsT=h2Tf[:, k, :], rhs=wr[:, k, :], start=(k == 0), stop=(k == 7)), reads=[Bh2Tf, Bwr], writes=[BPB[6]])
                S.op("dve", lambda e: e.tensor_tensor(out=sm[:, 16:48], in0=PB[6][:, 0:32], in1=prm2[:, 0:32], op=ALU.add), reads=[BPB[6], Bprm2, Bsm], writes=[Bsm])
                S.op("dve", lambda e: e.max(out=sm[:, 0:8], in_=sm[:, 16:48]), reads=[Bsm], writes=[Bsm])
                S.op("dve", lambda e: e.max_index(out=i8[:], in_max=sm[:, 0:8], in_values=sm[:, 16:48]), reads=[Bsm], writes=[Bi8])
                S.op("dve", lambda e, t=t: e.tensor_copy(out=I4[:, t, :], in_=i8[:, 0:4]), reads=[Bi8], writes=[Brt[t]])
                S.op("dve", lambda e: e.tensor_scalar(out=sm[:, 8:9], in0=sm[:, 0:1], scalar1=-1.0, scalar2=None, op0=ALU.mult), reads=[Bsm], writes=[Bsm])
                S.op("act", lambda e: e.activation(out=ex[:], in_=sm[:, 16:48], func=AF.Exp, bias=sm[:, 8:9], scale=1.0), reads=[Bsm], writes=[Bex])
                S.op("act", lambda e: e.activation(out=sm[:, 48:52], in_=sm[:, 0:4], func=AF.Exp, bias=sm[:, 8:9], scale=1.0), reads=[Bsm], writes=[Bsm])
                S.op("dve", lambda e: e.tensor_scalar(out=Mb[:], in0=sm[:, 16:48], scalar1=sm[:, 3:4], scalar2=None, op0=ALU.is_ge), reads=[Bsm], writes=[BMb])
                S.op("dve", lambda e: e.scalar_tensor_tensor(out=ex[:], in0=sm[:, 16:48], scalar=sm[:, 3:4], in1=ex[:], op0=ALU.is_ge, op1=ALU.mult), reads=[Bsm, Bex], writes=[Bex])
                S.op("dve", lambda e: e.reduce_sum(out=sm[:, 9:10], in_=ex[:], axis=AX.X), reads=[Bex, Bsm], writes=[Bsm])
                S.op("dve", lambda e: e.reciprocal(out=sm[:, 10:11], in_=sm[:, 9:10]), reads=[Bsm], writes=[Bsm])
                S.op("dve", lambda e, t=t: e.tensor_scalar(out=Gt[:, t, :], in0=ex[:], scalar1=sm[:, 10:11], scalar2=None, op0=ALU.mult), reads=[Bex, Bsm], writes=[Brt[t]])
                S.op("dve", lambda e, t=t: e.tensor_scalar(out=GK[:, t, :], in0=sm[:, 48:52], scalar1=sm[:, 10:11], scalar2=None, op0=ALU.mult), reads=[Bsm], writes=[Brt[t]])
                S.op("pe", lambda e: e.matmul(PB[6][:, 32:64], lhsT=SUb[:], rhs=Mb[:], start=True, stop=True), reads=[BSUb, BMb], writes=[BPB[6]])
                S.op("pe", lambda e: e.matmul(PB[6][:, 64:96], lhsT=ones_b, rhs=Mb[:], start=True, stop=True), reads=[Bcstb, BMb], writes=[BPB[6]])
                S.op("dve", lambda e, t=t: e.tensor_tensor(out=Rk[:, t, :], in0=PB[6][:, 32:64], in1=cntbc[:], op=ALU.add), reads=[BPB[6], Bcnt], writes=[Brt[t]])
                S.op("dve", lambda e: e.tensor_tensor(out=cntbc[:], in0=PB[6][:, 64:96], in1=cntbc[:], op=ALU.add), reads=[BPB[6], Bcnt], writes=[Bcnt])
                S.mark(f"m_fe{t}")
            S.op("dve", lambda e: e.tensor_scalar(out=rtg[:, 0, :], in0=cntbc[:], scalar1=0.0, scalar2=None, op0=ALU.is_gt), reads=[Bcnt], writes=[Brtg])
            for j in range(1, 16):
                S.op("dve", lambda e, j=j: e.scalar_tensor_tensor(out=rtg[:, 0, :], in0=cntbc[:], scalar=128.0 * j, in1=rtg[:, 0, :], op0=ALU.is_gt, op1=ALU.add), reads=[Bcnt, Brtg], writes=[Brtg])
            S.op("dve", lambda e: e.tensor_copy(out=rtg[:, 1, :], in_=rtg[:, 0, :]), reads=[Brtg], writes=[Brtg])
            a_, b_ = 1, 2
            for sh in (1, 2, 4, 8, 16):
                S.op("dve", lambda e, a_=a_, b_=b_, sh=sh: e.tensor_tensor(out=rtg[:, b_, sh:32], in0=rtg[:, a_, sh:32], in1=rtg[:, a_, 0:32 - sh], op=ALU.add), reads=[Brtg], writes=[Brtg])
                S.op("dve", lambda e, a_=a_, b_=b_, sh=sh: e.tensor_copy(out=rtg[:, b_, 0:sh], in_=rtg[:, a_, 0:sh]), reads=[Brtg], writes=[Brtg])
                a_, b_ = b_, a_
            incl = a_
            S.op("dve", lambda e, incl=incl: e.tensor_tensor(out=rtg[:, 3, :], in0=rtg[:, incl, :], in1=rtg[:, 0, :], op=ALU.subtract), reads=[Brtg], writes=[Brtg])
            S.op("dve", lambda e: e.tensor_scalar(out=rtg[:, 3, :], in0=rtg[:, 3, :], scalar1=128.0, scalar2=None, op0=ALU.mult), reads=[Brtg], writes=[Brtg])
            S.op("dve", lambda e, incl=incl: e.tensor_scalar(out=ebf[:], in0=prm3[:, 32:128], scalar1=rtg[:, incl, 0:1], scalar2=None, op0=ALU.is_ge), reads=[Brtg, Bprm3], writes=[Beb])
            for e_ in range(1, 32):
                S.op("dve", lambda e, e_=e_, incl=incl: e.scalar_tensor_tensor(out=ebf[:], in0=prm3[:, 32:128], scalar=rtg[:, incl, e_:e_ + 1], in1=ebf[:], op0=ALU.is_ge, op1=ALU.add),
                     reads=[Brtg, Bprm3, Beb], writes=[Beb])
            S.op("dve", lambda e: e.tensor_scalar(out=ebf[:], in0=ebf[:], scalar1=31.0, scalar2=None, op0=ALU.min), reads=[Beb], writes=[Beb])
            S.op("dve", lambda e: e.memset(sam[:], 0.0), writes=[Beb])
            S.op("dve", lambda e: e.tensor_tensor(out=sam[:, 2:96], in0=ebf[:, 2:96], in1=ebf[:, 0:94], op=ALU.is_equal), reads=[Beb], writes=[Beb])
            S.op("dve", lambda e: e.tensor_scalar(out=idf[:, 0, :], in0=ebf[:], scalar1=128.0, scalar2=prm3[:, 128:129], op0=ALU.mult, op1=ALU.add), reads=[Beb, Bprm3], writes=[Beb])
            S.op("dve", lambda e: e.scalar_tensor_tensor(out=idf[:, 1, :], in0=sam[:], scalar=1.0e6, in1=idf[:, 0, :], op0=ALU.mult, op1=ALU.add), reads=[Beb], writes=[Beb])
            S.op("dve", lambda e: e.tensor_copy(out=idxW[:], in_=idf[:, :, :].rearrange("p a b -> p (a b)")), reads=[Beb], writes=[Beb])
            S.mark("m_rt")
            for t in range(16):
                S.op("dve", lambda e, t=t: e.tensor_tensor(out=dtm[:, 0, :], in0=Rk[:, t, :], in1=rtg[:, 3, :], op=ALU.add), reads=[Brt[t], Brtg, Bdtm], writes=[Bdtm])
                for k in range(4):
                    S.op("dve", lambda e, t=t, k=k: e.scalar_tensor_tensor(out=dtm[:, 1, :], in0=prm3[:, 0:32], scalar=I4[:, t, k:k + 1], in1=dtm[:, 0, :], op0=ALU.is_equal, op1=ALU.mult),
                         reads=[Brt[t], Bprm3, Bdtm], writes=[Bdtm])
                    S.op("dve", lambda e, t=t, k=k: e.reduce_sum(out=DIf[:, t, k:k + 1], in_=dtm[:, 1, :], axis=AX.X), reads=[Bdtm], writes=[BDI[t]])
                S.op("dve", lambda e, t=t: e.tensor_copy(out=DI[:, t * 4:t * 4 + 4], in_=DIf[:, t, :]), reads=[BDI[t]], writes=[BDI[t]])
                S.dma("sp", lambda e, t=t: e.dma_start(out=h2b[:], in_=h2s_d[t * 128:(t + 1) * 128, :]), reads=[Bh2s], writes=[Bh2b])
                for k in range(4):
                    S.dma("pool", lambda e, t=t, k=k: e.indirect_dma_start(out=xs_d[:, :], out_offset=bass.IndirectOffsetOnAxis(ap=DI[:, t * 4 + k:t * 4 + k + 1], axis=0), in_=h2b[:, :], in_offset=None), reads=[Bh2b, BDI[t]], writes=[Bxs])
                S.mark(f"m_d{t}")
            S.barrier()
            S.flush(); st.close()
            st = ExitStack()
            wu = [sb(f"wu{i}", [128, 8, 2048], BF16) for i in range(2)]; Bwu = [S.buf(f"wu{i}") for i in range(2)]
            wd = [sb(f"wd{i}", [128, 8, 1024], BF16) for i in range(2)]; Bwd = [S.buf(f"wd{i}") for i in range(2)]
            bup = [sb(f"bup{i}", [128, 16]) for i in range(2)]; Bbup = [S.buf(f"bup{i}") for i in range(2)]
            xblk = [sb(f"xblk{i}", [128, 1024], BF16) for i in range(2)]; Bxblk = [S.buf(f"xblk{i}") for i in range(2)]
            xT = [sb(f"xT{i}", [128, 8, 128], BF16) for i in range(2)]; BxT = [S.buf(f"xT{i}") for i in range(2)]
            gq = sb("gq", [128, 1024]); lq = sb("lq", [128, 1024]); sgm = sb("sgm", [128, 1024]); Bgq = S.buf("gq"); Blq = S.buf("lq"); Bsgm = S.buf("sgm")
            actT = [sb(f"actT{i}", [128, 8, 128], BF16) for i in range(2)]; BactT = [S.buf(f"actT{i}") for i in range(2)]
            yblk = [sb(f"yblk{i}", [128, 1024]) for i in range(2)]; Byblk = [S.buf(f"yblk{i}") for i in range(2)]

            regs = {}

            def bnd(e):
                if "bnd" not in regs:
                    regs["bnd"] = e.alloc_register("bnd4095")
                    e.reg_mov(regs["bnd"], 4095)
                return regs["bnd"]

            def load_wu(b):
                i = b % 2
                S.dma("pool", lambda e, b=b, i=i: e.indirect_dma_start(out=wu[i][:, :, :].rearrange("p k n -> p (k n)"), out_offset=None, in_=wub_d[:, :],
                                                                    in_offset=bass.IndirectOffsetOnAxis(ap=idxW[:, 96 + b:96 + b + 1], axis=0), bounds_check=bnd(e), oob_is_err=False),
                      reads=[Beb, Bwcv], writes=[Bwu[i]])
                S.dma("pool", lambda e, b=b, i=i: e.indirect_dma_start(out=bup[i][:, :], out_offset=None, in_=bupg_d[:, :],
                                                                    in_offset=bass.IndirectOffsetOnAxis(ap=idxW[:, b:b + 1], axis=0)),
                      reads=[Beb], writes=[Bbup[i]])

            def load_wd(b):
                i = b % 2
                S.dma("pool", lambda e, b=b, i=i: e.indirect_dma_start(out=wd[i][:, :, :].rearrange("p k n -> p (k n)"), out_offset=None, in_=wdb_d[:, :],
                                                                    in_offset=bass.IndirectOffsetOnAxis(ap=idxW[:, 96 + b:96 + b + 1], axis=0), bounds_check=bnd(e), oob_is_err=False),
                      reads=[Beb, Bwcv], writes=[Bwd[i]])

            def up_blk(b):
                i = b % 2
                S.dma("sp", lambda e, b=b, i=i: e.dma_start(out=xblk[i][:], in_=xs_d[b * 128:(b + 1) * 128, :]), reads=[Bxs], writes=[Bxblk[i]])
                for k in range(8):
                    S.op("pe", lambda e, i=i, k=k: e.transpose(PT[:, k * 128:(k + 1) * 128], xblk[i][:, k * 128:(k + 1) * 128], ident_b), reads=[Bxblk[i], Bcstb], writes=[BPTb])
                S.op("act", lambda e, i=i: e.activation(out=xT[i][:, :, :], in_=PT[:, :].rearrange("p (a b) -> p a b", a=8), func=AF.Identity), reads=[BPTb], writes=[BxT[i]])
                for j in range(16):
                    pb = PB[j // 4]; Bpb = BPB[j // 4]
                    for k in range(8):
                        S.op("pe", lambda e, pb=pb, i=i, j=j, k=k: e.matmul(pb[:, (j % 4) * 128:(j % 4 + 1) * 128], lhsT=wu[i][:, k, j * 128:(j + 1) * 128], rhs=xT[i][:, k, :],
                                                                           start=(k == 0), stop=(k == 7)), reads=[Bwu[i], BxT[i]], writes=[Bpb])
                for j in range(16):
                    pb = PB[j // 4]; Bpb = BPB[j // 4]
                    dst = gq if j < 8 else lq
                    Bdst = Bgq if j < 8 else Blq
                    jj = j % 8
                    S.op("dve", lambda e, pb=pb, i=i, j=j, jj=jj, dst=dst: e.tensor_scalar(out=dst[:, jj * 128:(jj + 1) * 128], in0=pb[:, (j % 4) * 128:(j % 4 + 1) * 128], scalar1=bup[i][:, j:j + 1],
                                                                                       scalar2=7.0, op0=ALU.add, op1=ALU.min), reads=[Bpb, Bbup[i]], writes=[Bdst])
                S.op("act", lambda e: e.activation(out=sgm[:], in_=gq[:], func=AF.Sigmoid, scale=1.702), reads=[Bgq], writes=[Bsgm])
                S.op("dve", lambda e: e.tensor_scalar(out=lq[:], in0=lq[:], scalar1=-7.0, scalar2=1.0, op0=ALU.max, op1=ALU.add), reads=[Blq], writes=[Blq])
                S.op("dve", lambda e: e.tensor_tensor(out=gq[:], in0=gq[:], in1=sgm[:], op=ALU.mult), reads=[Bgq, Bsgm], writes=[Bgq])
                S.op("dve", lambda e, i=i: e.tensor_tensor(out=actT[i][:, :, :].rearrange("p a b -> p (a b)"), in0=gq[:], in1=lq[:], op=ALU.mult), reads=[Bgq, Blq], writes=[BactT[i]])

            def down_blk(b):
                i = b % 2
                for hh in range(2):
                    pb = PB[4 + hh]; Bpb = BPB[4 + hh]
                    for jd in range(8):
                        S.op("pe", lambda e, pb=pb, i=i, jd=jd, hh=hh: e.matmul(pb[:, :], lhsT=actT[i][:, jd, :], rhs=wd[i][:, jd, hh * 512:(hh + 1) * 512], start=(jd == 0), stop=(jd == 7)),
                             reads=[BactT[i], Bwd[i]], writes=[Bpb])
                    S.op("act", lambda e, pb=pb, i=i, hh=hh: e.activation(out=yblk[i][:, hh * 512:(hh + 1) * 512], in_=pb[:, :], func=AF.Identity), reads=[Bpb], writes=[Byblk[i]])
                S.dma("sp", lambda e, b=b, i=i: e.dma_start(out=ys_d[b * 128:(b + 1) * 128, :], in_=yblk[i][:]), reads=[Byblk[i]], writes=[Bys])

            load_wu(0); load_wd(0); load_wu(1); load_wd(1)
            S.mark("m_ld")
            up_blk(0)
            S.mark("m_b0")
            for b in range(1, NBLK):
                up_blk(b)
                if b + 1 < NBLK:
                    load_wu(b + 1)
                down_blk(b - 1)
                if b + 1 < NBLK:
                    load_wd(b + 1)
                S.mark(f"m_b{b}")
            down_blk(NBLK - 1)
            S.mark("m_blk")
            S.barrier()
            S.flush(); st.close()
            st = ExitStack()
            Yg = sb("Yg", [128, 4, 1024]); BYg = S.buf("Yg")
            xb = sb("xb2", [128, 1024]); Bxb = S.buf("xb2")
            acc = sb("acc", [128, 1024]); Bacc = S.buf("acc")
            for t in range(16):
                for k in range(4):
                    S.dma("pool", lambda e, t=t, k=k: e.indirect_dma_start(out=Yg[:, k, :], out_offset=None, in_=ys_d[:, :],
                                                                       in_offset=bass.IndirectOffsetOnAxis(ap=DI[:, t * 4 + k:t * 4 + k + 1], axis=0)),
                          reads=[BDI[t], Bys], writes=[BYg])
                S.dma("sp", lambda e, t=t: e.dma_start(out=xb[:], in_=out_d[t * 128:(t + 1) * 128, :]), reads=[Bout], writes=[Bxb])
                S.op("pe", lambda e, t=t: e.transpose(PB[6][0:32, 128:256], Gt[:, t, :], cs(C_ID)), reads=[Brt[t], Bcst], writes=[BPB[6]])
                S.op("act", lambda e: e.activation(out=GT[:, :], in_=PB[6][0:32, 128:256], func=AF.Identity), reads=[BPB[6]], writes=[BGT])
                S.op("dve", lambda e, t=t: e.tensor_scalar(out=acc[:], in0=Yg[:, 0, :], scalar1=GK[:, t, 0:1], scalar2=None, op0=ALU.mult), reads=[BYg, Brt[t]], writes=[Bacc])
                for k in range(1, 4):
                    S.op("dve", lambda e, t=t, k=k: e.scalar_tensor_tensor(out=acc[:], in0=Yg[:, k, :], scalar=GK[:, t, k:k + 1], in1=acc[:], op0=ALU.mult, op1=ALU.add),
                         reads=[BYg, Brt[t], Bacc], writes=[Bacc])
                for hh in range(2):
                    pb = PB[4 + hh]; Bpb = BPB[4 + hh]
                    S.op("pe", lambda e, pb=pb, hh=hh: e.matmul(pb[:, :], lhsT=GT[0:32, :], rhs=bdn[0:32, hh * 512:(hh + 1) * 512], start=True, stop=True), reads=[BGT, Bbdn], writes=[Bpb])
                    S.op("dve", lambda e, pb=pb, hh=hh: e.tensor_tensor(out=acc[:, hh * 512:(hh + 1) * 512], in0=pb[:, :], in1=acc[:, hh * 512:(hh + 1) * 512], op=ALU.add),
                         reads=[Bpb, Bacc], writes=[Bacc])
                S.op("dve", lambda e: e.tensor_tensor(out=acc[:], in0=acc[:], in1=g2[:], op=ALU.mult), reads=[Bacc, Bmb], writes=[Bacc])
                S.op("dve", lambda e: e.tensor_tensor(out=acc[:], in0=acc[:], in1=xb[:], op=ALU.add), reads=[Bacc, Bxb], writes=[Bacc])
                S.dma("sp", lambda e, t=t: e.dma_start(out=out_d[t * 128:(t + 1) * 128, :], in_=acc[:]), reads=[Bacc], writes=[Bout])
            S.flush(); st.close()
            st = st_moe
        if stage >= 2 and not SPARSE and not S.stopped:
            sh2 = sb("sh2", [128, 1024]); w2s = sb("w2s", [128, 1024]); g2 = sb("g2", [128, 1024]); Bmb = S.buf("mb")
            for i_, dst in enumerate((sh2, w2s, g2)):
                S.dma("sp", lambda e, i_=i_, dst=dst: e.dma_start(out=dst[:], in_=scr_d[i_:i_ + 1, :].to_broadcast([128, 1024])), reads=[Bscr], writes=[Bmb])
            wr = sb("wr", [128, 8, 32]); Bwr = S.buf("wr")
            S.dma("sp", lambda e: e.dma_start(out=wr[:], in_=wr_d.rearrange("(k p) n -> p k n", p=128)), writes=[Bwr])
            prm2 = sb("prm2", [128, 544]); Bprm2 = S.buf("prm2")
            S.dma("sp", lambda e: e.dma_start(out=prm2[:], in_=prm2_d), writes=[Bprm2])
            bdn = sb("bdn", [32, 1024]); Bbdn = S.buf("bdn")
            S.dma("sp", lambda e: e.dma_start(out=bdn[:], in_=bdn_d), writes=[Bbdn])
            h2T = sb("h2T", [128, 8, 1024], BF16); Bh2T = [S.buf(f"h2T{t}") for t in range(8)]
            xb = sb("xb", [128, 1024]); Bxb = S.buf("xb")
            h2f = sb("h2f", [128, 1024]); Bh2f = S.buf("h2f")
            h2Tf = sb("h2Tf", [128, 8, 128]); Bh2Tf = S.buf("h2Tf")
            Gt = sb("Gt", [128, 8, 32]); BG = [S.buf(f"G{t}") for t in range(8)]
            GT = sb("GT", [32, 128]); BGT = S.buf("GT")
            acc = sb("acc", [128, 8, 1024]); Bacc = [S.buf(f"acc{t}") for t in range(8)]
            wu = [sb(f"wu{i}", [128, 8, 2048], BF16) for i in range(2)]; Bwu = [S.buf(f"wu{i}") for i in range(2)]
            wd = sb("wd", [128, 8, 1024], BF16); Bwd = S.buf("wd")
            actT = [sb(f"actT{i}", [128, 8, 512], BF16) for i in range(2)]; BactT = [S.buf(f"actT{i}") for i in range(2)]
            gq = [sb(f"gq{i}", [128, 512]) for i in range(2)]; sg = [sb(f"sg{i}", [128, 512]) for i in range(1)] * 2; lq = [sb(f"lq{i}", [128, 512]) for i in range(1)] * 2
            Bgq = [S.buf(f"gq{i}") for i in range(2)]; Bsg = [S.buf(f"sg{i}") for i in range(1)] * 2; Blq = [S.buf(f"lq{i}") for i in range(1)] * 2
            sm = sb("sm", [128, 64]); Bsm = S.buf("sm")
            ex = sb("ex", [128, 32]); Bex = S.buf("ex")
            wupv = wup_d.rearrange("(e k p) n -> e p k n", e=32, p=128)
            wdnv = wdn_d.rearrange("(e k p) n -> e p k n", e=32, p=128)

            def load_wu(e_):
                for hh in range(2):
                    S.dma("pool", lambda e, e_=e_, hh=hh: e.dma_start(out=wu[e_ % 2][:, :, hh * 1024:(hh + 1) * 1024], in_=wupv[e_, :, :, hh * 1024:(hh + 1) * 1024]), writes=[Bwu[e_ % 2]])

            def load_wd(e_):
                S.dma("pool", lambda e, e_=e_: e.dma_start(out=wd[:], in_=wdnv[e_]), writes=[Bwd])

            rot = {"up": 0, "dn": 0, "q": 0}

            def up_item(e_, tg, ai):
                for jj in range(8):
                    banks = []
                    for j in (jj, jj + 8):
                        bi = rot["up"] % 4
                        rot["up"] += 1
                        pb = PB[bi]; Bpb = BPB[bi]
                        for k in range(8):
                            S.op("pe", lambda e, pb=pb, k=k, j=j, e_=e_, tg=tg: e.matmul(pb[:, :], lhsT=wu[e_ % 2][:, k, j * 128:(j + 1) * 128], rhs=h2T[:, k, tg * 512:(tg + 1) * 512],
                                                                                       start=(k == 0), stop=(k == 7)), reads=[Bwu[e_ % 2]] + Bh2T[tg * 4:(tg + 1) * 4], writes=[Bpb])
                        banks.append((pb, Bpb))
                    qi = rot["q"] % 2
                    rot["q"] += 1
                    (pa, Bpa), (pbb, Bpbb) = banks
                    bg = 32 + e_ * 16 + jj
                    bl = 32 + e_ * 16 + jj + 8
                    S.op("dve", lambda e, pa=pa, qi=qi, bg=bg: e.tensor_scalar(out=gq[qi][:], in0=pa[:, :], scalar1=prm2[:, bg:bg + 1], scalar2=7.0, op0=ALU.add, op1=ALU.min),
                         reads=[Bpa, Bprm2], writes=[Bgq[qi]])
                    S.op("act", lambda e, qi=qi: e.activation(out=sg[qi][:], in_=gq[qi][:], func=AF.Sigmoid, scale=1.702), reads=[Bgq[qi]], writes=[Bsg[qi]])
                    S.op("dve", lambda e, pbb=pbb, qi=qi, bl=bl: e.tensor_scalar(out=lq[qi][:], in0=pbb[:, :], scalar1=prm2[:, bl:bl + 1], scalar2=7.0, op0=ALU.add, op1=ALU.min),
                         reads=[Bpbb, Bprm2], writes=[Blq[qi]])
                    S.op("dve", lambda e, qi=qi: e.tensor_scalar(out=lq[qi][:], in0=lq[qi][:], scalar1=-7.0, scalar2=1.0, op0=ALU.max, op1=ALU.add), reads=[Blq[qi]], writes=[Blq[qi]])
                    S.op("dve", lambda e, qi=qi: e.tensor_tensor(out=gq[qi][:], in0=gq[qi][:], in1=sg[qi][:], op=ALU.mult), reads=[Bgq[qi], Bsg[qi]], writes=[Bgq[qi]])
                    S.op("dve", lambda e, qi=qi, ai=ai, jj=jj: e.tensor_tensor(out=actT[ai][:, jj, :], in0=gq[qi][:], in1=lq[qi][:], op=ALU.mult), reads=[Bgq[qi], Blq[qi]], writes=[BactT[ai]])

            def down_item(e_, tg, ai):
                for tt in range(4):
                    t = tg * 4 + tt
                    for hh in range(2):
                        bi = 4 + rot["dn"] % 2
                        rot["dn"] += 1
                        pb = PB[bi]; Bpb = BPB[bi]
                        for jd in range(8):
                            S.op("pe", lambda e, pb=pb, jd=jd, tt=tt, hh=hh, ai=ai: e.matmul(pb[:, :], lhsT=actT[ai][:, jd, tt * 128:(tt + 1) * 128], rhs=wd[:, jd, hh * 512:(hh + 1) * 512],
                                                                                         start=(jd == 0), stop=(jd == 7)), reads=[BactT[ai], Bwd], writes=[Bpb])
                        if e_ == 0:
                            S.op("dve", lambda e, pb=pb, t=t, hh=hh, e_=e_: e.tensor_scalar(out=acc[:, t, hh * 512:(hh + 1) * 512], in0=pb[:, :], scalar1=Gt[:, t, e_:e_ + 1], scalar2=None, op0=ALU.mult),
                                 reads=[Bpb, BG[t]], writes=[Bacc[t]])
                        else:
                            S.op("dve", lambda e, pb=pb, t=t, hh=hh, e_=e_: e.scalar_tensor_tensor(out=acc[:, t, hh * 512:(hh + 1) * 512], in0=pb[:, :], scalar=Gt[:, t, e_:e_ + 1],
                                                                                                in1=acc[:, t, hh * 512:(hh + 1) * 512], op0=ALU.mult, op1=ALU.add),
                                 reads=[Bpb, BG[t], Bacc[t]], writes=[Bacc[t]])

            for pss in range(2):
                for t in range(8):
                    gt = pss * 8 + t
                    S.dma("sp", lambda e, gt=gt: e.dma_start(out=xb[:], in_=out_d[gt * 128:(gt + 1) * 128, :]), reads=[Bout], writes=[Bxb])
                    S.op("act", lambda e: e.activation(out=h2f[:], in_=xb[:], func=AF.Square, accum_out=sm[:, 11:12]), reads=[Bxb], writes=[Bh2f, Bsm])
                    S.op("act", lambda e: e.activation(out=sm[:, 12:13], in_=sm[:, 11:12], func=AF.Sqrt, scale=1.0 / 1024, bias=EPS), reads=[Bsm], writes=[Bsm])
                    S.op("dve", lambda e: e.reciprocal(out=sm[:, 12:13], in_=sm[:, 12:13]), reads=[Bsm], writes=[Bsm])
                    S.op("dve", lambda e: e.scalar_tensor_tensor(out=h2f[:], in0=xb[:], scalar=sm[:, 12:13], in1=w2s[:], op0=ALU.mult, op1=ALU.mult), reads=[Bxb, Bsm, Bmb, Bh2f], writes=[Bh2f])
                    S.op("dve", lambda e: e.tensor_tensor(out=h2f[:], in0=h2f[:], in1=sh2[:], op=ALU.add), reads=[Bh2f, Bmb], writes=[Bh2f])
                    for b2 in range(2):
                        for kk in range(4):
                            k = b2 * 4 + kk
                            S.op("pe", lambda e, b2=b2, kk=kk, k=k: e.transpose(PB[b2][:, kk * 128:(kk + 1) * 128], h2f[:, k * 128:(k + 1) * 128], cs(C_ID)), reads=[Bh2f, Bcst], writes=[BPB[b2]])
                        S.op("act", lambda e, b2=b2, t=t: e.activation(out=h2T[:, b2 * 4:(b2 + 1) * 4, t * 128:(t + 1) * 128], in_=PB[b2][:, :].rearrange("p (a b) -> p a b", a=4), func=AF.Identity),
                             reads=[BPB[b2]], writes=[Bh2T[t]])
                        S.op("dve", lambda e, b2=b2: e.tensor_copy(out=h2Tf[:, b2 * 4:(b2 + 1) * 4, :], in_=PB[b2][:, :].rearrange("p (a b) -> p a b", a=4)), reads=[BPB[b2]], writes=[Bh2Tf])
                    for k in range(8):
                        S.op("pe", lambda e, k=k: e.matmul(PB[6][:, 0:32], lhsT=h2Tf[:, k, :], rhs=wr[:, k, :], start=(k == 0), stop=(k == 7)), reads=[Bh2Tf, Bwr], writes=[BPB[6]])
                    S.op("dve", lambda e: e.tensor_tensor(out=sm[:, 16:48], in0=PB[6][:, 0:32], in1=prm2[:, 0:32], op=ALU.add), reads=[BPB[6], Bprm2, Bsm], writes=[Bsm])
                    S.op("dve", lambda e: e.max(out=sm[:, 0:8], in_=sm[:, 16:48]), reads=[Bsm], writes=[Bsm])
                    S.op("dve", lambda e: e.tensor_scalar(out=sm[:, 8:9], in0=sm[:, 0:1], scalar1=-1.0, scalar2=None, op0=ALU.mult), reads=[Bsm], writes=[Bsm])
                    S.op("act", lambda e: e.activation(out=ex[:], in_=sm[:, 16:48], func=AF.Exp, bias=sm[:, 8:9], scale=1.0), reads=[Bsm], writes=[Bex])
                    S.op("dve", lambda e: e.scalar_tensor_tensor(out=ex[:], in0=sm[:, 16:48], scalar=sm[:, 3:4], in1=ex[:], op0=ALU.is_ge, op1=ALU.mult), reads=[Bsm, Bex], writes=[Bex])
                    S.op("dve", lambda e: e.reduce_sum(out=sm[:, 9:10], in_=ex[:], axis=AX.X), reads=[Bex, Bsm], writes=[Bsm])
                    S.op("dve", lambda e: e.reciprocal(out=sm[:, 10:11], in_=sm[:, 9:10]), reads=[Bsm], writes=[Bsm])
                    S.op("dve", lambda e, t=t: e.tensor_scalar(out=Gt[:, t, :], in0=ex[:], scalar1=sm[:, 10:11], scalar2=None, op0=ALU.mult), reads=[Bex, Bsm], writes=[BG[t]])
                items = [(e_, tg) for e_ in range(32) for tg in range(2)]
                load_wu(0)
                load_wd(0)
                load_wu(1)
                up_item(0, 0, 0)
                for i in range(1, len(items)):
                    e_, tg = items[i]
                    pe_, ptg = items[i - 1]
                    if tg == 0 and e_ + 1 < 32:
                        load_wu(e_ + 1)
                    up_item(e_, tg, i % 2)
                    down_item(pe_, ptg, (i - 1) % 2)
                    if ptg == 1 and pe_ + 1 < 32:
                        load_wd(pe_ + 1)
                down_item(31, 1, (len(items) - 1) % 2)
                for t in range(8):
                    gt = pss * 8 + t
                    S.op("pe", lambda e, t=t: e.transpose(PB[6][0:32, 128:256], Gt[:, t, :], cs(C_ID)), reads=[BG[t], Bcst], writes=[BPB[6]])
                    S.op("act", lambda e: e.activation(out=GT[:, :], in_=PB[6][0:32, 128:256], func=AF.Identity), reads=[BPB[6]], writes=[BGT])
                    S.dma("sp", lambda e, gt=gt: e.dma_start(out=xb[:], in_=out_d[gt * 128:(gt + 1) * 128, :]), reads=[Bout], writes=[Bxb])
                    for hh in range(2):
                        pb = PB[4 + hh]; Bpb = BPB[4 + hh]
                        S.op("pe", lambda e, pb=pb, hh=hh: e.matmul(pb[:, :], lhsT=GT[0:32, :], rhs=bdn[0:32, hh * 512:(hh + 1) * 512], start=True, stop=True), reads=[BGT, Bbdn], writes=[Bpb])
                        S.op("dve", lambda e, pb=pb, t=t, hh=hh: e.tensor_tensor(out=acc[:, t, hh * 512:(hh + 1) * 512], in0=pb[:, :], in1=acc[:, t, hh * 512:(hh + 1) * 512], op=ALU.add),
                             reads=[Bpb, Bacc[t]], writes=[Bacc[t]])
                    S.op("dve", lambda e, t=t: e.tensor_tensor(out=acc[:, t, :], in0=acc[:, t, :], in1=g2[:], op=ALU.mult), reads=[Bacc[t], Bmb], writes=[Bacc[t]])
                    S.op("dve", lambda e, t=t: e.tensor_tensor(out=acc[:, t, :], in0=acc[:, t, :], in1=xb[:], op=ALU.add), reads=[Bacc[t], Bxb], writes=[Bacc[t]])
                    S.dma("sp", lambda e, gt=gt, t=t: e.dma_start(out=out_d[gt * 128:(gt + 1) * 128, :], in_=acc[:, t, :]), reads=[Bacc[t]], writes=[Bout])

        S.flush(); st.close()
        st = st_root
        if S.stopped:
            S.dma("sp", lambda e: e.dma_start(out=out_d[0:128, 0:NPRM], in_=prm[:, :]), reads=[Bprm], writes=[Bout], force=True)
        S.final_wait("sp", [Bout])
        S.flush()
    return nc


def prep_inputs(inp):
    f = lambda a: np.ascontiguousarray(np.asarray(a, dtype=np.float32))
    x = f(inp["x"]); c = f(inp["c"])
    w_in = f(inp["w_in"][0])
    q_, k_, v_ = w_in[:, 0:512], w_in[:, 512:1024], w_in[:, 1024:1536]
    z_, a_, b_ = w_in[:, 1536:2048], w_in[:, 2048:2052], w_in[:, 2052:2056]
    sq_, sk_, sv_ = w_in[:, 2056:2568], w_in[:, 2568:2696], w_in[:, 2696:2824]
    w_fm = f(np.concatenate([q_, k_, v_, sq_, sk_[:, 0:64], sk_[:, 0:64], sk_[:, 64:128], sk_[:, 64:128]], axis=1))
    w_tm = f(np.concatenate([z_, a_, b_, sv_], axis=1))
    cst = make_consts()
    ii = np.arange(128)
    bd32 = (ii[:, None] // 32 == ii[None, :] // 32)
    m1 = ((ii[:, None] // 32) % 2 == 0) & (ii[None, :] // 32 == ii[:, None] // 32 + 1)
    m2 = (ii[:, None] < 64) & (ii[None, :] >= 64)
    cstm = np.concatenate([bd32, m1.T, m2.T], axis=1).astype(np.float32).astype(ml_dtypes.bfloat16)
    b_ada = f(inp["b_ada"][0])
    n2bc = f(np.broadcast_to(f(inp["norm2_w"][0])[None, :], (128, 1024)))
    shared = {"cst": cst, "w_ada": f(inp["w_ada"][0]), "b_ada": b_ada[None, :], "w_fm": w_fm, "w_tm": w_tm,
              "w_out": f(inp["w_out"][0]), "n2bc": n2bc, "cstm": cstm,
              "w_router": f(inp["w_router"][0]), "b_down": f(inp["b_down"][0]),
              "w_up": f(inp["w_up"][0]).reshape(32 * 1024, 2048), "w_down": f(inp["w_down"][0]).reshape(32 * 1024, 1024)}
    prm2 = np.zeros((128, 544), np.float32)
    prm2[:, 0:32] = f(inp["b_router"][0])[None, :]
    prm2[:, 32:544] = f(inp["b_up"][0]).reshape(32, 16, 128).transpose(2, 0, 1).reshape(128, 512)
    shared["prm2"] = prm2
    if SPARSE:
        prm3 = np.zeros((128, 160), np.float32)
        prm3[:, 0:32] = np.arange(32, dtype=np.float32)[None, :]
        prm3[:, 32:128] = np.arange(96, dtype=np.float32)[None, :]
        prm3[:, 128] = np.arange(128, dtype=np.float32)
        shared["prm3"] = prm3
        shared["b_upg"] = f(f(inp["b_up"][0]).reshape(32, 16, 128).transpose(0, 2, 1).reshape(4096, 16))
        shared["w_upg"] = f(f(inp["w_up"][0]).reshape(32, 8, 128, 2048).transpose(0, 2, 1, 3).reshape(4096, 8, 2048))
        shared["w_dng"] = f(f(inp["w_down"][0]).reshape(32, 8, 128, 1024).transpose(0, 2, 1, 3).reshape(4096, 8, 1024))
        del shared["w_up"], shared["w_down"]
    maps = []
    for core in range(8):
        b, hf = core // 2, core % 2
        prm = np.zeros((128, NPRM), np.float32)
        prm[:, P_FLAG] = float(hf)
        prm[:, P_HALO] = (float(hf) - 1.0) * 30000.0
        prm[:, P_C:P_C + 8] = c[b].reshape(8, 128).T
        prm[:, P_ALOG:P_ALOG + 4] = f(inp["a_log"][0])[None, :]
        prm[:, P_DTB:P_DTB + 4] = f(inp["dt_bias"][0])[None, :]
        prm[:, P_SINK:P_SINK + 8] = f(inp["sinks"][0])[None, :]
        prm[:, P_DNW:P_DNW + 128] = f(inp["dn_norm_w"][0])[None, :]
        prm[0:64, P_QNW] = f(inp["q_norm_w"][0])
        prm[64:128, P_QNW_HI] = f(inp["q_norm_w"][0])
        prm[:, P_KNW] = np.tile(f(inp["k_norm_w"][0]), 2)
        prm[:, P_N1:P_N1 + 8] = f(inp["norm1_w"][0]).reshape(8, 128).T
        prm[:, P_BADA:P_BADA + 48] = b_ada.reshape(48, 128).T
        prm[:, P_CONV:P_CONV + 48] = f(inp["conv_w"][0]).T.reshape(12, 128, 4).transpose(1, 0, 2).reshape(128, 48)
        m = dict(shared)
        m["prm"] = prm
        m["xp"] = f(x[b, 0:2048])
        m["xo"] = f(x[b, hf * 2048:(hf + 1) * 2048])
        maps.append(m)
    return maps


_NC_CACHE = {}


def kernel(**inputs):
    maps = prep_inputs(inputs)
    if "nc" not in _NC_CACHE:
        _NC_CACHE["nc"] = build()
    res = run_bass_kernel_spmd(_NC_CACHE["nc"], maps, core_ids=list(range(8)))
    out = np.zeros((4, 4096, 1024), np.float32)
    for core in range(8):
        b, hf = core // 2, core % 2
        out[b, hf * 2048:(hf + 1) * 2048] = res.results[core]["out"]
    return out
```

```python
import numpy as np
import ml_dtypes
import concourse.bass as bass
import concourse.mybir as mybir
from concourse.bass_utils import run_bass_kernel_spmd
from contextlib import ExitStack

F32 = mybir.dt.float32
BF16 = mybir.dt.bfloat16
I32 = mybir.dt.int32
U32 = mybir.dt.uint32
AF = mybir.ActivationFunctionType
ALU = mybir.AluOpType
AX = mybir.AxisListType

EPS = 1e-6
NEGBIG = -30000.0


class Buf:
    __slots__ = ("name", "w", "rs", "excl")

    def __init__(self, name, excl=False):
        self.name = name
        self.w = None
        self.rs = []
        self.excl = excl


class Stager:
    def __init__(self, S):
        self.S = S
        self.st = {}

    def op(self, k, eng, fn, reads=(), writes=()):
        self.st.setdefault(k, []).append((eng, fn, reads, writes, False))

    def dma(self, k, eng, fn, reads=(), writes=()):
        self.st.setdefault(k, []).append((eng, fn, reads, writes, True))

    def run(self):
        for k in sorted(self.st):
            for eng, fn, r, w, isdma in self.st[k]:
                if isdma:
                    self.S.dma(eng, fn, reads=r, writes=w)
                else:
                    self.S.op(eng, fn, reads=r, writes=w)
        self.st = {}


class Sched:
    ENG = ("pe", "act", "dve", "pool", "sp")

    def __init__(self, nc, stack):
        self.nc = nc
        self.stack = stack
        self.prog = {e: [] for e in self.ENG}
        self.esem = {e: stack.enter_context(nc.semaphore("es_" + e)) for e in self.ENG}
        self.tick = {e: 0 for e in self.ENG}
        self.waited = {e: {} for e in self.ENG}
        self.dsems = {}
        self.nbuf = 0

    def buf(self, name=None, excl=False):
        self.nbuf += 1
        return Buf((name or "b") + str(self.nbuf), excl)

    def _collect(self, eng, reads, writes):
        deps = {}

        def add(d):
            if d is None:
                return
            s, v = d
            if eng == "pe" and s is self.esem["pe"]:
                return
            k = id(s)
            if k not in deps or deps[k][1] < v:
                deps[k] = (s, v)
        for b in reads:
            add(b.w)
            if b.excl:
                for r in b.rs:
                    add(r)
        for b in writes:
            add(b.w)
            for r in b.rs:
                add(r)
        out = []
        wd = self.waited[eng]
        for k, (s, v) in deps.items():
            if wd.get(k, 0) >= v:
                continue
            wd[k] = v
            out.append((s, v))
        return out

    def _commit(self, reads, writes, done):
        for b in writes:
            b.w = done
            b.rs = []
        for b in reads:
            if b.excl:
                b.w = done
                b.rs = []
            elif b not in writes:
                b.rs.append(done)
                if len(b.rs) > 24:
                    m = {}
                    for s, v in b.rs:
                        if id(s) not in m or m[id(s)][1] < v:
                            m[id(s)] = (s, v)
                    b.rs = list(m.values())

    stopped = False
    stop_at = None

    def mark(self, label):
        if self.stop_at is not None and label == self.stop_at:
            self.stopped = True

    def op(self, eng, fn, reads=(), writes=()):
        if self.stopped:
            return
        waits = self._collect(eng, reads, writes)
        self.tick[eng] += 1
        done = (self.esem[eng], self.tick[eng])
        self.prog[eng].append((waits, fn, self.esem[eng], 1))
        self._commit(reads, writes, done)

    def dma(self, eng, fn, reads=(), writes=(), key=None, force=False):
        if self.stopped and not force:
            return
        waits = self._collect(eng, reads, writes)
        kb = key if key is not None else (writes[0] if writes else reads[0])
        if kb not in self.dsems:
            self.dsems[kb] = [self.stack.enter_context(self.nc.semaphore("ds_" + kb.name)), 0]
        d = self.dsems[kb]
        d[1] += 16
        done = (d[0], d[1])
        self.prog[eng].append((waits, fn, d[0], 16))
        self._commit(reads, writes, done)

    def barrier(self):
        allw = [(self.esem[e], self.tick[e]) for e in self.ENG if self.tick[e] > 0]
        allw += [(d[0], d[1]) for d in self.dsems.values()]
        for e in self.ENG:
            wd = self.waited[e]
            waits = []
            for s_, v in allw:
                if s_ is self.esem[e]:
                    continue
                if wd.get(id(s_), 0) >= v:
                    continue
                wd[id(s_)] = v
                waits.append((s_, v))
            self.prog[e].append((waits, None, None, 0))

    def final_wait(self, eng, bufs):
        waits = self._collect(eng, bufs, bufs)
        self.prog[eng].append((waits, None, None, 0))

    def flush(self):
        with self.nc.Block() as block:
            self.emit(block)
        self.prog = {e: [] for e in self.ENG}

    def emit(self, block):
        m = {"pe": block.tensor, "act": block.scalar, "dve": block.vector,
             "pool": block.gpsimd, "sp": block.sync}
        for e in self.ENG:
            plist = self.prog[e]

            def body(engobj, plist=plist):
                for waits, fn, sem, inc in plist:
                    for (s, v) in waits:
                        engobj.wait_ge(s, v)
                    if fn is not None:
                        fn(engobj).then_inc(sem, inc)
            m[e](body)


C_ID, C_UI, C_SL, C_NEG, C_OFFD, C_ONE, C_BD, C_SWB = 0, 128, 256, 384, 512, 640, 768, 896
NCST = 896 + 2 * 8 * 128


def make_consts():
    c = np.zeros((128, NCST), np.float32)
    i = np.arange(128)
    c[:, C_ID:C_ID + 128] = np.eye(128)
    c[:, C_UI:C_UI + 128] = (i[:, None] <= i[None, :])
    c[:, C_SL:C_SL + 128] = (i[:, None] > i[None, :])
    c[:, C_NEG:C_NEG + 128] = np.where(i[:, None] > i[None, :], NEGBIG, 0.0)
    c[:, C_OFFD:C_OFFD + 128] = 1.0 - np.eye(128)
    c[:, C_ONE:C_ONE + 128] = 1.0
    c[:, C_BD:C_BD + 128] = ((i[:, None] // 64) == (i[None, :] // 64))
    s = i[:, None].astype(np.float32)
    q = i[None, :].astype(np.float32)
    for h in range(8):
        slope = 2.0 ** (-(h + 1))
        prev = np.where(s > q, -slope * (q + 128.0 - s), NEGBIG)
        own = np.where(s <= q, -slope * (q - s), NEGBIG)
        c[:, C_SWB + (0 * 8 + h) * 128: C_SWB + (0 * 8 + h) * 128 + 128] = prev
        c[:, C_SWB + (1 * 8 + h) * 128: C_SWB + (1 * 8 + h) * 128 + 128] = own
    return c


P_FLAG, P_HALO, P_C, P_ALOG, P_DTB, P_SINK, P_DNW, P_QNW, P_KNW, P_N1, P_BADA, P_CONV = \
    0, 1, 2, 10, 14, 18, 26, 154, 155, 156, 164, 212
NPRM = 212 + 48 + 1
P_QNW_HI = 212 + 48

NFM = 18 * 128
NTM = 512 + 8 + 128


SPARSE = True
NBLK = 96


def build(stage=9, stop_at=None, dbg=False):
    nc = bass.Bass("TRN2", target_bir_lowering=False)
    dbg_d = nc.dram_tensor("dbg", [128, 4096], F32, kind="ExternalOutput").ap() if dbg else None

    def din(name, shape, dt=F32):
        return nc.dram_tensor(name, shape, dt, kind="ExternalInput").ap()
    xp_d = din("xp", [2048, 1024])
    xo_d = din("xo", [2048, 1024])
    cst_d = din("cst", [128, NCST])
    prm_d = din("prm", [128, NPRM])
    wada_d = din("w_ada", [1024, 6144])
    bada_d = din("b_ada", [1, 6144])
    wfm_d = din("w_fm", [1024, NFM])
    wtm_d = din("w_tm", [1024, NTM])
    wout_d = din("w_out", [1024, 1024])
    n2_d = din("n2bc", [128, 1024])
    cstm_d = din("cstm", [128, 384], BF16)
    wr_d = din("w_router", [1024, 32])
    prm2_d = din("prm2", [128, 544])
    bdn_d = din("b_down", [32, 1024])
    if not SPARSE:
        wup_d = din("w_up", [32 * 1024, 2048])
        wdn_d = din("w_down", [32 * 1024, 1024])
    if SPARSE:
        prm3_d = din("prm3", [128, 160])
        bupg_d = din("b_upg", [4096, 16])
        wupg_d = nc.dram_tensor("w_upg", [4096, 8, 2048], F32, kind="ExternalInput").ap()
        wdng_d = nc.dram_tensor("w_dng", [4096, 8, 1024], F32, kind="ExternalInput").ap()
        wub_d = nc.dram_tensor("wub", [4096, 16384], BF16, kind="Internal").ap()
        wdb_d = nc.dram_tensor("wdb", [4096, 8192], BF16, kind="Internal").ap()
    out_d = nc.dram_tensor("out", [2048, 1024], F32, kind="ExternalOutput").ap()

    with ExitStack() as st:
        S = Sched(nc, st)
        G = Stager(S)
        S.stop_at = stop_at

        def sb(name, shape, dt=F32):
            return st.enter_context(nc.sbuf_tensor("s_" + name, shape, dt))

        def ps(name, shape, dt=F32):
            return st.enter_context(nc.psum_tensor("p_" + name, shape, dt))

        cst = sb("cst", [128, NCST]); Bcst = S.buf("cst")
        cstb = sb("cstb", [128, 896], BF16); Bcstb = S.buf("cstb")
        prm = sb("prm", [128, NPRM]); Bprm = S.buf("prm")

        cstm = sb("cstm", [128, 384], BF16); Bcstm = S.buf("cstm")
        S.dma("sp", lambda e: e.dma_start(out=cstm[:], in_=cstm_d), writes=[Bcstm])

        def cs(off, n=128):
            return cst[:, off:off + n]

        def csb(off, n=128):
            return cstb[:, off:off + n]

        S.dma("sp", lambda e: e.dma_start(out=cst[:], in_=cst_d), writes=[Bcst])
        S.dma("sp", lambda e: e.dma_start(out=prm[:], in_=prm_d), writes=[Bprm])
        S.op("dve", lambda e: e.tensor_copy(out=cstb[:], in_=cst[:, 0:896]), reads=[Bcst], writes=[Bcstb])

        PB = [ps(f"pb{i}", [128, 512]) for i in range(7)]
        BPB = [S.buf(f"pb{i}", excl=True) for i in range(7)]
        PT = ps("ptr", [128, 1024], BF16)
        BPTb = S.buf("ptr", excl=True)
        BPT = [BPTb for i in range(8)]

        cact = sb("cact", [128, 8]); Bcact = S.buf("cact")
        S.op("act", lambda e: e.activation(out=cact[:], in_=prm[:, P_C:P_C + 8], func=AF.Silu), reads=[Bprm], writes=[Bcact])
        scr_d = nc.dram_tensor("scr", [4, 1024], F32, kind="Internal").ap()
        Bscr = S.buf("scr")
        gate1 = sb("gate1", [128, 1024]); Bmodbc = S.buf("modbc")
        modT = sb("modT", [128, 16]); BmodT = S.buf("modT")
        A1 = sb("A1", [128, 8]); B1 = sb("B1", [128, 8]); BA1 = S.buf("A1")
        st_root = st
        st = ExitStack()
        wfm = sb("wfm", [128, 8, NFM], BF16); Bwfm = S.buf("wfm")
        wtm = sb("wtm", [128, 8, NTM], BF16); Bwtm = S.buf("wtm")
        wout = sb("wout", [128, 8, 1024], BF16); Bwout = S.buf("wout")
        for hh in range(2):
            S.dma("pool", lambda e, hh=hh: e.dma_start(out=wfm[:, :, hh * 1152:(hh + 1) * 1152],
                                                     in_=wfm_d.rearrange("(k p) n -> p k n", p=128)[:, :, hh * 1152:(hh + 1) * 1152]), writes=[Bwfm])
        S.dma("pool", lambda e: e.dma_start(out=wtm[:], in_=wtm_d.rearrange("(k p) n -> p k n", p=128)), writes=[Bwtm])
        S.dma("pool", lambda e: e.dma_start(out=wout[:], in_=wout_d.rearrange("(k p) n -> p k n", p=128)), writes=[Bwout])
        st_outer = st
        st = ExitStack()
        n2bc = sb("n2bc", [128, 1024]); Bn2 = S.buf("n2")
        S.dma("sp", lambda e: e.dma_start(out=n2bc[:], in_=n2_d), writes=[Bn2])
        wa = [sb(f"wa{i}", [128, 8, 512]) for i in range(2)]
        Bwa = [S.buf(f"wa{i}") for i in range(2)]
        wav = wada_d.rearrange("(k p) n -> p k n", p=128)
        modrow = sb("modrow", [1, 4096]); Bmodrow = S.buf("modrow")
        badarow = sb("badarow", [1, 4096]); Bbadarow = S.buf("badarow")
        S.dma("sp", lambda e: e.dma_start(out=badarow[:], in_=bada_d[:, 2048:6144]), writes=[Bbadarow])
        for grp in range(12):
            w_ = wa[grp % 2]; Bw_ = Bwa[grp % 2]
            S.dma("sp", lambda e, w_=w_, grp=grp: e.dma_start(out=w_[:], in_=wav[:, :, grp * 512:(grp + 1) * 512]), writes=[Bw_])
            if grp < 4:
                for j in range(4):
                    col = grp * 4 + j
                    for k in range(8):
                        S.op("pe", lambda e, w_=w_, j=j, k=k, col=col: e.matmul(
                            PB[0][:, col:col + 1], lhsT=w_[:, k, j * 128:(j + 1) * 128], rhs=cact[:, k:k + 1],
                            start=(k == 0), stop=(k == 7)), reads=[Bw_, Bcact], writes=[BPB[0]])
                if grp == 3:
                    S.op("dve", lambda e: e.tensor_tensor(out=modT[:], in0=PB[0][:, 0:16], in1=prm[:, P_BADA:P_BADA + 16], op=ALU.add),
                         reads=[BPB[0], Bprm], writes=[BmodT])
                    S.op("dve", lambda e: e.scalar_tensor_tensor(out=A1[:], in0=modT[:, 8:16], scalar=1.0, in1=prm[:, P_N1:P_N1 + 8],
                                                               op0=ALU.add, op1=ALU.mult), reads=[BmodT, Bprm], writes=[BA1])
                    S.op("dve", lambda e: e.tensor_copy(out=B1[:], in_=modT[:, 0:8]), reads=[BmodT], writes=[BA1])
            else:
                g2 = grp - 4
                pb = PB[1 + (g2 % 2)]; Bpb = BPB[1 + (g2 % 2)]
                for k in range(8):
                    S.op("pe", lambda e, w_=w_, k=k, pb=pb: e.matmul(pb[0:1, :], lhsT=cact[:, k:k + 1], rhs=w_[:, k, :],
                                                                     start=(k == 0), stop=(k == 7)), reads=[Bw_, Bcact], writes=[Bpb])
                S.op("dve", lambda e, pb=pb, g2=g2: e.tensor_tensor(out=modrow[0:1, g2 * 512:(g2 + 1) * 512], in0=pb[0:1, :],
                                                                    in1=badarow[0:1, g2 * 512:(g2 + 1) * 512], op=ALU.add),
                     reads=[Bpb, Bbadarow], writes=[Bmodrow])
        w2row = sb("w2row", [1, 1024]); Bw2row = S.buf("w2row")
        S.op("dve", lambda e: e.scalar_tensor_tensor(out=w2row[0:1, :], in0=modrow[0:1, 2048:3072], scalar=1.0, in1=n2bc[0:1, :], op0=ALU.add, op1=ALU.mult),
             reads=[Bmodrow, Bn2], writes=[Bw2row])
        S.dma("sp", lambda e: e.dma_start(out=scr_d[0:1, :], in_=modrow[0:1, 1024:2048]), reads=[Bmodrow], writes=[Bscr])
        S.dma("sp", lambda e: e.dma_start(out=scr_d[1:2, :], in_=w2row[0:1, :]), reads=[Bw2row], writes=[Bscr])
        S.dma("sp", lambda e: e.dma_start(out=scr_d[2:3, :], in_=modrow[0:1, 3072:4096]), reads=[Bmodrow], writes=[Bscr])
        for v, dst in enumerate((gate1,)):
            for hh in range(2):
                pb = PB[1 + hh]; Bpb = BPB[1 + hh]
                S.op("pe", lambda e, pb=pb, v=v, hh=hh: e.matmul(pb[:, :], lhsT=cst[0:1, C_ONE:C_ONE + 128],
                                                               rhs=modrow[0:1, v * 1024 + hh * 512: v * 1024 + (hh + 1) * 512],
                                                               start=True, stop=True), reads=[Bmodrow, Bcst], writes=[Bpb])
                if True:
                    S.op("act", lambda e, pb=pb, hh=hh, dst=dst: e.activation(out=dst[:, hh * 512:(hh + 1) * 512], in_=pb[:, :], func=AF.Identity),
                         reads=[Bpb], writes=[Bmodbc])

        S.mark("adaln")
        S.barrier()
        S.flush(); st.close()
        st = st_outer
        nA = sb("nA", [128, 4]); esink = sb("esink", [128, 8]); Bder = S.buf("der")
        S.op("act", lambda e: e.activation(out=nA[:], in_=prm[:, P_ALOG:P_ALOG + 4], func=AF.Exp), reads=[Bprm], writes=[Bder])
        S.op("dve", lambda e: e.tensor_scalar(out=nA[:], in0=nA[:], scalar1=-1.0, scalar2=None, op0=ALU.mult), reads=[Bder], writes=[Bder])
        S.op("act", lambda e: e.activation(out=esink[:], in_=prm[:, P_SINK:P_SINK + 8], func=AF.Exp), reads=[Bprm], writes=[Bder])

        S.mark("m0")
        xt = [sb(f"xt{i}", [128, 1024]) for i in range(4)]; Bxt = [S.buf(f"xt{i}") for i in range(4)]
        stat = [sb(f"stat{i}", [128, 2]) for i in range(2)]; Bstat = [S.buf(f"stat{i}") for i in range(2)]
        xn = [sb(f"xn{i}", [128, 1024], BF16) for i in range(2)]; Bxn = [S.buf(f"xn{i}") for i in range(2)]
        hT = sb("hT", [128, 8, 512], BF16); BhT = S.buf("hT")
        Ub = [sb(f"Ub{i}", [128, 516]) for i in range(2)]; BUb = [S.buf(f"Ub{i}") for i in range(2)]
        halo = sb("halo", [128, 12, 4]); Bhalo = [S.buf(f"halo{c}") for c in range(12)]
        ctmp = [sb(f"ctmp{i}", [128, 512]) for i in range(2)]; Bctmp = [S.buf(f"ctmp{i}") for i in range(2)]
        cs2 = [sb(f"csil{i}", [128, 512]) for i in range(2)]; Bcs2 = [S.buf(f"csil{i}") for i in range(2)]
        ctmp2 = None; Bctmp2 = None
        sqb2 = [sb(f"sqb{i}", [128, 512], BF16) for i in range(2)]; Bsqb2 = [S.buf(f"sqb{i}") for i in range(2)]
        rst = sb("rst", [128, 512]); Brst = S.buf("rst")
        fm = sb("fm", [128, 12, 512], BF16); Bfm = [S.buf(f"fm{c}") for c in range(12)]
        swq = sb("swq", [128, 8, 512], BF16); Bswq = [S.buf(f"swq{c}") for c in range(4)]
        swk = sb("swk", [128, 2, 640], BF16); Bswk = [S.buf(f"swk{c}") for c in range(2)]
        vv = sb("vv", [128, 5, 2, 128], BF16); Bvv = [S.buf(f"vv{t}") for t in range(5)]
        siluz = sb("siluz", [128, 4, 512], BF16); Bsz = [S.buf(f"sz{t}") for t in range(4)]
        tmf = sb("tmf", [128, 4, 32]); Btmf = [S.buf(f"tmf{t}") for t in range(4)]
        S32 = sb("S32", [128, 4, 128]); Sbf = sb("Sbf", [128, 4, 128], BF16); BS = [S.buf(f"S{h}") for h in range(4)]
        mixT = sb("mixT", [128, 8, 512], BF16); Bmix = [S.buf(f"mix{t}") for t in range(4)]
        Kdec = sb("Kdec", [128, 2, 4, 128], BF16); Vtm = sb("Vtm", [128, 2, 4, 128], BF16)
        Aqk = sb("Aqk", [128, 2, 4, 128], BF16); Minv = sb("Minv", [128, 2, 4, 128], BF16)
        Bdn = [[S.buf(f"dn{t}_{h}") for h in range(4)] for t in range(2)]
        Bfull = sb("Bfull", [128, 4, 2, 128], BF16); BBf = [S.buf(f"Bf{h}") for h in range(4)]
        WT = sb("WT", [128, 4, 2, 128], BF16); BWT = [S.buf(f"WT{h}") for h in range(4)]
        T1s = sb("T1s", [128, 4, 128], BF16); BT1 = [S.buf(f"T1{h}") for h in range(4)]
        D2T = sb("D2T", [128, 4, 128], BF16); BD2T = [S.buf(f"D2T{h}") for h in range(4)]
        lg = sb("lg", [128, 4, 128]); Blg = [S.buf(f"lg{h}") for h in range(4)]
        DT = sb("DT", [128, 4, 128]); BDT = [S.buf(f"DT{h}") for h in range(4)]
        ZPR = sb("ZPR", [128, 4, 3, 128], BF16); BZPR = [S.buf(f"ZPR{h}") for h in range(4)]
        PTt = sb("PTt", [128, 4, 128], BF16); BPTt = [S.buf(f"PTt{h}") for h in range(4)]
        Rm = sb("Rm", [128, 4, 128], BF16); BRm = [S.buf(f"Rm{h}") for h in range(4)]
        vnew = sb("vnew", [128, 4, 128], BF16); Bvn = [S.buf(f"vn{h}") for h in range(4)]
        QSs = sb("QSs", [128, 4, 128]); BQSs = [S.buf(f"QSs{h}") for h in range(4)]
        ot = sb("ot", [128, 4, 128]); Bot = [S.buf(f"ot{h}") for h in range(4)]
        om = sb("om", [128, 4, 128], BF16); Bom = [S.buf(f"om{h}") for h in range(4)]
        ost = sb("ost", [128, 4, 2]); Bost = [S.buf(f"ost{h}") for h in range(4)]
        sc = [sb(f"sc{i}", [128, 512]) for i in range(2)]; Bsc = [S.buf(f"sc{i}") for i in range(2)]
        pTt = sb("pTt", [128, 2, 512], BF16); BpT = [S.buf(f"pT{i}") for i in range(2)]
        den = sb("den", [128, 512]); Bden = S.buf("den")
        x1t = [sb(f"x1t{i}", [128, 1024]) for i in range(1)] * 2; Bx1t = [S.buf(f"x1t{i}") for i in range(1)] * 2
        Bout = S.buf("out")
        BP4 = [BPB[4] for h in range(4)]

        S.op("pool", lambda e: e.memset(halo[:], 0.0), writes=Bhalo)
        S.op("pool", lambda e: e.memset(S32[:], 0.0), writes=BS)
        S.op("pool", lambda e: e.memset(Sbf[:], 0.0), writes=BS)
        S.op("pool", lambda e: e.memset(ZPR[:], 0.0), writes=BZPR)
        S.op("pool", lambda e: e.memset(swk[:], 0.0), writes=Bswk)
        S.op("pool", lambda e: e.memset(vv[:], 0.0), writes=Bvv)

        S.mark("m1")
        Bwcv = S.buf("wcv")
        conv_q = []
        if SPARSE and stage >= 2:
            for c in range(64):
                conv_q.append(lambda e, c=c: e.dma_start(out=wub_d[c * 64:(c + 1) * 64, :].rearrange("r (k n) -> r k n", k=8), in_=wupg_d[c * 64:(c + 1) * 64, :, :]))
                conv_q.append(lambda e, c=c: e.dma_start(out=wdb_d[c * 64:(c + 1) * 64, :].rearrange("r (k n) -> r k n", k=8), in_=wdng_d[c * 64:(c + 1) * 64, :, :]))

        def conv_step(n=1, stage=None):
            for _ in range(n):
                if conv_q:
                    if stage is None:
                        S.dma("pool", conv_q.pop(0), writes=[Bwcv])
                    else:
                        G.dma(stage, "pool", conv_q.pop(0), writes=[Bwcv])
        ident_b = csb(C_ID)
        ones_b = csb(C_ONE)
        bd_b = csb(C_BD)
        rr = {"fmps": 0, "eng": 0}

        def evac_eng():
            rr["eng"] += 1
            return "act" if rr["eng"] % 2 else "dve"

        def supertile(phase, sti):
            own = (phase == 1)
            xsrc = xo_d if own else xp_d
            for tl in range(4):
                gt = sti * 4 + tl
                i2 = gt % 2
                S.dma("sp", lambda e, gt=gt, tl=tl: e.dma_start(out=xt[tl][:], in_=xsrc[gt * 128:(gt + 1) * 128, :]), writes=[Bxt[tl]])
                S.mark("m2")
                S.op("act", lambda e, i2=i2, tl=tl: e.activation(out=xn[i2][:], in_=xt[tl][:], func=AF.Square, accum_out=stat[i2][:, 0:1]),
                     reads=[Bxt[tl]], writes=[Bxn[i2], Bstat[i2]])
                S.mark("a1")
                S.op("act", lambda e, i2=i2: e.activation(out=stat[i2][:, 1:2], in_=stat[i2][:, 0:1], func=AF.Sqrt, scale=1.0 / 1024, bias=EPS),
                     reads=[Bstat[i2]], writes=[Bstat[i2]])
                S.op("dve", lambda e, i2=i2: e.reciprocal(out=stat[i2][:, 1:2], in_=stat[i2][:, 1:2]), reads=[Bstat[i2]], writes=[Bstat[i2]])
                S.op("dve", lambda e, i2=i2, tl=tl: e.tensor_scalar(out=xn[i2][:], in0=xt[tl][:], scalar1=stat[i2][:, 1:2], scalar2=None, op0=ALU.mult),
                     reads=[Bxt[tl], Bstat[i2]], writes=[Bxn[i2]])
                S.mark("a2")
                for k in range(8):
                    S.op("pe", lambda e, i2=i2, k=k: e.transpose(PT[:, k * 128:(k + 1) * 128], xn[i2][:, k * 128:(k + 1) * 128], ident_b),
                         reads=[Bxn[i2], Bcstb], writes=[BPT[k]])
                    S.mark("a3")
                    eng = evac_eng()
                    if eng == "act":
                        S.op("act", lambda e, k=k, tl=tl: e.activation(out=hT[:, k, tl * 128:(tl + 1) * 128], in_=PT[:, k * 128:(k + 1) * 128],
                                                                      func=AF.Identity, scale=A1[:, k:k + 1], bias=B1[:, k:k + 1]),
                             reads=[BPT[k], BA1], writes=[BhT])
                        S.mark(f"ea{gt}_{k}")
                    else:
                        S.op("dve", lambda e, k=k, tl=tl: e.tensor_scalar(out=hT[:, k, tl * 128:(tl + 1) * 128], in0=PT[:, k * 128:(k + 1) * 128],
                                                                         scalar1=A1[:, k:k + 1], scalar2=B1[:, k:k + 1], op0=ALU.mult, op1=ALU.add),
                             reads=[BPT[k], BA1], writes=[BhT])
                        S.mark(f"ed{gt}_{k}")
            S.mark(f"p{phase}s{sti}a")
            chunks = list(range(18)) if own else ((list(range(0, 12)) if sti == 3 else list(range(4, 12))) + [16, 17])
            for ci, c in enumerate(chunks):
                SH = 2 * ci
                ST = 2 * ci + 3
                conv_step(1, SH)
                cs_ = cs2[ci % 2]; Bcs = Bcs2[ci % 2]; sqb = sqb2[ci % 2]; Bsqb = Bsqb2[ci % 2]
                bi = rr["fmps"] % 2
                rr["fmps"] += 1
                pb = PB[bi]; Bpb = BPB[bi]
                for k in range(8):
                    G.op(SH, "pe", lambda e, cs_=cs_, sqb=sqb, pb=pb, c=c, k=k: e.matmul(pb[:, :], lhsT=wfm[:, k, c * 128:(c + 1) * 128], rhs=hT[:, k, :],
                                                                   start=(k == 0), stop=(k == 7)), reads=[Bwfm, BhT], writes=[Bpb])
                if c < 12:
                    ci = c % 2
                    U_ = Ub[ci]; BU_ = BUb[ci]
                    G.op(SH, "pool", lambda e, cs_=cs_, sqb=sqb, c=c, U_=U_: e.tensor_copy(out=U_[:, 1:4], in_=halo[:, c, 0:3]), reads=[Bhalo[c]], writes=[BU_])
                    G.op(SH, "act", lambda e, cs_=cs_, sqb=sqb, pb=pb, U_=U_: e.activation(out=U_[:, 4:516], in_=pb[:, :], func=AF.Identity), reads=[Bpb], writes=[BU_])
                    G.op(SH, "pool", lambda e, cs_=cs_, sqb=sqb, c=c, U_=U_: e.tensor_copy(out=halo[:, c, 0:3], in_=U_[:, 513:516]), reads=[BU_], writes=[Bhalo[c]])
                    ce = "dve"
                    cw = P_CONV + c * 4
                    G.op(SH, ce, lambda e, cs_=cs_, sqb=sqb, U_=U_, ci=ci, cw=cw: e.tensor_scalar(out=ctmp[ci][:], in0=U_[:, 1:513], scalar1=prm[:, cw:cw + 1], scalar2=None, op0=ALU.mult),
                         reads=[BU_, Bprm], writes=[Bctmp[ci]])
                    for j in range(1, 4):
                        if ce == "pool":
                            G.op(SH, ce, lambda e, cs_=cs_, sqb=sqb, U_=U_, cw=cw, j=j: e.tensor_scalar(out=ctmp2[:], in0=U_[:, 1 + j:513 + j], scalar1=prm[:, cw + j:cw + j + 1], scalar2=None, op0=ALU.mult),
                                 reads=[BU_, Bprm], writes=[Bctmp2])
                            G.op(SH, ce, lambda e, cs_=cs_, sqb=sqb, ci=ci: e.tensor_tensor(out=ctmp[ci][:], in0=ctmp[ci][:], in1=ctmp2[:], op=ALU.add), reads=[Bctmp[ci], Bctmp2], writes=[Bctmp[ci]])
                            continue
                        G.op(SH, ce, lambda e, cs_=cs_, sqb=sqb, U_=U_, ci=ci, cw=cw, j=j: e.scalar_tensor_tensor(out=ctmp[ci][:], in0=U_[:, 1 + j:513 + j], scalar=prm[:, cw + j:cw + j + 1],
                                                                                         in1=ctmp[ci][:], op0=ALU.mult, op1=ALU.add),
                             reads=[BU_, Bprm, Bctmp[ci]], writes=[Bctmp[ci]])
                    if c >= 8:
                        G.op(SH, "act", lambda e, cs_=cs_, sqb=sqb, c=c, ci=ci: e.activation(out=fm[:, c, :], in_=ctmp[ci][:], func=AF.Silu), reads=[Bctmp[ci]], writes=[Bfm[c]])
                    else:
                        G.op(SH, "act", lambda e, cs_=cs_, sqb=sqb, ci=ci: e.activation(out=cs_[:], in_=ctmp[ci][:], func=AF.Silu), reads=[Bctmp[ci]], writes=[Bcs])
                        G.op(SH, "pool", lambda e, cs_=cs_, sqb=sqb: e.tensor_tensor(out=sqb[:], in0=cs_[:], in1=cs_[:], op=ALU.mult), reads=[Bcs], writes=[Bsqb])
                        G.op(ST, "pe", lambda e, cs_=cs_, sqb=sqb: e.matmul(PB[2][:, :], lhsT=ones_b, rhs=sqb[:], start=True, stop=True), reads=[Bsqb, Bcstb], writes=[BPB[2]])
                        G.op(ST, "act", lambda e, cs_=cs_, sqb=sqb: e.activation(out=rst[:], in_=PB[2][:, :], func=AF.Sqrt, bias=EPS, scale=1.0), reads=[BPB[2]], writes=[Brst])
                        G.op(ST, "dve", lambda e, cs_=cs_, sqb=sqb: e.reciprocal(out=rst[:], in_=rst[:]), reads=[Brst], writes=[Brst])
                        qs = (128.0 ** -0.5) if c < 4 else 1.0
                        G.op(ST, "dve", lambda e, cs_=cs_, sqb=sqb, c=c, qs=qs: e.scalar_tensor_tensor(out=fm[:, c, :], in0=cs_[:], scalar=qs, in1=rst[:], op0=ALU.mult, op1=ALU.mult),
                             reads=[Bcs, Brst], writes=[Bfm[c]])
                else:
                    G.op(SH, "act", lambda e, cs_=cs_, sqb=sqb, pb=pb: e.activation(out=cs_[:], in_=pb[:, :], func=AF.Identity), reads=[Bpb], writes=[Bcs])
                    G.op(SH, "pool", lambda e, cs_=cs_, sqb=sqb: e.tensor_tensor(out=sqb[:], in0=cs_[:], in1=cs_[:], op=ALU.mult), reads=[Bcs], writes=[Bsqb])
                    G.op(ST, "pe", lambda e, cs_=cs_, sqb=sqb: e.matmul(PB[2][:, :], lhsT=bd_b, rhs=sqb[:], start=True, stop=True), reads=[Bsqb, Bcstb], writes=[BPB[2]])
                    G.op(ST, "act", lambda e, cs_=cs_, sqb=sqb: e.activation(out=rst[:], in_=PB[2][:, :], func=AF.Sqrt, bias=EPS, scale=1.0 / 64), reads=[BPB[2]], writes=[Brst])
                    G.op(ST, "dve", lambda e, cs_=cs_, sqb=sqb: e.reciprocal(out=rst[:], in_=rst[:]), reads=[Brst], writes=[Brst])
                    if c < 16:
                        for par in range(2):
                            G.op(ST, "dve", lambda e, cs_=cs_, sqb=sqb, c=c, par=par: e.scalar_tensor_tensor(out=swq[:, 2 * (c - 12) + par, :], in0=cs_[:], scalar=prm[:, (P_QNW if par == 0 else P_QNW_HI):(P_QNW if par == 0 else P_QNW_HI) + 1], in1=rst[:],
                                                                                      op0=ALU.mult, op1=ALU.mult), reads=[Bcs, Brst, Bprm], writes=[Bswq[c - 12]])
                    else:
                        j = c - 16
                        G.op(ST, "pool", lambda e, cs_=cs_, sqb=sqb, j=j: e.tensor_copy(out=swk[:, j, 0:128], in_=swk[:, j, 512:640]), reads=[Bswk[j]], writes=[Bswk[j]])
                        G.op(ST, "dve", lambda e, cs_=cs_, sqb=sqb, j=j: e.scalar_tensor_tensor(out=swk[:, j, 128:640], in0=cs_[:], scalar=prm[:, P_KNW:P_KNW + 1], in1=rst[:],
                                                                         op0=ALU.mult, op1=ALU.mult), reads=[Bcs, Brst, Bprm], writes=[Bswk[j]])
            G.run()
            S.mark(f"p{phase}s{sti}b")
            S.op("pool", lambda e: e.tensor_copy(out=vv[:, 0, :, :], in_=vv[:, 4, :, :]), reads=[Bvv[4]], writes=[Bvv[0]])
            for tl in range(4):
                tsl = slice(tl * 128, (tl + 1) * 128)
                if own:
                    for k in range(8):
                        S.op("pe", lambda e, k=k, tsl=tsl: e.matmul(PB[3][:, :], lhsT=hT[:, k, tsl], rhs=wtm[:, k, 0:512], start=(k == 0), stop=(k == 7)),
                             reads=[BhT, Bwtm], writes=[BPB[3]])
                    S.op("act", lambda e, tl=tl: e.activation(out=siluz[:, tl, :], in_=PB[3][:, :], func=AF.Silu), reads=[BPB[3]], writes=[Bsz[tl]])
                for k in range(8):
                    S.op("pe", lambda e, k=k, tsl=tsl: e.matmul(PB[4][:, 0:136], lhsT=hT[:, k, tsl], rhs=wtm[:, k, 512:648], start=(k == 0), stop=(k == 7)),
                         reads=[BhT, Bwtm], writes=[BPB[4]])
                T = tmf[:, tl, :]
                Bt = Btmf[tl]
                S.op("dve", lambda e, T=T: e.tensor_tensor(out=T[:, 28:32], in0=PB[4][:, 0:4], in1=prm[:, P_DTB:P_DTB + 4], op=ALU.add), reads=[BPB[4], Bprm], writes=[Bt])
                S.op("act", lambda e, T=T: e.activation(out=T[:, 28:32], in_=T[:, 28:32], func=AF.Exp), reads=[Bt], writes=[Bt])
                S.op("act", lambda e, T=T: e.activation(out=T[:, 28:32], in_=T[:, 28:32], func=AF.Ln, bias=1.0, scale=1.0), reads=[Bt], writes=[Bt])
                S.op("dve", lambda e, T=T: e.tensor_tensor(out=T[:, 0:4], in0=T[:, 28:32], in1=nA[:], op=ALU.mult), reads=[Bt, Bder], writes=[Bt])
                S.op("act", lambda e, T=T: e.activation(out=T[:, 4:8], in_=PB[4][:, 4:8], func=AF.Sigmoid), reads=[BPB[4]], writes=[Bt])
                S.op("dve", lambda e, T=T: e.tensor_scalar(out=T[:, 24:28], in0=T[:, 4:8], scalar1=-1.0, scalar2=None, op0=ALU.mult), reads=[Bt], writes=[Bt])
                for j in range(2):
                    for d in range(2):
                        S.op("act" if d == 0 else "dve",
                             (lambda e, tl=tl, j=j, d=d: e.activation(out=vv[:, 1 + tl, j, d * 64:(d + 1) * 64], in_=PB[4][:, 8 + j * 64: 8 + (j + 1) * 64], func=AF.Identity))
                             if d == 0 else
                             (lambda e, tl=tl, j=j, d=d: e.tensor_copy(out=vv[:, 1 + tl, j, d * 64:(d + 1) * 64], in_=PB[4][:, 8 + j * 64: 8 + (j + 1) * 64])),
                             reads=[BPB[4]], writes=[Bvv[1 + tl]])
                S.op("pe", lambda e, T=T: e.matmul(PB[5][:, 0:4], lhsT=cs(C_UI), rhs=T[:, 0:4], start=True, stop=True), reads=[Bt, Bcst], writes=[BPB[5]])
                S.op("pe", lambda e, T=T: e.matmul(PB[5][:, 4:8], lhsT=cs(C_ONE), rhs=T[:, 0:4], start=True, stop=True), reads=[Bt, Bcst], writes=[BPB[5]])
                S.op("act", lambda e, T=T: e.activation(out=T[:, 8:12], in_=PB[5][:, 0:4], func=AF.Exp), reads=[BPB[5]], writes=[Bt])
                S.op("dve", lambda e, T=T: e.tensor_scalar(out=T[:, 12:16], in0=T[:, 8:12], scalar1=-1.0, scalar2=None, op0=ALU.mult), reads=[Bt], writes=[Bt])
                S.op("act", lambda e, T=T: e.activation(out=T[:, 28:32], in_=PB[5][:, 0:4], func=AF.Identity), reads=[BPB[5]], writes=[Bt])
                S.op("dve", lambda e, T=T: e.tensor_tensor(out=T[:, 28:32], in0=PB[5][:, 4:8], in1=T[:, 28:32], op=ALU.subtract), reads=[BPB[5], Bt], writes=[Bt])
                S.op("act", lambda e, T=T: e.activation(out=T[:, 16:20], in_=T[:, 28:32], func=AF.Exp), reads=[Bt], writes=[Bt])
                S.op("act", lambda e, T=T: e.activation(out=T[:, 20:24], in_=PB[5][:, 4:8], func=AF.Exp), reads=[BPB[5]], writes=[Bt])
            S.mark(f"p{phase}s{sti}b2")
            if dbg and phase == 0 and sti == 0:
                Bdbg = S.buf("dbg")
                S.dma("pool", lambda e: e.dma_start(out=dbg_d[:, 0:1536].rearrange("p (c t) -> p c t", c=12), in_=fm[:, :, 0:128]), reads=Bfm, writes=[Bdbg])
                S.dma("sp", lambda e: e.dma_start(out=dbg_d[:, 1536:1568], in_=tmf[:, 0, :]), reads=Btmf, writes=[Bdbg])
                S.dma("sp", lambda e: e.dma_start(out=dbg_d[:, 1600:1608], in_=A1[:, :]), reads=[BA1], writes=[Bdbg])
                S.dma("sp", lambda e: e.dma_start(out=dbg_d[:, 1608:1616], in_=B1[:, :]), reads=[BA1], writes=[Bdbg])
                S.dma("pool", lambda e: e.dma_start(out=dbg_d[:, 2048:3072].rearrange("p (c t) -> p c t", c=8), in_=hT[:, :, 0:128]), reads=[BhT], writes=[Bdbg])
            def dn_pre(tl):
                tsl = slice(tl * 128, (tl + 1) * 128)
                tb = tl % 2
                T = tmf[:, tl, :]
                Bt = Btmf[tl]
                for h in range(4):
                    Bd = Bdn[tb][h]
                    qT = fm[:, h, tsl]; kT = fm[:, 4 + h, tsl]; vT = fm[:, 8 + h, tsl]
                    Bq, Bk, Bv = Bfm[h], Bfm[4 + h], Bfm[8 + h]
                    G.op(0, "pe", lambda e, kT=kT, h=h: e.transpose(PT[:, h * 128:(h + 1) * 128], kT, ident_b), reads=[Bk, Bcstb], writes=[BPT[h]])
                    G.op(1, "act", lambda e, tl=tl, tb=tb, h=h, T=T: e.activation(out=Kdec[:, tb, h, :], in_=PT[:, h * 128:(h + 1) * 128], func=AF.Identity, scale=T[:, 16 + h:17 + h]),
                         reads=[BPT[h], Bt], writes=[Bd])
                    G.op(0, "pe", lambda e, vT=vT, h=h: e.transpose(PT[:, (4 + h) * 128:(5 + h) * 128], vT, ident_b), reads=[Bv, Bcstb], writes=[BPT[4 + h]])
                    G.op(1, "dve", lambda e, tl=tl, tb=tb, h=h: e.tensor_copy(out=Vtm[:, tb, h, :], in_=PT[:, (4 + h) * 128:(5 + h) * 128]), reads=[BPT[4 + h]], writes=[Bd])
                    pb = PB[h]; Bpb = BPB[h]
                    G.op(2, "pe", lambda e, pb=pb, kT=kT: e.matmul(pb[:, 0:128], lhsT=kT, rhs=kT, start=True, stop=True), reads=[Bk], writes=[Bpb])
                    if own:
                        G.op(2, "pe", lambda e, pb=pb, kT=kT, qT=qT: e.matmul(pb[:, 128:256], lhsT=kT, rhs=qT, start=True, stop=True), reads=[Bk, Bq], writes=[Bpb])
                    G.op(2, "pool", lambda e, h=h, T=T: e.tensor_scalar(out=lg[:, h, :], in0=cs(C_SL), scalar1=T[:, h:h + 1], scalar2=None, op0=ALU.mult),
                         reads=[Bcst, Bt], writes=[Blg[h]])
                    G.op(3, "pe", lambda e, pb=pb, h=h: e.matmul(pb[:, 256:384], lhsT=lg[:, h, :], rhs=cs(C_UI), start=True, stop=False), reads=[Blg[h], Bcst], writes=[Bpb])
                    G.op(3, "pe", lambda e, pb=pb: e.matmul(pb[:, 256:384], lhsT=cs(C_ID), rhs=cs(C_NEG), start=False, stop=True), reads=[Bcst], writes=[Bpb])
                    G.op(4, "act", lambda e, pb=pb, h=h: e.activation(out=DT[:, h, :], in_=pb[:, 256:384], func=AF.Exp), reads=[Bpb], writes=[BDT[h]])
                    if own:
                        G.op(5, "dve", lambda e, pb=pb, h=h, tl=tl, tb=tb: e.tensor_tensor(out=Aqk[:, tb, h, :], in0=pb[:, 128:256], in1=DT[:, h, :], op=ALU.mult),
                             reads=[Bpb, BDT[h]], writes=[Bd])
                    G.op(5, "dve", lambda e, pb=pb, h=h, T=T: e.scalar_tensor_tensor(out=lg[:, h, :], in0=pb[:, 0:128], scalar=T[:, 24 + h:25 + h], in1=DT[:, h, :],
                                                                                 op0=ALU.mult, op1=ALU.mult), reads=[Bpb, Bt, BDT[h], Blg[h]], writes=[Blg[h]])
                    G.op(6, "pool", lambda e, h=h: e.tensor_tensor(out=Bfull[:, h, 0, :], in0=lg[:, h, :], in1=cs(C_OFFD), op=ALU.mult), reads=[Blg[h], Bcst], writes=[BBf[h]])
                    G.op(7, "pe", lambda e, h=h: e.transpose(PT[:, h * 128:(h + 1) * 128], Bfull[:, h, 0, :], ident_b), reads=[BBf[h], Bcstb], writes=[BPT[h]])
                    G.op(8, "act", lambda e, h=h: e.activation(out=Bfull[:, h, 1, :], in_=PT[:, h * 128:(h + 1) * 128], func=AF.Identity), reads=[BPT[h]], writes=[BBf[h]])
                    G.op(9, "pool", lambda e, h=h: e.tensor_tensor(out=ZPR[:, h, 1, :], in0=Bfull[:, h, 0, :], in1=cstm[:, 0:128], op=ALU.mult), reads=[BBf[h], Bcstm], writes=[BZPR[h]])
                    G.op(9, "pool", lambda e, h=h: e.tensor_copy(out=ZPR[:, h, 2, :], in_=cs(C_ID)), reads=[Bcst], writes=[BZPR[h]])
                    G.op(9, "pool", lambda e, h=h: e.tensor_tensor(out=PTt[:, h, :], in0=Bfull[:, h, 1, :], in1=cstm[:, 0:128], op=ALU.mult), reads=[BBf[h], Bcstm], writes=[BPTt[h]])
                    G.op(9, "pool", lambda e, h=h: e.tensor_tensor(out=WT[:, h, 0, :], in0=Bfull[:, h, 1, :], in1=cstm[:, 128:256], op=ALU.mult), reads=[BBf[h], Bcstm], writes=[BWT[h]])
                    G.op(9, "pool", lambda e, h=h: e.tensor_tensor(out=WT[:, h, 1, :], in0=Bfull[:, h, 1, :], in1=cstm[:, 256:384], op=ALU.mult), reads=[BBf[h], Bcstm], writes=[BWT[h]])
                G.run()
                for lvl in range(5):
                    for h in range(4):
                        pb = PB[h]; Bpb = BPB[h]
                        if lvl < 4:
                            S.op("pe", lambda e, pb=pb, h=h: e.matmul(pb[:, 0:256], lhsT=PTt[:, h, :], rhs=ZPR[:, h, 1:3, :], start=True, stop=True),
                                 reads=[BPTt[h], BZPR[h]], writes=[Bpb])
                            S.op("pe", lambda e, pb=pb, h=h: e.matmul(pb[:, 256:384], lhsT=ZPR[:, h, 1, :], rhs=PTt[:, h, :], start=True, stop=True),
                                 reads=[BPTt[h], BZPR[h]], writes=[Bpb])
                            S.op("dve", lambda e, pb=pb, h=h: e.tensor_tensor(out=ZPR[:, h, 1:3, :], in0=pb[:, 0:256].rearrange("p (a b) -> p a b", a=2),
                                                                             in1=ZPR[:, h, 0:3:2, :], op=ALU.add), reads=[Bpb, BZPR[h]], writes=[BZPR[h]])
                            S.op("act", lambda e, pb=pb, h=h: e.activation(out=PTt[:, h, :], in_=pb[:, 256:384], func=AF.Identity), reads=[Bpb], writes=[BPTt[h]])
                        else:
                            S.op("pe", lambda e, pb=pb, h=h: e.matmul(pb[:, 0:128], lhsT=PTt[:, h, :], rhs=ZPR[:, h, 2, :], start=True, stop=True),
                                 reads=[BPTt[h], BZPR[h]], writes=[Bpb])
                            S.op("dve", lambda e, pb=pb, h=h: e.tensor_tensor(out=ZPR[:, h, 1, :], in0=pb[:, 0:128], in1=ZPR[:, h, 2, :], op=ALU.add),
                                 reads=[Bpb, BZPR[h]], writes=[BZPR[h]])
                for h in range(4):
                    S.op("pe", lambda e, h=h: e.transpose(PT[:, h * 128:(h + 1) * 128], ZPR[:, h, 1, :], ident_b), reads=[BZPR[h], Bcstb], writes=[BPT[h]])
                    S.op("act", lambda e, h=h: e.activation(out=PTt[:, h, :], in_=PT[:, h * 128:(h + 1) * 128], func=AF.Identity), reads=[BPT[h]], writes=[BPTt[h]])
                for h in range(4):
                    pb = PB[h]; Bpb = BPB[h]
                    S.op("pe", lambda e, pb=pb, h=h: e.matmul(pb[:, 0:128], lhsT=WT[:, h, 0, :], rhs=ZPR[:, h, 1, :], start=True, stop=True), reads=[BWT[h], BZPR[h]], writes=[Bpb])
                    S.op("act", lambda e, pb=pb, h=h: e.activation(out=T1s[:, h, :], in_=pb[:, 0:128], func=AF.Identity), reads=[Bpb], writes=[BT1[h]])
                for h in range(4):
                    pb = PB[h]; Bpb = BPB[h]
                    S.op("pe", lambda e, pb=pb, h=h: e.matmul(pb[:, 0:128], lhsT=PTt[:, h, :], rhs=T1s[:, h, :], start=True, stop=True), reads=[BPTt[h], BT1[h]], writes=[Bpb])
                    S.op("pe", lambda e, pb=pb, h=h: e.matmul(pb[:, 128:256], lhsT=T1s[:, h, :], rhs=PTt[:, h, :], start=True, stop=True), reads=[BPTt[h], BT1[h]], writes=[Bpb])
                    S.op("dve", lambda e, pb=pb, h=h: e.tensor_tensor(out=ZPR[:, h, 2, :], in0=pb[:, 0:128], in1=ZPR[:, h, 1, :], op=ALU.add), reads=[Bpb, BZPR[h]], writes=[BZPR[h]])
                    S.op("dve", lambda e, pb=pb, h=h: e.tensor_tensor(out=D2T[:, h, :], in0=pb[:, 128:256], in1=PTt[:, h, :], op=ALU.add), reads=[Bpb, BPTt[h]], writes=[BD2T[h]])
                for h in range(4):
                    pb = PB[h]; Bpb = BPB[h]
                    S.op("pe", lambda e, pb=pb, h=h: e.matmul(pb[:, 0:128], lhsT=WT[:, h, 1, :], rhs=ZPR[:, h, 2, :], start=True, stop=True), reads=[BWT[h], BZPR[h]], writes=[Bpb])
                    S.op("act", lambda e, pb=pb, h=h: e.activation(out=T1s[:, h, :], in_=pb[:, 0:128], func=AF.Identity), reads=[Bpb], writes=[BT1[h]])
                for h in range(4):
                    pb = PB[h]; Bpb = BPB[h]
                    S.op("pe", lambda e, pb=pb, h=h: e.matmul(pb[:, 0:128], lhsT=D2T[:, h, :], rhs=T1s[:, h, :], start=True, stop=True), reads=[BD2T[h], BT1[h]], writes=[Bpb])
                    S.op("dve", lambda e, pb=pb, h=h, tb=tb: e.tensor_tensor(out=Minv[:, tb, h, :], in0=pb[:, 0:128], in1=ZPR[:, h, 2, :], op=ALU.add),
                         reads=[Bpb, BZPR[h]], writes=[Bdn[tb][h]])
            def dn_scan(tl):
                tsl = slice(tl * 128, (tl + 1) * 128)
                tb = tl % 2
                T = tmf[:, tl, :]
                Bt = Btmf[tl]
                for h in range(4):
                    Bd = Bdn[tb][h]
                    pb = PB[h]; Bpb = BPB[h]
                    qT = fm[:, h, tsl]; kT = fm[:, 4 + h, tsl]
                    G.op(0, "pe", lambda e, pb=pb, kT=kT, h=h: e.matmul(pb[:, 0:128], lhsT=kT, rhs=Sbf[:, h, :], start=True, stop=True), reads=[Bfm[4 + h], BS[h]], writes=[Bpb])
                    if own:
                        G.op(0, "pe", lambda e, pb=pb, qT=qT, h=h: e.matmul(pb[:, 128:256], lhsT=qT, rhs=Sbf[:, h, :], start=True, stop=True), reads=[Bfm[h], BS[h]], writes=[Bpb])
                    G.op(1, "dve", lambda e, pb=pb, h=h, tl=tl, tb=tb, T=T: e.scalar_tensor_tensor(out=Rm[:, h, :], in0=pb[:, 0:128], scalar=T[:, 12 + h:13 + h], in1=Vtm[:, tb, h, :],
                                                                                        op0=ALU.mult, op1=ALU.add), reads=[Bpb, Bt, Bd], writes=[BRm[h]])
                    if own:
                        G.op(1, "act", lambda e, pb=pb, h=h, T=T: e.activation(out=QSs[:, h, :], in_=pb[:, 128:256], func=AF.Identity, scale=T[:, 8 + h:9 + h]),
                             reads=[Bpb, Bt], writes=[BQSs[h]])
                    G.op(2, "pe", lambda e, pb=pb, h=h, tl=tl, tb=tb: e.matmul(pb[:, 256:384], lhsT=Minv[:, tb, h, :], rhs=Rm[:, h, :], start=True, stop=True), reads=[Bd, BRm[h]], writes=[Bpb])
                    G.op(3, "act", lambda e, pb=pb, h=h, T=T: e.activation(out=vnew[:, h, :], in_=pb[:, 256:384], func=AF.Identity, scale=T[:, 4 + h:5 + h]),
                         reads=[Bpb, Bt], writes=[Bvn[h]])
                    p4 = PB[4][:, h * 128:(h + 1) * 128]
                    G.op(4, "pe", lambda e, p4=p4, h=h, tl=tl, tb=tb: e.matmul(p4, lhsT=Kdec[:, tb, h, :], rhs=vnew[:, h, :], start=True, stop=True), reads=[Bd, Bvn[h]], writes=[BP4[h]])
                    if own:
                        G.op(4, "pe", lambda e, pb=pb, h=h, tl=tl, tb=tb: e.matmul(pb[:, 384:512], lhsT=Aqk[:, tb, h, :], rhs=vnew[:, h, :], start=True, stop=True), reads=[Bd, Bvn[h]], writes=[Bpb])
                    G.op(5, "dve", lambda e, p4=p4, h=h, T=T: e.scalar_tensor_tensor(out=S32[:, h, :], in0=S32[:, h, :], scalar=T[:, 20 + h:21 + h], in1=p4, op0=ALU.mult, op1=ALU.add),
                         reads=[BP4[h], Bt, BS[h]], writes=[BS[h]])
                    G.op(6, "act", lambda e, h=h: e.activation(out=Sbf[:, h, :], in_=S32[:, h, :], func=AF.Identity), reads=[BS[h]], writes=[BS[h]])
                    if own:
                        G.op(5, "dve", lambda e, pb=pb, h=h: e.tensor_tensor(out=ot[:, h, :], in0=pb[:, 384:512], in1=QSs[:, h, :], op=ALU.add), reads=[Bpb, BQSs[h]], writes=[Bot[h]])
                        G.op(7, "act", lambda e, h=h: e.activation(out=QSs[:, h, :], in_=ot[:, h, :], func=AF.Square, accum_out=ost[:, h, 0:1]), reads=[Bot[h]], writes=[BQSs[h], Bost[h]])
                        G.op(8, "act", lambda e, h=h: e.activation(out=ost[:, h, 1:2], in_=ost[:, h, 0:1], func=AF.Sqrt, scale=1.0 / 128, bias=EPS), reads=[Bost[h]], writes=[Bost[h]])
                        G.op(9, "dve", lambda e, h=h: e.reciprocal(out=ost[:, h, 1:2], in_=ost[:, h, 1:2]), reads=[Bost[h]], writes=[Bost[h]])
                        G.op(10, "dve", lambda e, h=h: e.scalar_tensor_tensor(out=ot[:, h, :], in0=ot[:, h, :], scalar=ost[:, h, 1:2], in1=prm[:, P_DNW:P_DNW + 128], op0=ALU.mult, op1=ALU.mult),
                             reads=[Bot[h], Bost[h], Bprm], writes=[Bot[h]])
                        G.op(11, "pool", lambda e, h=h, tl=tl, tb=tb: e.tensor_tensor(out=om[:, h, :], in0=ot[:, h, :], in1=siluz[:, tl, h * 128:(h + 1) * 128], op=ALU.mult),
                             reads=[Bot[h], Bsz[tl]], writes=[Bom[h]])
                        G.op(12, "pe", lambda e, h=h: e.transpose(PT[:, h * 128:(h + 1) * 128], om[:, h, :], ident_b), reads=[Bom[h], Bcstb], writes=[BPT[h]])
                        G.op(13, "act", lambda e, h=h, tsl=tsl: e.activation(out=mixT[:, h, tsl], in_=PT[:, h * 128:(h + 1) * 128], func=AF.Identity), reads=[BPT[h]], writes=[Bmix[tl]])
                G.run()
            dn_pre(0)
            if dbg and phase == 0 and sti == 0:
                for i_, src in enumerate((Kdec, Vtm, Minv)):
                    S.dma("pool", lambda e, i_=i_, src=src: e.dma_start(out=dbg_d[:, 2048 + i_ * 512: 2048 + (i_ + 1) * 512].rearrange("p (h t) -> p h t", h=4), in_=src[:, 0, :, :]),
                          reads=Bdn[0] + [Bdbg], writes=[Bdbg])
            S.mark(f"p{phase}s{sti}c")
            dn_pre(1)
            dn_scan(0)
            dn_pre(2)
            dn_scan(1)
            dn_pre(3)
            dn_scan(2)
            dn_scan(3)
            S.mark(f"p{phase}s{sti}d")
            if not own:
                return
            for tl in range(4):
                gt = sti * 4 + tl
                for j in range(2):
                    for kb in range(2):
                        k0 = tl * 128 + kb * 128
                        pb = PB[kb]; Bpb = BPB[kb]
                        for g in range(4):
                            h = j * 4 + g
                            pr = (h % 2) * 64
                            S.op("pe", lambda e, pb=pb, j=j, k0=k0, pr=pr, h=h, g=g, tl=tl: e.matmul(
                                pb[:, g * 128:(g + 1) * 128], lhsT=swk[:, j, k0:k0 + 128], rhs=swq[:, h, tl * 128:(tl + 1) * 128],
                                start=True, stop=True), reads=[Bswk[j], Bswq[h // 2]], writes=[Bpb])
                        bofs = C_SWB + (kb * 8 + j * 4) * 128
                        S.op("dve", lambda e, pb=pb, kb=kb, bofs=bofs: e.scalar_tensor_tensor(out=sc[kb][:], in0=pb[:, :], scalar=0.125, in1=cst[:, bofs:bofs + 512],
                                                                                            op0=ALU.mult, op1=ALU.add), reads=[Bpb, Bcst], writes=[Bsc[kb]])
                        if kb == 0 and gt == 0:
                            S.op("act", lambda e, kb=kb: e.activation(out=pTt[:, kb, :], in_=sc[kb][:], func=AF.Exp, bias=prm[:, P_HALO:P_HALO + 1], scale=1.0),
                                 reads=[Bsc[kb], Bprm], writes=[BpT[kb]])
                        else:
                            S.op("act", lambda e, kb=kb: e.activation(out=pTt[:, kb, :], in_=sc[kb][:], func=AF.Exp), reads=[Bsc[kb]], writes=[BpT[kb]])
                    for kb in range(2):
                        S.op("pe", lambda e, kb=kb, tl=tl, j=j: e.matmul(PB[2][:, :], lhsT=vv[:, tl + kb, j, :], rhs=pTt[:, kb, :], start=(kb == 0), stop=(kb == 1)),
                             reads=[Bvv[tl + kb], BpT[kb]], writes=[BPB[2]])
                    for kb in range(2):
                        S.op("pe", lambda e, kb=kb: e.matmul(PB[3][:, :], lhsT=ones_b, rhs=pTt[:, kb, :], start=(kb == 0), stop=(kb == 1)),
                             reads=[Bcstb, BpT[kb]], writes=[BPB[3]])
                    for g in range(4):
                        h = j * 4 + g
                        S.op("dve", lambda e, g=g, h=h: e.tensor_scalar(out=den[:, g * 128:(g + 1) * 128], in0=PB[3][:, g * 128:(g + 1) * 128], scalar1=esink[:, h:h + 1],
                                                                       scalar2=None, op0=ALU.add), reads=[BPB[3], Bder], writes=[Bden])
                    S.op("dve", lambda e: e.reciprocal(out=den[:], in_=den[:]), reads=[Bden], writes=[Bden])
                    for g in range(4):
                        h = j * 4 + g
                        pr = (h % 2) * 64
                        S.op("dve", lambda e, g=g, h=h, pr=pr, tl=tl: e.tensor_tensor(out=mixT[pr:pr + 64, 4 + h // 2, tl * 128:(tl + 1) * 128],
                                                                                     in0=PB[2][pr:pr + 64, g * 128:(g + 1) * 128], in1=den[pr:pr + 64, g * 128:(g + 1) * 128], op=ALU.mult),
                             reads=[BPB[2], Bden], writes=[Bmix[tl]])
            S.mark(f"p{phase}s{sti}e")
            for tl in range(4):
                gt = sti * 4 + tl
                i2 = gt % 2
                for hh in range(2):
                    pb = PB[5 + hh]; Bpb = BPB[5 + hh]
                    for k in range(8):
                        S.op("pe", lambda e, pb=pb, k=k, tl=tl, hh=hh: e.matmul(pb[:, :], lhsT=mixT[:, k, tl * 128:(tl + 1) * 128], rhs=wout[:, k, hh * 512:(hh + 1) * 512],
                                                                               start=(k == 0), stop=(k == 7)), reads=[Bmix[tl], Bwout], writes=[Bpb])
                    S.op("dve", lambda e, pb=pb, i2=i2, hh=hh: e.tensor_tensor(out=x1t[i2][:, hh * 512:(hh + 1) * 512], in0=pb[:, :], in1=gate1[:, hh * 512:(hh + 1) * 512], op=ALU.mult),
                         reads=[Bpb, Bmodbc], writes=[Bx1t[i2]])
                    S.op("pool", lambda e, tl=tl, hh=hh, i2=i2: e.tensor_tensor(out=x1t[i2][:, hh * 512:(hh + 1) * 512], in0=x1t[i2][:, hh * 512:(hh + 1) * 512],
                                                                               in1=xt[tl][:, hh * 512:(hh + 1) * 512], op=ALU.add), reads=[Bx1t[i2], Bxt[tl]], writes=[Bx1t[i2]])
                S.dma("sp", lambda e, gt=gt, i2=i2: e.dma_start(out=out_d[gt * 128:(gt + 1) * 128, :], in_=x1t[i2][:]), reads=[Bx1t[i2]], writes=[Bout])

        for phase in range(2):
            for sti in range(4):
                supertile(phase, sti)
            if phase == 0:
                for h in range(4):
                    S.op("dve", lambda e, h=h: e.tensor_scalar(out=S32[:, h, :], in0=S32[:, h, :], scalar1=prm[:, P_FLAG:P_FLAG + 1], scalar2=None, op0=ALU.mult),
                         reads=[BS[h], Bprm], writes=[BS[h]])
                    S.op("act", lambda e, h=h: e.activation(out=Sbf[:, h, :], in_=S32[:, h, :], func=AF.Identity), reads=[BS[h]], writes=[BS[h]])
                for c in range(12):
                    S.op("dve", lambda e, c=c: e.tensor_scalar(out=halo[:, c, :], in0=halo[:, c, :], scalar1=prm[:, P_FLAG:P_FLAG + 1], scalar2=None, op0=ALU.mult),
                         reads=[Bhalo[c], Bprm], writes=[Bhalo[c]])


        conv_step(1000)
        S.mark("mixer_done")
        S.barrier()
        S.flush(); st.close()
        st = ExitStack()

        if stage >= 2 and SPARSE and not S.stopped:
            xs_d = nc.dram_tensor("xs", [NBLK * 128, 1024], BF16, kind="Internal").ap(); Bxs = S.buf("xs")
            ys_d = nc.dram_tensor("ysc", [NBLK * 128, 1024], F32, kind="Internal").ap(); Bys = S.buf("ys")
            h2s_d = nc.dram_tensor("h2s", [2048, 1024], BF16, kind="Internal").ap(); Bh2s = S.buf("h2s")
            sh2 = sb("sh2", [128, 1024]); w2s = sb("w2s", [128, 1024]); g2 = sb("g2", [128, 1024]); Bmb = S.buf("mb")
            for i_, dst in enumerate((sh2, w2s, g2)):
                S.dma("sp", lambda e, i_=i_, dst=dst: e.dma_start(out=dst[:], in_=scr_d[i_:i_ + 1, :].to_broadcast([128, 1024])), reads=[Bscr], writes=[Bmb])
            bdn = sb("bdn", [32, 1024]); Bbdn = S.buf("bdn")
            S.dma("sp", lambda e: e.dma_start(out=bdn[:], in_=bdn_d), writes=[Bbdn])
            prm2 = sb("prm2", [128, 32]); Bprm2 = S.buf("prm2")
            S.dma("sp", lambda e: e.dma_start(out=prm2[:], in_=prm2_d[:, 0:32]), writes=[Bprm2])
            prm3 = sb("prm3", [128, 160]); Bprm3 = S.buf("prm3")
            S.dma("sp", lambda e: e.dma_start(out=prm3[:], in_=prm3_d), writes=[Bprm3])
            Rk = sb("Rk", [128, 16, 32]); I4 = sb("I4", [128, 16, 4]); GK = sb("GK", [128, 16, 4]); Gt = sb("Gt", [128, 16, 32])
            DIf = sb("DIf", [128, 16, 4]); DI = sb("DI", [128, 64], I32)
            Brt = [S.buf(f"rt{t}") for t in range(16)]; BDI = [S.buf(f"DI{t}") for t in range(16)]
            cntbc = sb("cntbc", [128, 32]); Bcnt = S.buf("cnt")
            rtg = sb("rtg", [128, 4, 32]); Brtg = S.buf("rtg")
            ebf = sb("ebf", [128, 96]); sam = sb("sam", [128, 96]); idf = sb("idf", [128, 2, 96]); idxW = sb("idxW", [128, 192], I32); Beb = S.buf("eb")
            GT = sb("GT", [32, 128]); BGT = S.buf("GT")
            S.op("pool", lambda e: e.memset(cntbc[:], 0.0), writes=[Bcnt])
            st_moe = st
            st = ExitStack()
            wr = sb("wr", [128, 8, 32]); Bwr = S.buf("wr")
            S.dma("sp", lambda e: e.dma_start(out=wr[:], in_=wr_d.rearrange("(k p) n -> p k n", p=128)), writes=[Bwr])
            xb = sb("xb", [128, 1024]); Bxb = S.buf("xb")
            h2f = sb("h2f", [128, 1024]); Bh2f = S.buf("h2f")
            h2b = sb("h2b", [128, 1024], BF16); Bh2b = S.buf("h2b")
            h2Tf = sb("h2Tf", [128, 8, 128]); Bh2Tf = S.buf("h2Tf")
            SUb = sb("SUb", [128, 128], BF16); BSUb = S.buf("SUb")
            Mb = sb("Mb", [128, 32], BF16); BMb = S.buf("Mb")
            sm = sb("sm", [128, 64]); Bsm = S.buf("sm")
            i8 = sb("i8", [128, 8], U32); Bi8 = S.buf("i8")
            ex = sb("ex", [128, 32]); Bex = S.buf("ex")
            dtm = sb("dtm", [128, 2, 32]); Bdtm = S.buf("dtm")
            S.op("dve", lambda e: e.tensor_tensor(out=SUb[:], in0=cs(C_UI), in1=cs(C_OFFD), op=ALU.mult), reads=[Bcst], writes=[BSUb])
            for t in range(16):
                S.dma("sp", lambda e, t=t: e.dma_start(out=xb[:], in_=out_d[t * 128:(t + 1) * 128, :]), reads=[Bout], writes=[Bxb])
                S.op("act", lambda e: e.activation(out=h2f[:], in_=xb[:], func=AF.Square, accum_out=sm[:, 11:12]), reads=[Bxb], writes=[Bh2f, Bsm])
                S.op("act", lambda e: e.activation(out=sm[:, 12:13], in_=sm[:, 11:12], func=AF.Sqrt, scale=1.0 / 1024, bias=EPS), reads=[Bsm], writes=[Bsm])
                S.op("dve", lambda e: e.reciprocal(out=sm[:, 12:13], in_=sm[:, 12:13]), reads=[Bsm], writes=[Bsm])
                S.op("dve", lambda e: e.scalar_tensor_tensor(out=h2f[:], in0=xb[:], scalar=sm[:, 12:13], in1=w2s[:], op0=ALU.mult, op1=ALU.mult), reads=[Bxb, Bsm, Bmb, Bh2f], writes=[Bh2f])
                S.op("dve", lambda e: e.tensor_tensor(out=h2f[:], in0=h2f[:], in1=sh2[:], op=ALU.add), reads=[Bh2f, Bmb], writes=[Bh2f])
                S.op("act", lambda e: e.activation(out=h2b[:], in_=h2f[:], func=AF.Identity), reads=[Bh2f], writes=[Bh2b])
                S.dma("sp", lambda e, t=t: e.dma_start(out=h2s_d[t * 128:(t + 1) * 128, :], in_=h2b[:]), reads=[Bh2b], writes=[Bh2s])
                for b2 in range(2):
                    for kk in range(4):
                        k = b2 * 4 + kk
                        S.op("pe", lambda e, b2=b2, kk=kk, k=k: e.transpose(PB[b2][:, kk * 128:(kk + 1) * 128], h2f[:, k * 128:(k + 1) * 128], cs(C_ID)), reads=[Bh2f, Bcst], writes=[BPB[b2]])
                    S.op("dve", lambda e, b2=b2: e.tensor_copy(out=h2Tf[:, b2 * 4:(b2 + 1) * 4, :], in_=PB[b2][:, :].rearrange("p (a b) -> p a b", a=4)), reads=[BPB[b2]], writes=[Bh2Tf])
                for k in range(8):
                    S.op("pe", lambda e, k=k: e.matmul(PB[6][:, 0:32], lhsT=h2Tf[:, k, :], rhs=wr[:, k, :], start=(k == 0), stop=(k == 7)), reads=[Bh2Tf, Bwr], writes=[BPB[6]])
                S.op("dve", lambda e: e.tensor_tensor(out=sm[:, 16:48], in0=PB[6][:, 0:32], in1=prm2[:, 0:32], op=ALU.add), reads=[BPB[6], Bprm2, Bsm], writes=[Bsm])
                S.op("dve", lambda e: e.max(out=sm[:, 0:8], in_=sm[:, 16:48]), reads=[Bsm], writes=[Bsm])
                S.op("dve", lambda e: e.max_index(out=i8[:], in_max=sm[:, 0:8], in_values=sm[:, 16:48]), reads=[Bsm], writes=[Bi8])
                S.op("dve", lambda e, t=t: e.tensor_copy(out=I4[:, t, :], in_=i8[:, 0:4]), reads=[Bi8], writes=[Brt[t]])
                S.op("dve", lambda e: e.tensor_scalar(out=sm[:, 8:9], in0=sm[:, 0:1], scalar1=-1.0, scalar2=None, op0=ALU.mult), reads=[Bsm], writes=[Bsm])
                S.op("act", lambda e: e.activation(out=ex[:], in_=sm[:, 16:48], func=AF.Exp, bias=sm[:, 8:9], scale=1.0), reads=[Bsm], writes=[Bex])
                S.op("act", lambda e: e.activation(out=sm[:, 48:52], in_=sm[:, 0:4], func=AF.Exp, bias=sm[:, 8:9], scale=1.0), reads=[Bsm], writes=[Bsm])
                S.op("dve", lambda e: e.tensor_scalar(out=Mb[:], in0=sm[:, 16:48], scalar1=sm[:, 3:4], scalar2=None, op0=ALU.is_ge), reads=[Bsm], writes=[BMb])
                S.op("dve", lambda e: e.scalar_tensor_tensor(out=ex[:], in0=sm[:, 16:48], scalar=sm[:, 3:4], in1=ex[:], op0=ALU.is_ge, op1=ALU.mult), reads=[Bsm, Bex], writes=[Bex])
                S.op("dve", lambda e: e.reduce_sum(out=sm[:, 9:10], in_=ex[:], axis=AX.X), reads=[Bex, Bsm], writes=[Bsm])
                S.op("dve", lambda e: e.reciprocal(out=sm[:, 10:11], in_=sm[:, 9:10]), reads=[Bsm], writes=[Bsm])
                S.op("dve", lambda e, t=t: e.tensor_scalar(out=Gt[:, t, :], in0=ex[:], scalar1=sm[:, 10:11], scalar2=None, op0=ALU.mult), reads=[Bex, Bsm], writes=[Brt[t]])
                S.op("dve", lambda e, t=t: e.tensor_scalar(out=GK[:, t, :], in0=sm[:, 48:52], scalar1=sm[:, 10:11], scalar2=None, op0=ALU.mult), reads=[Bsm], writes=[Brt[t]])
                S.op("pe", lambda e: e.matmul(PB[6][:, 32:64], lhsT=SUb[:], rhs=Mb[:], start=True, stop=True), reads=[BSUb, BMb], writes=[BPB[6]])
                S.op("pe", lambda e: e.matmul(PB[6][:, 64:96], lhsT=ones_b, rhs=Mb[:], start=True, stop=True), reads=[Bcstb, BMb], writes=[BPB[6]])
                S.op("dve", lambda e, t=t: e.tensor_tensor(out=Rk[:, t, :], in0=PB[6][:, 32:64], in1=cntbc[:], op=ALU.add), reads=[BPB[6], Bcnt], writes=[Brt[t]])
                S.op("dve", lambda e: e.tensor_tensor(out=cntbc[:], in0=PB[6][:, 64:96], in1=cntbc[:], op=ALU.add), reads=[BPB[6], Bcnt], writes=[Bcnt])
                S.mark(f"m_fe{t}")
            S.op("dve", lambda e: e.tensor_scalar(out=rtg[:, 0, :], in0=cntbc[:], scalar1=0.0, scalar2=None, op0=ALU.is_gt), reads=[Bcnt], writes=[Brtg])
            for j in range(1, 16):
                S.op("dve", lambda e, j=j: e.scalar_tensor_tensor(out=rtg[:, 0, :], in0=cntbc[:], scalar=128.0 * j, in1=rtg[:, 0, :], op0=ALU.is_gt, op1=ALU.add), reads=[Bcnt, Brtg], writes=[Brtg])
            S.op("dve", lambda e: e.tensor_copy(out=rtg[:, 1, :], in_=rtg[:, 0, :]), reads=[Brtg], writes=[Brtg])
            a_, b_ = 1, 2
            for sh in (1, 2, 4, 8, 16):
                S.op("dve", lambda e, a_=a_, b_=b_, sh=sh: e.tensor_tensor(out=rtg[:, b_, sh:32], in0=rtg[:, a_, sh:32], in1=rtg[:, a_, 0:32 - sh], op=ALU.add), reads=[Brtg], writes=[Brtg])
                S.op("dve", lambda e, a_=a_, b_=b_, sh=sh: e.tensor_copy(out=rtg[:, b_, 0:sh], in_=rtg[:, a_, 0:sh]), reads=[Brtg], writes=[Brtg])
                a_, b_ = b_, a_
            incl = a_
            S.op("dve", lambda e, incl=incl: e.tensor_tensor(out=rtg[:, 3, :], in0=rtg[:, incl, :], in1=rtg[:, 0, :], op=ALU.subtract), reads=[Brtg], writes=[Brtg])
            S.op("dve", lambda e: e.tensor_scalar(out=rtg[:, 3, :], in0=rtg[:, 3, :], scalar1=128.0, scalar2=None, op0=ALU.mult), reads=[Brtg], writes=[Brtg])
            S.op("dve", lambda e, incl=incl: e.tensor_scalar(out=ebf[:], in0=prm3[:, 32:128], scalar1=rtg[:, incl, 0:1], scalar2=None, op0=ALU.is_ge), reads=[Brtg, Bprm3], writes=[Beb])
            for e_ in range(1, 32):
                S.op("dve", lambda e, e_=e_, incl=incl: e.scalar_tensor_tensor(out=ebf[:], in0=prm3[:, 32:128], scalar=rtg[:, incl, e_:e_ + 1], in1=ebf[:], op0=ALU.is_ge, op1=ALU.add),
                     reads=[Brtg, Bprm3, Beb], writes=[Beb])
            S.op("dve", lambda e: e.tensor_scalar(out=ebf[:], in0=ebf[:], scalar1=31.0, scalar2=None, op0=ALU.min), reads=[Beb], writes=[Beb])
            S.op("dve", lambda e: e.memset(sam[:], 0.0), writes=[Beb])
            S.op("dve", lambda e: e.tensor_tensor(out=sam[:, 2:96], in0=ebf[:, 2:96], in1=ebf[:, 0:94], op=ALU.is_equal), reads=[Beb], writes=[Beb])
            S.op("dve", lambda e: e.tensor_scalar(out=idf[:, 0, :], in0=ebf[:], scalar1=128.0, scalar2=prm3[:, 128:129], op0=ALU.mult, op1=ALU.add), reads=[Beb, Bprm3], writes=[Beb])
            S.op("dve", lambda e: e.scalar_tensor_tensor(out=idf[:, 1, :], in0=sam[:], scalar=1.0e6, in1=idf[:, 0, :], op0=ALU.mult, op1=ALU.add), reads=[Beb], writes=[Beb])
            S.op("dve", lambda e: e.tensor_copy(out=idxW[:], in_=idf[:, :, :].rearrange("p a b -> p (a b)")), reads=[Beb], writes=[Beb])
            S.mark("m_rt")
            for t in range(16):
                S.op("dve", lambda e, t=t: e.tensor_tensor(out=dtm[:, 0, :], in0=Rk[:, t, :], in1=rtg[:, 3, :], op=ALU.add), reads=[Brt[t], Brtg, Bdtm], writes=[Bdtm])
                for k in range(4):
                    S.op("dve", lambda e, t=t, k=k: e.scalar_tensor_tensor(out=dtm[:, 1, :], in0=prm3[:, 0:32], scalar=I4[:, t, k:k + 1], in1=dtm[:, 0, :], op0=ALU.is_equal, op1=ALU.mult),
                         reads=[Brt[t], Bprm3, Bdtm], writes=[Bdtm])
                    S.op("dve", lambda e, t=t, k=k: e.reduce_sum(out=DIf[:, t, k:k + 1], in_=dtm[:, 1, :], axis=AX.X), reads=[Bdtm], writes=[BDI[t]])
                S.op("dve", lambda e, t=t: e.tensor_copy(out=DI[:, t * 4:t * 4 + 4], in_=DIf[:, t, :]), reads=[BDI[t]], writes=[BDI[t]])
                S.dma("sp", lambda e, t=t: e.dma_start(out=h2b[:], in_=h2s_d[t * 128:(t + 1) * 128, :]), reads=[Bh2s], writes=[Bh2b])
                for k in range(4):
                    S.dma("pool", lambda e, t=t, k=k: e.indirect_dma_start(out=xs_d[:, :], out_offset=bass.IndirectOffsetOnAxis(ap=DI[:, t * 4 + k:t * 4 + k + 1], axis=0), in_=h2b[:, :], in_offset=None), reads=[Bh2b, BDI[t]], writes=[Bxs])
                S.mark(f"m_d{t}")
            S.barrier()
            S.flush(); st.close()
            st = ExitStack()
            wu = [sb(f"wu{i}", [128, 8, 2048], BF16) for i in range(2)]; Bwu = [S.buf(f"wu{i}") for i in range(2)]
            wd = [sb(f"wd{i}", [128, 8, 1024], BF16) for i in range(2)]; Bwd = [S.buf(f"wd{i}") for i in range(2)]
            bup = [sb(f"bup{i}", [128, 16]) for i in range(2)]; Bbup = [S.buf(f"bup{i}") for i in range(2)]
            xblk = [sb(f"xblk{i}", [128, 1024], BF16) for i in range(2)]; Bxblk = [S.buf(f"xblk{i}") for i in range(2)]
            xT = [sb(f"xT{i}", [128, 8, 128], BF16) for i in range(2)]; BxT = [S.buf(f"xT{i}") for i in range(2)]
            gq = sb("gq", [128, 1024]); lq = sb("lq", [128, 1024]); sgm = sb("sgm", [128, 1024]); Bgq = S.buf("gq"); Blq = S.buf("lq"); Bsgm = S.buf("sgm")
            actT = [sb(f"actT{i}", [128, 8, 128], BF16) for i in range(2)]; BactT = [S.buf(f"actT{i}") for i in range(2)]
            yblk = [sb(f"yblk{i}", [128, 1024]) for i in range(2)]; Byblk = [S.buf(f"yblk{i}") for i in range(2)]

            regs = {}

            def bnd(e):
                if "bnd" not in regs:
                    regs["bnd"] = e.alloc_register("bnd4095")
                    e.reg_mov(regs["bnd"], 4095)
                return regs["bnd"]

            def load_wu(b):
                i = b % 2
                S.dma("pool", lambda e, b=b, i=i: e.indirect_dma_start(out=wu[i][:, :, :].rearrange("p k n -> p (k n)"), out_offset=None, in_=wub_d[:, :],
                                                                    in_offset=bass.IndirectOffsetOnAxis(ap=idxW[:, 96 + b:96 + b + 1], axis=0), bounds_check=bnd(e), oob_is_err=False),
                      reads=[Beb, Bwcv], writes=[Bwu[i]])
                S.dma("pool", lambda e, b=b, i=i: e.indirect_dma_start(out=bup[i][:, :], out_offset=None, in_=bupg_d[:, :],
                                                                    in_offset=bass.IndirectOffsetOnAxis(ap=idxW[:, b:b + 1], axis=0)),
                      reads=[Beb], writes=[Bbup[i]])

            def load_wd(b):
                i = b % 2
                S.dma("pool", lambda e, b=b, i=i: e.indirect_dma_start(out=wd[i][:, :, :].rearrange("p k n -> p (k n)"), out_offset=None, in_=wdb_d[:, :],
                                                                    in_offset=bass.IndirectOffsetOnAxis(ap=idxW[:, 96 + b:96 + b + 1], axis=0), bounds_check=bnd(e), oob_is_err=False),
                      reads=[Beb, Bwcv], writes=[Bwd[i]])

            def up_blk(b):
                i = b % 2
                S.dma("sp", lambda e, b=b, i=i: e.dma_start(out=xblk[i][:], in_=xs_d[b * 128:(b + 1) * 128, :]), reads=[Bxs], writes=[Bxblk[i]])
                for k in range(8):
                    S.op("pe", lambda e, i=i, k=k: e.transpose(PT[:, k * 128:(k + 1) * 128], xblk[i][:, k * 128:(k + 1) * 128], ident_b), reads=[Bxblk[i], Bcstb], writes=[BPTb])
                S.op("act", lambda e, i=i: e.activation(out=xT[i][:, :, :], in_=PT[:, :].rearrange("p (a b) -> p a b", a=8), func=AF.Identity), reads=[BPTb], writes=[BxT[i]])
                for j in range(16):
                    pb = PB[j // 4]; Bpb = BPB[j // 4]
                    for k in range(8):
                        S.op("pe", lambda e, pb=pb, i=i, j=j, k=k: e.matmul(pb[:, (j % 4) * 128:(j % 4 + 1) * 128], lhsT=wu[i][:, k, j * 128:(j + 1) * 128], rhs=xT[i][:, k, :],
                                                                           start=(k == 0), stop=(k == 7)), reads=[Bwu[i], BxT[i]], writes=[Bpb])
                for j in range(16):
                    pb = PB[j // 4]; Bpb = BPB[j // 4]
                    dst = gq if j < 8 else lq
                    Bdst = Bgq if j < 8 else Blq
                    jj = j % 8
                    S.op("dve", lambda e, pb=pb, i=i, j=j, jj=jj, dst=dst: e.tensor_scalar(out=dst[:, jj * 128:(jj + 1) * 128], in0=pb[:, (j % 4) * 128:(j % 4 + 1) * 128], scalar1=bup[i][:, j:j + 1],
                                                                                       scalar2=7.0, op0=ALU.add, op1=ALU.min), reads=[Bpb, Bbup[i]], writes=[Bdst])
                S.op("act", lambda e: e.activation(out=sgm[:], in_=gq[:], func=AF.Sigmoid, scale=1.702), reads=[Bgq], writes=[Bsgm])
                S.op("dve", lambda e: e.tensor_scalar(out=lq[:], in0=lq[:], scalar1=-7.0, scalar2=1.0, op0=ALU.max, op1=ALU.add), reads=[Blq], writes=[Blq])
                S.op("dve", lambda e: e.tensor_tensor(out=gq[:], in0=gq[:], in1=sgm[:], op=ALU.mult), reads=[Bgq, Bsgm], writes=[Bgq])
                S.op("dve", lambda e, i=i: e.tensor_tensor(out=actT[i][:, :, :].rearrange("p a b -> p (a b)"), in0=gq[:], in1=lq[:], op=ALU.mult), reads=[Bgq, Blq], writes=[BactT[i]])

            def down_blk(b):
                i = b % 2
                for hh in range(2):
                    pb = PB[4 + hh]; Bpb = BPB[4 + hh]
                    for jd in range(8):
                        S.op("pe", lambda e, pb=pb, i=i, jd=jd, hh=hh: e.matmul(pb[:, :], lhsT=actT[i][:, jd, :], rhs=wd[i][:, jd, hh * 512:(hh + 1) * 512], start=(jd == 0), stop=(jd == 7)),
                             reads=[BactT[i], Bwd[i]], writes=[Bpb])
                    S.op("act", lambda e, pb=pb, i=i, hh=hh: e.activation(out=yblk[i][:, hh * 512:(hh + 1) * 512], in_=pb[:, :], func=AF.Identity), reads=[Bpb], writes=[Byblk[i]])
                S.dma("sp", lambda e, b=b, i=i: e.dma_start(out=ys_d[b * 128:(b + 1) * 128, :], in_=yblk[i][:]), reads=[Byblk[i]], writes=[Bys])

            load_wu(0); load_wd(0); load_wu(1); load_wd(1)
            S.mark("m_ld")
            up_blk(0)
            S.mark("m_b0")
            for b in range(1, NBLK):
                up_blk(b)
                if b + 1 < NBLK:
                    load_wu(b + 1)
                down_blk(b - 1)
                if b + 1 < NBLK:
                    load_wd(b + 1)
                S.mark(f"m_b{b}")
            down_blk(NBLK - 1)
            S.mark("m_blk")
            S.barrier()
            S.flush(); st.close()
            st = ExitStack()
            Yg = sb("Yg", [128, 4, 1024]); BYg = S.buf("Yg")
            xb = sb("xb2", [128, 1024]); Bxb = S.buf("xb2")
            acc = sb("acc", [128, 1024]); Bacc = S.buf("acc")
            for t in range(16):
                for k in range(4):
                    S.dma("pool", lambda e, t=t, k=k: e.indirect_dma_start(out=Yg[:, k, :], out_offset=None, in_=ys_d[:, :],
                                                                       in_offset=bass.IndirectOffsetOnAxis(ap=DI[:, t * 4 + k:t * 4 + k + 1], axis=0)),
                          reads=[BDI[t], Bys], writes=[BYg])
                S.dma("sp", lambda e, t=t: e.dma_start(out=xb[:], in_=out_d[t * 128:(t + 1) * 128, :]), reads=[Bout], writes=[Bxb])
                S.op("pe", lambda e, t=t: e.transpose(PB[6][0:32, 128:256], Gt[:, t, :], cs(C_ID)), reads=[Brt[t], Bcst], writes=[BPB[6]])
                S.op("act", lambda e: e.activation(out=GT[:, :], in_=PB[6][0:32, 128:256], func=AF.Identity), reads=[BPB[6]], writes=[BGT])
                S.op("dve", lambda e, t=t: e.tensor_scalar(out=acc[:], in0=Yg[:, 0, :], scalar1=GK[:, t, 0:1], scalar2=None, op0=ALU.mult), reads=[BYg, Brt[t]], writes=[Bacc])
                for k in range(1, 4):
                    S.op("dve", lambda e, t=t, k=k: e.scalar_tensor_tensor(out=acc[:], in0=Yg[:, k, :], scalar=GK[:, t, k:k + 1], in1=acc[:], op0=ALU.mult, op1=ALU.add),
                         reads=[BYg, Brt[t], Bacc], writes=[Bacc])
                for hh in range(2):
                    pb = PB[4 + hh]; Bpb = BPB[4 + hh]
                    S.op("pe", lambda e, pb=pb, hh=hh: e.matmul(pb[:, :], lhsT=GT[0:32, :], rhs=bdn[0:32, hh * 512:(hh + 1) * 512], start=True, stop=True), reads=[BGT, Bbdn], writes=[Bpb])
                    S.op("dve", lambda e, pb=pb, hh=hh: e.tensor_tensor(out=acc[:, hh * 512:(hh + 1) * 512], in0=pb[:, :], in1=acc[:, hh * 512:(hh + 1) * 512], op=ALU.add),
                         reads=[Bpb, Bacc], writes=[Bacc])
                S.op("dve", lambda e: e.tensor_tensor(out=acc[:], in0=acc[:], in1=g2[:], op=ALU.mult), reads=[Bacc, Bmb], writes=[Bacc])
                S.op("dve", lambda e: e.tensor_tensor(out=acc[:], in0=acc[:], in1=xb[:], op=ALU.add), reads=[Bacc, Bxb], writes=[Bacc])
                S.dma("sp", lambda e, t=t: e.dma_start(out=out_d[t * 128:(t + 1) * 128, :], in_=acc[:]), reads=[Bacc], writes=[Bout])
            S.flush(); st.close()
            st = st_moe
        if stage >= 2 and not SPARSE and not S.stopped:
            sh2 = sb("sh2", [128, 1024]); w2s = sb("w2s", [128, 1024]); g2 = sb("g2", [128, 1024]); Bmb = S.buf("mb")
            for i_, dst in enumerate((sh2, w2s, g2)):
                S.dma("sp", lambda e, i_=i_, dst=dst: e.dma_start(out=dst[:], in_=scr_d[i_:i_ + 1, :].to_broadcast([128, 1024])), reads=[Bscr], writes=[Bmb])
            wr = sb("wr", [128, 8, 32]); Bwr = S.buf("wr")
            S.dma("sp", lambda e: e.dma_start(out=wr[:], in_=wr_d.rearrange("(k p) n -> p k n", p=128)), writes=[Bwr])
            prm2 = sb("prm2", [128, 544]); Bprm2 = S.buf("prm2")
            S.dma("sp", lambda e: e.dma_start(out=prm2[:], in_=prm2_d), writes=[Bprm2])
            bdn = sb("bdn", [32, 1024]); Bbdn = S.buf("bdn")
            S.dma("sp", lambda e: e.dma_start(out=bdn[:], in_=bdn_d), writes=[Bbdn])
            h2T = sb("h2T", [128, 8, 1024], BF16); Bh2T = [S.buf(f"h2T{t}") for t in range(8)]
            xb = sb("xb", [128, 1024]); Bxb = S.buf("xb")
            h2f = sb("h2f", [128, 1024]); Bh2f = S.buf("h2f")
            h2Tf = sb("h2Tf", [128, 8, 128]); Bh2Tf = S.buf("h2Tf")
            Gt = sb("Gt", [128, 8, 32]); BG = [S.buf(f"G{t}") for t in range(8)]
            GT = sb("GT", [32, 128]); BGT = S.buf("GT")
            acc = sb("acc", [128, 8, 1024]); Bacc = [S.buf(f"acc{t}") for t in range(8)]
            wu = [sb(f"wu{i}", [128, 8, 2048], BF16) for i in range(2)]; Bwu = [S.buf(f"wu{i}") for i in range(2)]
            wd = sb("wd", [128, 8, 1024], BF16); Bwd = S.buf("wd")
            actT = [sb(f"actT{i}", [128, 8, 512], BF16) for i in range(2)]; BactT = [S.buf(f"actT{i}") for i in range(2)]
            gq = [sb(f"gq{i}", [128, 512]) for i in range(2)]; sg = [sb(f"sg{i}", [128, 512]) for i in range(1)] * 2; lq = [sb(f"lq{i}", [128, 512]) for i in range(1)] * 2
            Bgq = [S.buf(f"gq{i}") for i in range(2)]; Bsg = [S.buf(f"sg{i}") for i in range(1)] * 2; Blq = [S.buf(f"lq{i}") for i in range(1)] * 2
            sm = sb("sm", [128, 64]); Bsm = S.buf("sm")
            ex = sb("ex", [128, 32]); Bex = S.buf("ex")
            wupv = wup_d.rearrange("(e k p) n -> e p k n", e=32, p=128)
            wdnv = wdn_d.rearrange("(e k p) n -> e p k n", e=32, p=128)

            def load_wu(e_):
                for hh in range(2):
                    S.dma("pool", lambda e, e_=e_, hh=hh: e.dma_start(out=wu[e_ % 2][:, :, hh * 1024:(hh + 1) * 1024], in_=wupv[e_, :, :, hh * 1024:(hh + 1) * 1024]), writes=[Bwu[e_ % 2]])

            def load_wd(e_):
                S.dma("pool", lambda e, e_=e_: e.dma_start(out=wd[:], in_=wdnv[e_]), writes=[Bwd])

            rot = {"up": 0, "dn": 0, "q": 0}

            def up_item(e_, tg, ai):
                for jj in range(8):
                    banks = []
                    for j in (jj, jj + 8):
                        bi = rot["up"] % 4
                        rot["up"] += 1
                        pb = PB[bi]; Bpb = BPB[bi]
                        for k in range(8):
                            S.op("pe", lambda e, pb=pb, k=k, j=j, e_=e_, tg=tg: e.matmul(pb[:, :], lhsT=wu[e_ % 2][:, k, j * 128:(j + 1) * 128], rhs=h2T[:, k, tg * 512:(tg + 1) * 512],
                                                                                       start=(k == 0), stop=(k == 7)), reads=[Bwu[e_ % 2]] + Bh2T[tg * 4:(tg + 1) * 4], writes=[Bpb])
                        banks.append((pb, Bpb))
                    qi = rot["q"] % 2
                    rot["q"] += 1
                    (pa, Bpa), (pbb, Bpbb) = banks
                    bg = 32 + e_ * 16 + jj
                    bl = 32 + e_ * 16 + jj + 8
                    S.op("dve", lambda e, pa=pa, qi=qi, bg=bg: e.tensor_scalar(out=gq[qi][:], in0=pa[:, :], scalar1=prm2[:, bg:bg + 1], scalar2=7.0, op0=ALU.add, op1=ALU.min),
                         reads=[Bpa, Bprm2], writes=[Bgq[qi]])
                    S.op("act", lambda e, qi=qi: e.activation(out=sg[qi][:], in_=gq[qi][:], func=AF.Sigmoid, scale=1.702), reads=[Bgq[qi]], writes=[Bsg[qi]])
                    S.op("dve", lambda e, pbb=pbb, qi=qi, bl=bl: e.tensor_scalar(out=lq[qi][:], in0=pbb[:, :], scalar1=prm2[:, bl:bl + 1], scalar2=7.0, op0=ALU.add, op1=ALU.min),
                         reads=[Bpbb, Bprm2], writes=[Blq[qi]])
                    S.op("dve", lambda e, qi=qi: e.tensor_scalar(out=lq[qi][:], in0=lq[qi][:], scalar1=-7.0, scalar2=1.0, op0=ALU.max, op1=ALU.add), reads=[Blq[qi]], writes=[Blq[qi]])
                    S.op("dve", lambda e, qi=qi: e.tensor_tensor(out=gq[qi][:], in0=gq[qi][:], in1=sg[qi][:], op=ALU.mult), reads=[Bgq[qi], Bsg[qi]], writes=[Bgq[qi]])
                    S.op("dve", lambda e, qi=qi, ai=ai, jj=jj: e.tensor_tensor(out=actT[ai][:, jj, :], in0=gq[qi][:], in1=lq[qi][:], op=ALU.mult), reads=[Bgq[qi], Blq[qi]], writes=[BactT[ai]])

            def down_item(e_, tg, ai):
                for tt in range(4):
                    t = tg * 4 + tt
                    for hh in range(2):
                        bi = 4 + rot["dn"] % 2
                        rot["dn"] += 1
                        pb = PB[bi]; Bpb = BPB[bi]
                        for jd in range(8):
                            S.op("pe", lambda e, pb=pb, jd=jd, tt=tt, hh=hh, ai=ai: e.matmul(pb[:, :], lhsT=actT[ai][:, jd, tt * 128:(tt + 1) * 128], rhs=wd[:, jd, hh * 512:(hh + 1) * 512],
                                                                                         start=(jd == 0), stop=(jd == 7)), reads=[BactT[ai], Bwd], writes=[Bpb])
                        if e_ == 0:
                            S.op("dve", lambda e, pb=pb, t=t, hh=hh, e_=e_: e.tensor_scalar(out=acc[:, t, hh * 512:(hh + 1) * 512], in0=pb[:, :], scalar1=Gt[:, t, e_:e_ + 1], scalar2=None, op0=ALU.mult),
                                 reads=[Bpb, BG[t]], writes=[Bacc[t]])
                        else:
                            S.op("dve", lambda e, pb=pb, t=t, hh=hh, e_=e_: e.scalar_tensor_tensor(out=acc[:, t, hh * 512:(hh + 1) * 512], in0=pb[:, :], scalar=Gt[:, t, e_:e_ + 1],
                                                                                                in1=acc[:, t, hh * 512:(hh + 1) * 512], op0=ALU.mult, op1=ALU.add),
                                 reads=[Bpb, BG[t], Bacc[t]], writes=[Bacc[t]])

            for pss in range(2):
                for t in range(8):
                    gt = pss * 8 + t
                    S.dma("sp", lambda e, gt=gt: e.dma_start(out=xb[:], in_=out_d[gt * 128:(gt + 1) * 128, :]), reads=[Bout], writes=[Bxb])
                    S.op("act", lambda e: e.activation(out=h2f[:], in_=xb[:], func=AF.Square, accum_out=sm[:, 11:12]), reads=[Bxb], writes=[Bh2f, Bsm])
                    S.op("act", lambda e: e.activation(out=sm[:, 12:13], in_=sm[:, 11:12], func=AF.Sqrt, scale=1.0 / 1024, bias=EPS), reads=[Bsm], writes=[Bsm])
                    S.op("dve", lambda e: e.reciprocal(out=sm[:, 12:13], in_=sm[:, 12:13]), reads=[Bsm], writes=[Bsm])
                    S.op("dve", lambda e: e.scalar_tensor_tensor(out=h2f[:], in0=xb[:], scalar=sm[:, 12:13], in1=w2s[:], op0=ALU.mult, op1=ALU.mult), reads=[Bxb, Bsm, Bmb, Bh2f], writes=[Bh2f])
                    S.op("dve", lambda e: e.tensor_tensor(out=h2f[:], in0=h2f[:], in1=sh2[:], op=ALU.add), reads=[Bh2f, Bmb], writes=[Bh2f])
                    for b2 in range(2):
                        for kk in range(4):
                            k = b2 * 4 + kk
                            S.op("pe", lambda e, b2=b2, kk=kk, k=k: e.transpose(PB[b2][:, kk * 128:(kk + 1) * 128], h2f[:, k * 128:(k + 1) * 128], cs(C_ID)), reads=[Bh2f, Bcst], writes=[BPB[b2]])
                        S.op("act", lambda e, b2=b2, t=t: e.activation(out=h2T[:, b2 * 4:(b2 + 1) * 4, t * 128:(t + 1) * 128], in_=PB[b2][:, :].rearrange("p (a b) -> p a b", a=4), func=AF.Identity),
                             reads=[BPB[b2]], writes=[Bh2T[t]])
                        S.op("dve", lambda e, b2=b2: e.tensor_copy(out=h2Tf[:, b2 * 4:(b2 + 1) * 4, :], in_=PB[b2][:, :].rearrange("p (a b) -> p a b", a=4)), reads=[BPB[b2]], writes=[Bh2Tf])
                    for k in range(8):
                        S.op("pe", lambda e, k=k: e.matmul(PB[6][:, 0:32], lhsT=h2Tf[:, k, :], rhs=wr[:, k, :], start=(k == 0), stop=(k == 7)), reads=[Bh2Tf, Bwr], writes=[BPB[6]])
                    S.op("dve", lambda e: e.tensor_tensor(out=sm[:, 16:48], in0=PB[6][:, 0:32], in1=prm2[:, 0:32], op=ALU.add), reads=[BPB[6], Bprm2, Bsm], writes=[Bsm])
                    S.op("dve", lambda e: e.max(out=sm[:, 0:8], in_=sm[:, 16:48]), reads=[Bsm], writes=[Bsm])
                    S.op("dve", lambda e: e.tensor_scalar(out=sm[:, 8:9], in0=sm[:, 0:1], scalar1=-1.0, scalar2=None, op0=ALU.mult), reads=[Bsm], writes=[Bsm])
                    S.op("act", lambda e: e.activation(out=ex[:], in_=sm[:, 16:48], func=AF.Exp, bias=sm[:, 8:9], scale=1.0), reads=[Bsm], writes=[Bex])
                    S.op("dve", lambda e: e.scalar_tensor_tensor(out=ex[:], in0=sm[:, 16:48], scalar=sm[:, 3:4], in1=ex[:], op0=ALU.is_ge, op1=ALU.mult), reads=[Bsm, Bex], writes=[Bex])
                    S.op("dve", lambda e: e.reduce_sum(out=sm[:, 9:10], in_=ex[:], axis=AX.X), reads=[Bex, Bsm], writes=[Bsm])
                    S.op("dve", lambda e: e.reciprocal(out=sm[:, 10:11], in_=sm[:, 9:10]), reads=[Bsm], writes=[Bsm])
                    S.op("dve", lambda e, t=t: e.tensor_scalar(out=Gt[:, t, :], in0=ex[:], scalar1=sm[:, 10:11], scalar2=None, op0=ALU.mult), reads=[Bex, Bsm], writes=[BG[t]])
                items = [(e_, tg) for e_ in range(32) for tg in range(2)]
                load_wu(0)
                load_wd(0)
                load_wu(1)
                up_item(0, 0, 0)
                for i in range(1, len(items)):
                    e_, tg = items[i]
                    pe_, ptg = items[i - 1]
                    if tg == 0 and e_ + 1 < 32:
                        load_wu(e_ + 1)
                    up_item(e_, tg, i % 2)
                    down_item(pe_, ptg, (i - 1) % 2)
                    if ptg == 1 and pe_ + 1 < 32:
                        load_wd(pe_ + 1)
                down_item(31, 1, (len(items) - 1) % 2)
                for t in range(8):
                    gt = pss * 8 + t
                    S.op("pe", lambda e, t=t: e.transpose(PB[6][0:32, 128:256], Gt[:, t, :], cs(C_ID)), reads=[BG[t], Bcst], writes=[BPB[6]])
                    S.op("act", lambda e: e.activation(out=GT[:, :], in_=PB[6][0:32, 128:256], func=AF.Identity), reads=[BPB[6]], writes=[BGT])
                    S.dma("sp", lambda e, gt=gt: e.dma_start(out=xb[:], in_=out_d[gt * 128:(gt + 1) * 128, :]), reads=[Bout], writes=[Bxb])
                    for hh in range(2):
                        pb = PB[4 + hh]; Bpb = BPB[4 + hh]
                        S.op("pe", lambda e, pb=pb, hh=hh: e.matmul(pb[:, :], lhsT=GT[0:32, :], rhs=bdn[0:32, hh * 512:(hh + 1) * 512], start=True, stop=True), reads=[BGT, Bbdn], writes=[Bpb])
                        S.op("dve", lambda e, pb=pb, t=t, hh=hh: e.tensor_tensor(out=acc[:, t, hh * 512:(hh + 1) * 512], in0=pb[:, :], in1=acc[:, t, hh * 512:(hh + 1) * 512], op=ALU.add),
                             reads=[Bpb, Bacc[t]], writes=[Bacc[t]])
                    S.op("dve", lambda e, t=t: e.tensor_tensor(out=acc[:, t, :], in0=acc[:, t, :], in1=g2[:], op=ALU.mult), reads=[Bacc[t], Bmb], writes=[Bacc[t]])
                    S.op("dve", lambda e, t=t: e.tensor_tensor(out=acc[:, t, :], in0=acc[:, t, :], in1=xb[:], op=ALU.add), reads=[Bacc[t], Bxb], writes=[Bacc[t]])
                    S.dma("sp", lambda e, gt=gt, t=t: e.dma_start(out=out_d[gt * 128:(gt + 1) * 128, :], in_=acc[:, t, :]), reads=[Bacc[t]], writes=[Bout])

        S.flush(); st.close()
        st = st_root
        if S.stopped:
            S.dma("sp", lambda e: e.dma_start(out=out_d[0:128, 0:NPRM], in_=prm[:, :]), reads=[Bprm], writes=[Bout], force=True)
        S.final_wait("sp", [Bout])
        S.flush()
    return nc


def prep_inputs(inp):
    f = lambda a: np.ascontiguousarray(np.asarray(a, dtype=np.float32))
    x = f(inp["x"]); c = f(inp["c"])
    w_in = f(inp["w_in"][0])
    q_, k_, v_ = w_in[:, 0:512], w_in[:, 512:1024], w_in[:, 1024:1536]
    z_, a_, b_ = w_in[:, 1536:2048], w_in[:, 2048:2052], w_in[:, 2052:2056]
    sq_, sk_, sv_ = w_in[:, 2056:2568], w_in[:, 2568:2696], w_in[:, 2696:2824]
    w_fm = f(np.concatenate([q_, k_, v_, sq_, sk_[:, 0:64], sk_[:, 0:64], sk_[:, 64:128], sk_[:, 64:128]], axis=1))
    w_tm = f(np.concatenate([z_, a_, b_, sv_], axis=1))
    cst = make_consts()
    ii = np.arange(128)
    bd32 = (ii[:, None] // 32 == ii[None, :] // 32)
    m1 = ((ii[:, None] // 32) % 2 == 0) & (ii[None, :] // 32 == ii[:, None] // 32 + 1)
    m2 = (ii[:, None] < 64) & (ii[None, :] >= 64)
    cstm = np.concatenate([bd32, m1.T, m2.T], axis=1).astype(np.float32).astype(ml_dtypes.bfloat16)
    b_ada = f(inp["b_ada"][0])
    n2bc = f(np.broadcast_to(f(inp["norm2_w"][0])[None, :], (128, 1024)))
    shared = {"cst": cst, "w_ada": f(inp["w_ada"][0]), "b_ada": b_ada[None, :], "w_fm": w_fm, "w_tm": w_tm,
              "w_out": f(inp["w_out"][0]), "n2bc": n2bc, "cstm": cstm,
              "w_router": f(inp["w_router"][0]), "b_down": f(inp["b_down"][0]),
              "w_up": f(inp["w_up"][0]).reshape(32 * 1024, 2048), "w_down": f(inp["w_down"][0]).reshape(32 * 1024, 1024)}
    prm2 = np.zeros((128, 544), np.float32)
    prm2[:, 0:32] = f(inp["b_router"][0])[None, :]
    prm2[:, 32:544] = f(inp["b_up"][0]).reshape(32, 16, 128).transpose(2, 0, 1).reshape(128, 512)
    shared["prm2"] = prm2
    if SPARSE:
        prm3 = np.zeros((128, 160), np.float32)
        prm3[:, 0:32] = np.arange(32, dtype=np.float32)[None, :]
        prm3[:, 32:128] = np.arange(96, dtype=np.float32)[None, :]
        prm3[:, 128] = np.arange(128, dtype=np.float32)
        shared["prm3"] = prm3
        shared["b_upg"] = f(f(inp["b_up"][0]).reshape(32, 16, 128).transpose(0, 2, 1).reshape(4096, 16))
        shared["w_upg"] = f(f(inp["w_up"][0]).reshape(32, 8, 128, 2048).transpose(0, 2, 1, 3).reshape(4096, 8, 2048))
        shared["w_dng"] = f(f(inp["w_down"][0]).reshape(32, 8, 128, 1024).transpose(0, 2, 1, 3).reshape(4096, 8, 1024))
        del shared["w_up"], shared["w_down"]
    maps = []
    for core in range(8):
        b, hf = core // 2, core % 2
        prm = np.zeros((128, NPRM), np.float32)
        prm[:, P_FLAG] = float(hf)
        prm[:, P_HALO] = (float(hf) - 1.0) * 30000.0
        prm[:, P_C:P_C + 8] = c[b].reshape(8, 128).T
        prm[:, P_ALOG:P_ALOG + 4] = f(inp["a_log"][0])[None, :]
        prm[:, P_DTB:P_DTB + 4] = f(inp["dt_bias"][0])[None, :]
        prm[:, P_SINK:P_SINK + 8] = f(inp["sinks"][0])[None, :]
        prm[:, P_DNW:P_DNW + 128] = f(inp["dn_norm_w"][0])[None, :]
        prm[0:64, P_QNW] = f(inp["q_norm_w"][0])
        prm[64:128, P_QNW_HI] = f(inp["q_norm_w"][0])
        prm[:, P_KNW] = np.tile(f(inp["k_norm_w"][0]), 2)
        prm[:, P_N1:P_N1 + 8] = f(inp["norm1_w"][0]).reshape(8, 128).T
        prm[:, P_BADA:P_BADA + 48] = b_ada.reshape(48, 128).T
        prm[:, P_CONV:P_CONV + 48] = f(inp["conv_w"][0]).T.reshape(12, 128, 4).transpose(1, 0, 2).reshape(128, 48)
        m = dict(shared)
        m["prm"] = prm
        m["xp"] = f(x[b, 0:2048])
        m["xo"] = f(x[b, hf * 2048:(hf + 1) * 2048])
        maps.append(m)
    return maps


_NC_CACHE = {}


def kernel(**inputs):
    maps = prep_inputs(inputs)
    if "nc" not in _NC_CACHE:
        _NC_CACHE["nc"] = build()
    res = run_bass_kernel_spmd(_NC_CACHE["nc"], maps, core_ids=list(range(8)))
    out = np.zeros((4, 4096, 1024), np.float32)
    for core in range(8):
        b, hf = core // 2, core % 2
        out[b, hf * 2048:(hf + 1) * 2048] = res.results[core]["out"]
    return out
```

```python
import numpy as np
import ml_dtypes
import concourse.bass as bass
import concourse.mybir as mybir
from concourse.bass_utils import run_bass_kernel_spmd
from contextlib import ExitStack

F32 = mybir.dt.float32
BF16 = mybir.dt.bfloat16
I32 = mybir.dt.int32
U32 = mybir.dt.uint32
AF = mybir.ActivationFunctionType
ALU = mybir.AluOpType
AX = mybir.AxisListType

EPS = 1e-6
NEGBIG = -30000.0


class Buf:
    __slots__ = ("name", "w", "rs", "excl")

    def __init__(self, name, excl=False):
        self.name = name
        self.w = None
        self.rs = []
        self.excl = excl


class Stager:
    def __init__(self, S):
        self.S = S
        self.st = {}

    def op(self, k, eng, fn, reads=(), writes=()):
        self.st.setdefault(k, []).append((eng, fn, reads, writes, False))

    def dma(self, k, eng, fn, reads=(), writes=()):
        self.st.setdefault(k, []).append((eng, fn, reads, writes, True))

    def run(self):
        for k in sorted(self.st):
            for eng, fn, r, w, isdma in self.st[k]:
                if isdma:
                    self.S.dma(eng, fn, reads=r, writes=w)
                else:
                    self.S.op(eng, fn, reads=r, writes=w)
        self.st = {}


class Sched:
    ENG = ("pe", "act", "dve", "pool", "sp")

    def __init__(self, nc, stack):
        self.nc = nc
        self.stack = stack
        self.prog = {e: [] for e in self.ENG}
        self.esem = {e: stack.enter_context(nc.semaphore("es_" + e)) for e in self.ENG}
        self.tick = {e: 0 for e in self.ENG}
        self.waited = {e: {} for e in self.ENG}
        self.dsems = {}
        self.nbuf = 0

    def buf(self, name=None, excl=False):
        self.nbuf += 1
        return Buf((name or "b") + str(self.nbuf), excl)

    def _collect(self, eng, reads, writes):
        deps = {}

        def add(d):
            if d is None:
                return
            s, v = d
            if eng == "pe" and s is self.esem["pe"]:
                return
            k = id(s)
            if k not in deps or deps[k][1] < v:
                deps[k] = (s, v)
        for b in reads:
            add(b.w)
            if b.excl:
                for r in b.rs:
                    add(r)
        for b in writes:
            add(b.w)
            for r in b.rs:
                add(r)
        out = []
        wd = self.waited[eng]
        for k, (s, v) in deps.items():
            if wd.get(k, 0) >= v:
                continue
            wd[k] = v
            out.append((s, v))
        return out

    def _commit(self, reads, writes, done):
        for b in writes:
            b.w = done
            b.rs = []
        for b in reads:
            if b.excl:
                b.w = done
                b.rs = []
            elif b not in writes:
                b.rs.append(done)
                if len(b.rs) > 24:
                    m = {}
                    for s, v in b.rs:
                        if id(s) not in m or m[id(s)][1] < v:
                            m[id(s)] = (s, v)
                    b.rs = list(m.values())

    stopped = False
    stop_at = None

    def mark(self, label):
        if self.stop_at is not None and label == self.stop_at:
            self.stopped = True

    def op(self, eng, fn, reads=(), writes=()):
        if self.stopped:
            return
        waits = self._collect(eng, reads, writes)
        self.tick[eng] += 1
        done = (self.esem[eng], self.tick[eng])
        self.prog[eng].append((waits, fn, self.esem[eng], 1))
        self._commit(reads, writes, done)

    def dma(self, eng, fn, reads=(), writes=(), key=None, force=False):
        if self.stopped and not force:
            return
        waits = self._collect(eng, reads, writes)
        kb = key if key is not None else (writes[0] if writes else reads[0])
        if kb not in self.dsems:
            self.dsems[kb] = [self.stack.enter_context(self.nc.semaphore("ds_" + kb.name)), 0]
        d = self.dsems[kb]
        d[1] += 16
        done = (d[0], d[1])
        self.prog[eng].append((waits, fn, d[0], 16))
        self._commit(reads, writes, done)

    def barrier(self):
        allw = [(self.esem[e], self.tick[e]) for e in self.ENG if self.tick[e] > 0]
        allw += [(d[0], d[1]) for d in self.dsems.values()]
        for e in self.ENG:
            wd = self.waited[e]
            waits = []
            for s_, v in allw:
                if s_ is self.esem[e]:
                    continue
                if wd.get(id(s_), 0) >= v:
                    continue
                wd[id(s_)] = v
                waits.append((s_, v))
            self.prog[e].append((waits, None, None, 0))

    def final_wait(self, eng, bufs):
        waits = self._collect(eng, bufs, bufs)
        self.prog[eng].append((waits, None, None, 0))

    def flush(self):
        with self.nc.Block() as block:
            self.emit(block)
        self.prog = {e: [] for e in self.ENG}

    def emit(self, block):
        m = {"pe": block.tensor, "act": block.scalar, "dve": block.vector,
             "pool": block.gpsimd, "sp": block.sync}
        for e in self.ENG:
            plist = self.prog[e]

            def body(engobj, plist=plist):
                for waits, fn, sem, inc in plist:
                    for (s, v) in waits:
                        engobj.wait_ge(s, v)
                    if fn is not None:
                        fn(engobj).then_inc(sem, inc)
            m[e](body)


C_ID, C_UI, C_SL, C_NEG, C_OFFD, C_ONE, C_BD, C_SWB = 0, 128, 256, 384, 512, 640, 768, 896
NCST = 896 + 2 * 8 * 128


def make_consts():
    c = np.zeros((128, NCST), np.float32)
    i = np.arange(128)
    c[:, C_ID:C_ID + 128] = np.eye(128)
    c[:, C_UI:C_UI + 128] = (i[:, None] <= i[None, :])
    c[:, C_SL:C_SL + 128] = (i[:, None] > i[None, :])
    c[:, C_NEG:C_NEG + 128] = np.where(i[:, None] > i[None, :], NEGBIG, 0.0)
    c[:, C_OFFD:C_OFFD + 128] = 1.0 - np.eye(128)
    c[:, C_ONE:C_ONE + 128] = 1.0
    c[:, C_BD:C_BD + 128] = ((i[:, None] // 64) == (i[None, :] // 64))
    s = i[:, None].astype(np.float32)
    q = i[None, :].astype(np.float32)
    for h in range(8):
        slope = 2.0 ** (-(h + 1))
        prev = np.where(s > q, -slope * (q + 128.0 - s), NEGBIG)
        own = np.where(s <= q, -slope * (q - s), NEGBIG)
        c[:, C_SWB + (0 * 8 + h) * 128: C_SWB + (0 * 8 + h) * 128 + 128] = prev
        c[:, C_SWB + (1 * 8 + h) * 128: C_SWB + (1 * 8 + h) * 128 + 128] = own
    return c


P_FLAG, P_HALO, P_C, P_ALOG, P_DTB, P_SINK, P_DNW, P_QNW, P_KNW, P_N1, P_BADA, P_CONV = \
    0, 1, 2, 10, 14, 18, 26, 154, 155, 156, 164, 212
NPRM = 212 + 48 + 1
P_QNW_HI = 212 + 48

NFM = 18 * 128
NTM = 512 + 8 + 128


SPARSE = True
NBLK = 96


def build(stage=9, stop_at=None, dbg=False):
    nc = bass.Bass("TRN2", target_bir_lowering=False)
    dbg_d = nc.dram_tensor("dbg", [128, 4096], F32, kind="ExternalOutput").ap() if dbg else None

    def din(name, shape, dt=F32):
        return nc.dram_tensor(name, shape, dt, kind="ExternalInput").ap()
    xp_d = din("xp", [2048, 1024])
    xo_d = din("xo", [2048, 1024])
    cst_d = din("cst", [128, NCST])
    prm_d = din("prm", [128, NPRM])
    wada_d = din("w_ada", [1024, 6144])
    bada_d = din("b_ada", [1, 6144])
    wfm_d = din("w_fm", [1024, NFM])
    wtm_d = din("w_tm", [1024, NTM])
    wout_d = din("w_out", [1024, 1024])
    n2_d = din("n2bc", [128, 1024])
    cstm_d = din("cstm", [128, 384], BF16)
    wr_d = din("w_router", [1024, 32])
    prm2_d = din("prm2", [128, 544])
    bdn_d = din("b_down", [32, 1024])
    if not SPARSE:
        wup_d = din("w_up", [32 * 1024, 2048])
        wdn_d = din("w_down", [32 * 1024, 1024])
    if SPARSE:
        prm3_d = din("prm3", [128, 160])
        bupg_d = din("b_upg", [4096, 16])
        wupg_d = nc.dram_tensor("w_upg", [4096, 8, 2048], F32, kind="ExternalInput").ap()
        wdng_d = nc.dram_tensor("w_dng", [4096, 8, 1024], F32, kind="ExternalInput").ap()
        wub_d = nc.dram_tensor("wub", [4096, 16384], BF16, kind="Internal").ap()
        wdb_d = nc.dram_tensor("wdb", [4096, 8192], BF16, kind="Internal").ap()
    out_d = nc.dram_tensor("out", [2048, 1024], F32, kind="ExternalOutput").ap()

    with ExitStack() as st:
        S = Sched(nc, st)
        G = Stager(S)
        S.stop_at = stop_at

        def sb(name, shape, dt=F32):
            return st.enter_context(nc.sbuf_tensor("s_" + name, shape, dt))

        def ps(name, shape, dt=F32):
            return st.enter_context(nc.psum_tensor("p_" + name, shape, dt))

        cst = sb("cst", [128, NCST]); Bcst = S.buf("cst")
        cstb = sb("cstb", [128, 896], BF16); Bcstb = S.buf("cstb")
        prm = sb("prm", [128, NPRM]); Bprm = S.buf("prm")

        cstm = sb("cstm", [128, 384], BF16); Bcstm = S.buf("cstm")
        S.dma("sp", lambda e: e.dma_start(out=cstm[:], in_=cstm_d), writes=[Bcstm])

        def cs(off, n=128):
            return cst[:, off:off + n]

        def csb(off, n=128):
            return cstb[:, off:off + n]

        S.dma("sp", lambda e: e.dma_start(out=cst[:], in_=cst_d), writes=[Bcst])
        S.dma("sp", lambda e: e.dma_start(out=prm[:], in_=prm_d), writes=[Bprm])
        S.op("dve", lambda e: e.tensor_copy(out=cstb[:], in_=cst[:, 0:896]), reads=[Bcst], writes=[Bcstb])

        PB = [ps(f"pb{i}", [128, 512]) for i in range(7)]
        BPB = [S.buf(f"pb{i}", excl=True) for i in range(7)]
        PT = ps("ptr", [128, 1024], BF16)
        BPTb = S.buf("ptr", excl=True)
        BPT = [BPTb for i in range(8)]

        cact = sb("cact", [128, 8]); Bcact = S.buf("cact")
        S.op("act", lambda e: e.activation(out=cact[:], in_=prm[:, P_C:P_C + 8], func=AF.Silu), reads=[Bprm], writes=[Bcact])
        scr_d = nc.dram_tensor("scr", [4, 1024], F32, kind="Internal").ap()
        Bscr = S.buf("scr")
        gate1 = sb("gate1", [128, 1024]); Bmodbc = S.buf("modbc")
        modT = sb("modT", [128, 16]); BmodT = S.buf("modT")
        A1 = sb("A1", [128, 8]); B1 = sb("B1", [128, 8]); BA1 = S.buf("A1")
        st_root = st
        st = ExitStack()
        wfm = sb("wfm", [128, 8, NFM], BF16); Bwfm = S.buf("wfm")
        wtm = sb("wtm", [128, 8, NTM], BF16); Bwtm = S.buf("wtm")
        wout = sb("wout", [128, 8, 1024], BF16); Bwout = S.buf("wout")
        for hh in range(2):
            S.dma("pool", lambda e, hh=hh: e.dma_start(out=wfm[:, :, hh * 1152:(hh + 1) * 1152],
                                                     in_=wfm_d.rearrange("(k p) n -> p k n", p=128)[:, :, hh * 1152:(hh + 1) * 1152]), writes=[Bwfm])
        S.dma("pool", lambda e: e.dma_start(out=wtm[:], in_=wtm_d.rearrange("(k p) n -> p k n", p=128)), writes=[Bwtm])
        S.dma("pool", lambda e: e.dma_start(out=wout[:], in_=wout_d.rearrange("(k p) n -> p k n", p=128)), writes=[Bwout])
        st_outer = st
        st = ExitStack()
        n2bc = sb("n2bc", [128, 1024]); Bn2 = S.buf("n2")
        S.dma("sp", lambda e: e.dma_start(out=n2bc[:], in_=n2_d), writes=[Bn2])
        wa = [sb(f"wa{i}", [128, 8, 512]) for i in range(2)]
        Bwa = [S.buf(f"wa{i}") for i in range(2)]
        wav = wada_d.rearrange("(k p) n -> p k n", p=128)
        modrow = sb("modrow", [1, 4096]); Bmodrow = S.buf("modrow")
        badarow = sb("badarow", [1, 4096]); Bbadarow = S.buf("badarow")
        S.dma("sp", lambda e: e.dma_start(out=badarow[:], in_=bada_d[:, 2048:6144]), writes=[Bbadarow])
        for grp in range(12):
            w_ = wa[grp % 2]; Bw_ = Bwa[grp % 2]
            S.dma("sp", lambda e, w_=w_, grp=grp: e.dma_start(out=w_[:], in_=wav[:, :, grp * 512:(grp + 1) * 512]), writes=[Bw_])
            if grp < 4:
                for j in range(4):
                    col = grp * 4 + j
                    for k in range(8):
                        S.op("pe", lambda e, w_=w_, j=j, k=k, col=col: e.matmul(
                            PB[0][:, col:col + 1], lhsT=w_[:, k, j * 128:(j + 1) * 128], rhs=cact[:, k:k + 1],
                            start=(k == 0), stop=(k == 7)), reads=[Bw_, Bcact], writes=[BPB[0]])
                if grp == 3:
                    S.op("dve", lambda e: e.tensor_tensor(out=modT[:], in0=PB[0][:, 0:16], in1=prm[:, P_BADA:P_BADA + 16], op=ALU.add),
                         reads=[BPB[0], Bprm], writes=[BmodT])
                    S.op("dve", lambda e: e.scalar_tensor_tensor(out=A1[:], in0=modT[:, 8:16], scalar=1.0, in1=prm[:, P_N1:P_N1 + 8],
                                                               op0=ALU.add, op1=ALU.mult), reads=[BmodT, Bprm], writes=[BA1])
                    S.op("dve", lambda e: e.tensor_copy(out=B1[:], in_=modT[:, 0:8]), reads=[BmodT], writes=[BA1])
            else:
                g2 = grp - 4
                pb = PB[1 + (g2 % 2)]; Bpb = BPB[1 + (g2 % 2)]
                for k in range(8):
                    S.op("pe", lambda e, w_=w_, k=k, pb=pb: e.matmul(pb[0:1, :], lhsT=cact[:, k:k + 1], rhs=w_[:, k, :],
                                                                     start=(k == 0), stop=(k == 7)), reads=[Bw_, Bcact], writes=[Bpb])
                S.op("dve", lambda e, pb=pb, g2=g2: e.tensor_tensor(out=modrow[0:1, g2 * 512:(g2 + 1) * 512], in0=pb[0:1, :],
                                                                    in1=badarow[0:1, g2 * 512:(g2 + 1) * 512], op=ALU.add),
                     reads=[Bpb, Bbadarow], writes=[Bmodrow])
        w2row = sb("w2row", [1, 1024]); Bw2row = S.buf("w2row")
        S.op("dve", lambda e: e.scalar_tensor_tensor(out=w2row[0:1, :], in0=modrow[0:1, 2048:3072], scalar=1.0, in1=n2bc[0:1, :], op0=ALU.add, op1=ALU.mult),
             reads=[Bmodrow, Bn2], writes=[Bw2row])
        S.dma("sp", lambda e: e.dma_start(out=scr_d[0:1, :], in_=modrow[0:1, 1024:2048]), reads=[Bmodrow], writes=[Bscr])
        S.dma("sp", lambda e: e.dma_start(out=scr_d[1:2, :], in_=w2row[0:1, :]), reads=[Bw2row], writes=[Bscr])
        S.dma("sp", lambda e: e.dma_start(out=scr_d[2:3, :], in_=modrow[0:1, 3072:4096]), reads=[Bmodrow], writes=[Bscr])
        for v, dst in enumerate((gate1,)):
            for hh in range(2):
                pb = PB[1 + hh]; Bpb = BPB[1 + hh]
                S.op("pe", lambda e, pb=pb, v=v, hh=hh: e.matmul(pb[:, :], lhsT=cst[0:1, C_ONE:C_ONE + 128],
                                                               rhs=modrow[0:1, v * 1024 + hh * 512: v * 1024 + (hh + 1) * 512],
                                                               start=True, stop=True), reads=[Bmodrow, Bcst], writes=[Bpb])
                if True:
                    S.op("act", lambda e, pb=pb, hh=hh, dst=dst: e.activation(out=dst[:, hh * 512:(hh + 1) * 512], in_=pb[:, :], func=AF.Identity),
                         reads=[Bpb], writes=[Bmodbc])

        S.mark("adaln")
        S.barrier()
        S.flush(); st.close()
        st = st_outer
        nA = sb("nA", [128, 4]); esink = sb("esink", [128, 8]); Bder = S.buf("der")
        S.op("act", lambda e: e.activation(out=nA[:], in_=prm[:, P_ALOG:P_ALOG + 4], func=AF.Exp), reads=[Bprm], writes=[Bder])
        S.op("dve", lambda e: e.tensor_scalar(out=nA[:], in0=nA[:], scalar1=-1.0, scalar2=None, op0=ALU.mult), reads=[Bder], writes=[Bder])
        S.op("act", lambda e: e.activation(out=esink[:], in_=prm[:, P_SINK:P_SINK + 8], func=AF.Exp), reads=[Bprm], writes=[Bder])

        S.mark("m0")
        xt = [sb(f"xt{i}", [128, 1024]) for i in range(4)]; Bxt = [S.buf(f"xt{i}") for i in range(4)]
        stat = [sb(f"stat{i}", [128, 2]) for i in range(2)]; Bstat = [S.buf(f"stat{i}") for i in range(2)]
        xn = [sb(f"xn{i}", [128, 1024], BF16) for i in range(2)]; Bxn = [S.buf(f"xn{i}") for i in range(2)]
        hT = sb("hT", [128, 8, 512], BF16); BhT = S.buf("hT")
        Ub = [sb(f"Ub{i}", [128, 516]) for i in range(2)]; BUb = [S.buf(f"Ub{i}") for i in range(2)]
        halo = sb("halo", [128, 12, 4]); Bhalo = [S.buf(f"halo{c}") for c in range(12)]
        ctmp = [sb(f"ctmp{i}", [128, 512]) for i in range(2)]; Bctmp = [S.buf(f"ctmp{i}") for i in range(2)]
        cs2 = [sb(f"csil{i}", [128, 512]) for i in range(2)]; Bcs2 = [S.buf(f"csil{i}") for i in range(2)]
        ctmp2 = None; Bctmp2 = None
        sqb2 = [sb(f"sqb{i}", [128, 512], BF16) for i in range(2)]; Bsqb2 = [S.buf(f"sqb{i}") for i in range(2)]
        rst = sb("rst", [128, 512]); Brst = S.buf("rst")
        fm = sb("fm", [128, 12, 512], BF16); Bfm = [S.buf(f"fm{c}") for c in range(12)]
        swq = sb("swq", [128, 8, 512], BF16); Bswq = [S.buf(f"swq{c}") for c in range(4)]
        swk = sb("swk", [128, 2, 640], BF16); Bswk = [S.buf(f"swk{c}") for c in range(2)]
        vv = sb("vv", [128, 5, 2, 128], BF16); Bvv = [S.buf(f"vv{t}") for t in range(5)]
        siluz = sb("siluz", [128, 4, 512], BF16); Bsz = [S.buf(f"sz{t}") for t in range(4)]
        tmf = sb("tmf", [128, 4, 32]); Btmf = [S.buf(f"tmf{t}") for t in range(4)]
        S32 = sb("S32", [128, 4, 128]); Sbf = sb("Sbf", [128, 4, 128], BF16); BS = [S.buf(f"S{h}") for h in range(4)]
        mixT = sb("mixT", [128, 8, 512], BF16); Bmix = [S.buf(f"mix{t}") for t in range(4)]
        Kdec = sb("Kdec", [128, 2, 4, 128], BF16); Vtm = sb("Vtm", [128, 2, 4, 128], BF16)
        Aqk = sb("Aqk", [128, 2, 4, 128], BF16); Minv = sb("Minv", [128, 2, 4, 128], BF16)
        Bdn = [[S.buf(f"dn{t}_{h}") for h in range(4)] for t in range(2)]
        Bfull = sb("Bfull", [128, 4, 2, 128], BF16); BBf = [S.buf(f"Bf{h}") for h in range(4)]
        WT = sb("WT", [128, 4, 2, 128], BF16); BWT = [S.buf(f"WT{h}") for h in range(4)]
        T1s = sb("T1s", [128, 4, 128], BF16); BT1 = [S.buf(f"T1{h}") for h in range(4)]
        D2T = sb("D2T", [128, 4, 128], BF16); BD2T = [S.buf(f"D2T{h}") for h in range(4)]
        lg = sb("lg", [128, 4, 128]); Blg = [S.buf(f"lg{h}") for h in range(4)]
        DT = sb("DT", [128, 4, 128]); BDT = [S.buf(f"DT{h}") for h in range(4)]
        ZPR = sb("ZPR", [128, 4, 3, 128], BF16); BZPR = [S.buf(f"ZPR{h}") for h in range(4)]
        PTt = sb("PTt", [128, 4, 128], BF16); BPTt = [S.buf(f"PTt{h}") for h in range(4)]
        Rm = sb("Rm", [128, 4, 128], BF16); BRm = [S.buf(f"Rm{h}") for h in range(4)]
        vnew = sb("vnew", [128, 4, 128], BF16); Bvn = [S.buf(f"vn{h}") for h in range(4)]
        QSs = sb("QSs", [128, 4, 128]); BQSs = [S.buf(f"QSs{h}") for h in range(4)]
        ot = sb("ot", [128, 4, 128]); Bot = [S.buf(f"ot{h}") for h in range(4)]
        om = sb("om", [128, 4, 128], BF16); Bom = [S.buf(f"om{h}") for h in range(4)]
        ost = sb("ost", [128, 4, 2]); Bost = [S.buf(f"ost{h}") for h in range(4)]
        sc = [sb(f"sc{i}", [128, 512]) for i in range(2)]; Bsc = [S.buf(f"sc{i}") for i in range(2)]
        pTt = sb("pTt", [128, 2, 512], BF16); BpT = [S.buf(f"pT{i}") for i in range(2)]
        den = sb("den", [128, 512]); Bden = S.buf("den")
        x1t = [sb(f"x1t{i}", [128, 1024]) for i in range(1)] * 2; Bx1t = [S.buf(f"x1t{i}") for i in range(1)] * 2
        Bout = S.buf("out")
        BP4 = [BPB[4] for h in range(4)]

        S.op("pool", lambda e: e.memset(halo[:], 0.0), writes=Bhalo)
        S.op("pool", lambda e: e.memset(S32[:], 0.0), writes=BS)
        S.op("pool", lambda e: e.memset(Sbf[:], 0.0), writes=BS)
        S.op("pool", lambda e: e.memset(ZPR[:], 0.0), writes=BZPR)
        S.op("pool", lambda e: e.memset(swk[:], 0.0), writes=Bswk)
        S.op("pool", lambda e: e.memset(vv[:], 0.0), writes=Bvv)

        S.mark("m1")
        Bwcv = S.buf("wcv")
        Bo2 = []
        conv_q = []
        if SPARSE and stage >= 2:
            for c in range(64):
                conv_q.append(lambda e, c=c: e.dma_start(out=wub_d[c * 64:(c + 1) * 64, :].rearrange("r (k n) -> r k n", k=8), in_=wupg_d[c * 64:(c + 1) * 64, :, :]))
                conv_q.append(lambda e, c=c: e.dma_start(out=wdb_d[c * 64:(c + 1) * 64, :].rearrange("r (k n) -> r k n", k=8), in_=wdng_d[c * 64:(c + 1) * 64, :, :]))

        def conv_step(n=1, stage=None):
            for _ in range(n):
                if conv_q:
                    if stage is None:
                        S.dma("pool", conv_q.pop(0), writes=[Bwcv])
                    else:
                        G.dma(stage, "pool", conv_q.pop(0), writes=[Bwcv])
        ident_b = csb(C_ID)
        ones_b = csb(C_ONE)
        bd_b = csb(C_BD)
        rr = {"fmps": 0, "eng": 0}

        def evac_eng():
            rr["eng"] += 1
            return "act" if rr["eng"] % 2 else "dve"

        def supertile(phase, sti):
            own = (phase == 1)
            xsrc = xo_d if own else xp_d
            for tl in range(4):
                gt = sti * 4 + tl
                i2 = gt % 2
                S.dma("sp", lambda e, gt=gt, tl=tl: e.dma_start(out=xt[tl][:], in_=xsrc[gt * 128:(gt + 1) * 128, :]), writes=[Bxt[tl]])
                S.mark("m2")
                S.op("act", lambda e, i2=i2, tl=tl: e.activation(out=xn[i2][:], in_=xt[tl][:], func=AF.Square, accum_out=stat[i2][:, 0:1]),
                     reads=[Bxt[tl]], writes=[Bxn[i2], Bstat[i2]])
                S.mark("a1")
                S.op("act", lambda e, i2=i2: e.activation(out=stat[i2][:, 1:2], in_=stat[i2][:, 0:1], func=AF.Sqrt, scale=1.0 / 1024, bias=EPS),
                     reads=[Bstat[i2]], writes=[Bstat[i2]])
                S.op("dve", lambda e, i2=i2: e.reciprocal(out=stat[i2][:, 1:2], in_=stat[i2][:, 1:2]), reads=[Bstat[i2]], writes=[Bstat[i2]])
                S.op("dve", lambda e, i2=i2, tl=tl: e.tensor_scalar(out=xn[i2][:], in0=xt[tl][:], scalar1=stat[i2][:, 1:2], scalar2=None, op0=ALU.mult),
                     reads=[Bxt[tl], Bstat[i2]], writes=[Bxn[i2]])
                S.mark("a2")
                for k in range(8):
                    S.op("pe", lambda e, i2=i2, k=k: e.transpose(PT[:, k * 128:(k + 1) * 128], xn[i2][:, k * 128:(k + 1) * 128], ident_b),
                         reads=[Bxn[i2], Bcstb], writes=[BPT[k]])
                    S.mark("a3")
                    eng = evac_eng()
                    if eng == "act":
                        S.op("act", lambda e, k=k, tl=tl: e.activation(out=hT[:, k, tl * 128:(tl + 1) * 128], in_=PT[:, k * 128:(k + 1) * 128],
                                                                      func=AF.Identity, scale=A1[:, k:k + 1], bias=B1[:, k:k + 1]),
                             reads=[BPT[k], BA1], writes=[BhT])
                        S.mark(f"ea{gt}_{k}")
                    else:
                        S.op("dve", lambda e, k=k, tl=tl: e.tensor_scalar(out=hT[:, k, tl * 128:(tl + 1) * 128], in0=PT[:, k * 128:(k + 1) * 128],
                                                                         scalar1=A1[:, k:k + 1], scalar2=B1[:, k:k + 1], op0=ALU.mult, op1=ALU.add),
                             reads=[BPT[k], BA1], writes=[BhT])
                        S.mark(f"ed{gt}_{k}")
            S.mark(f"p{phase}s{sti}a")
            chunks = list(range(18)) if own else ((list(range(0, 12)) if sti == 3 else list(range(4, 12))) + [16, 17])
            for ci, c in enumerate(chunks):
                SH = 2 * ci
                ST = 2 * ci + 3
                conv_step(1, SH)
                cs_ = cs2[ci % 2]; Bcs = Bcs2[ci % 2]; sqb = sqb2[ci % 2]; Bsqb = Bsqb2[ci % 2]
                bi = rr["fmps"] % 2
                rr["fmps"] += 1
                pb = PB[bi]; Bpb = BPB[bi]
                for k in range(8):
                    G.op(SH, "pe", lambda e, cs_=cs_, sqb=sqb, pb=pb, c=c, k=k: e.matmul(pb[:, :], lhsT=wfm[:, k, c * 128:(c + 1) * 128], rhs=hT[:, k, :],
                                                                   start=(k == 0), stop=(k == 7)), reads=[Bwfm, BhT], writes=[Bpb])
                if c < 12:
                    ci = c % 2
                    U_ = Ub[ci]; BU_ = BUb[ci]
                    G.op(SH, "pool", lambda e, cs_=cs_, sqb=sqb, c=c, U_=U_: e.tensor_copy(out=U_[:, 1:4], in_=halo[:, c, 0:3]), reads=[Bhalo[c]], writes=[BU_])
                    G.op(SH, "act", lambda e, cs_=cs_, sqb=sqb, pb=pb, U_=U_: e.activation(out=U_[:, 4:516], in_=pb[:, :], func=AF.Identity), reads=[Bpb], writes=[BU_])
                    G.op(SH, "pool", lambda e, cs_=cs_, sqb=sqb, c=c, U_=U_: e.tensor_copy(out=halo[:, c, 0:3], in_=U_[:, 513:516]), reads=[BU_], writes=[Bhalo[c]])
                    ce = "dve"
                    cw = P_CONV + c * 4
                    G.op(SH, ce, lambda e, cs_=cs_, sqb=sqb, U_=U_, ci=ci, cw=cw: e.tensor_scalar(out=ctmp[ci][:], in0=U_[:, 1:513], scalar1=prm[:, cw:cw + 1], scalar2=None, op0=ALU.mult),
                         reads=[BU_, Bprm], writes=[Bctmp[ci]])
                    for j in range(1, 4):
                        if ce == "pool":
                            G.op(SH, ce, lambda e, cs_=cs_, sqb=sqb, U_=U_, cw=cw, j=j: e.tensor_scalar(out=ctmp2[:], in0=U_[:, 1 + j:513 + j], scalar1=prm[:, cw + j:cw + j + 1], scalar2=None, op0=ALU.mult),
                                 reads=[BU_, Bprm], writes=[Bctmp2])
                            G.op(SH, ce, lambda e, cs_=cs_, sqb=sqb, ci=ci: e.tensor_tensor(out=ctmp[ci][:], in0=ctmp[ci][:], in1=ctmp2[:], op=ALU.add), reads=[Bctmp[ci], Bctmp2], writes=[Bctmp[ci]])
                            continue
                        G.op(SH, ce, lambda e, cs_=cs_, sqb=sqb, U_=U_, ci=ci, cw=cw, j=j: e.scalar_tensor_tensor(out=ctmp[ci][:], in0=U_[:, 1 + j:513 + j], scalar=prm[:, cw + j:cw + j + 1],
                                                                                         in1=ctmp[ci][:], op0=ALU.mult, op1=ALU.add),
                             reads=[BU_, Bprm, Bctmp[ci]], writes=[Bctmp[ci]])
                    if c >= 8:
                        G.op(SH, "act", lambda e, cs_=cs_, sqb=sqb, c=c, ci=ci: e.activation(out=fm[:, c, :], in_=ctmp[ci][:], func=AF.Silu), reads=[Bctmp[ci]], writes=[Bfm[c]])
                    else:
                        G.op(SH, "act", lambda e, cs_=cs_, sqb=sqb, ci=ci: e.activation(out=cs_[:], in_=ctmp[ci][:], func=AF.Silu), reads=[Bctmp[ci]], writes=[Bcs])
                        G.op(SH, "pool", lambda e, cs_=cs_, sqb=sqb: e.tensor_tensor(out=sqb[:], in0=cs_[:], in1=cs_[:], op=ALU.mult), reads=[Bcs], writes=[Bsqb])
                        G.op(ST, "pe", lambda e, cs_=cs_, sqb=sqb: e.matmul(PB[2][:, :], lhsT=ones_b, rhs=sqb[:], start=True, stop=True), reads=[Bsqb, Bcstb], writes=[BPB[2]])
                        G.op(ST, "act", lambda e, cs_=cs_, sqb=sqb: e.activation(out=rst[:], in_=PB[2][:, :], func=AF.Sqrt, bias=EPS, scale=1.0), reads=[BPB[2]], writes=[Brst])
                        G.op(ST, "dve", lambda e, cs_=cs_, sqb=sqb: e.reciprocal(out=rst[:], in_=rst[:]), reads=[Brst], writes=[Brst])
                        qs = (128.0 ** -0.5) if c < 4 else 1.0
                        G.op(ST, "dve", lambda e, cs_=cs_, sqb=sqb, c=c, qs=qs: e.scalar_tensor_tensor(out=fm[:, c, :], in0=cs_[:], scalar=qs, in1=rst[:], op0=ALU.mult, op1=ALU.mult),
                             reads=[Bcs, Brst], writes=[Bfm[c]])
                else:
                    G.op(SH, "act", lambda e, cs_=cs_, sqb=sqb, pb=pb: e.activation(out=cs_[:], in_=pb[:, :], func=AF.Identity), reads=[Bpb], writes=[Bcs])
                    G.op(SH, "pool", lambda e, cs_=cs_, sqb=sqb: e.tensor_tensor(out=sqb[:], in0=cs_[:], in1=cs_[:], op=ALU.mult), reads=[Bcs], writes=[Bsqb])
                    G.op(ST, "pe", lambda e, cs_=cs_, sqb=sqb: e.matmul(PB[2][:, :], lhsT=bd_b, rhs=sqb[:], start=True, stop=True), reads=[Bsqb, Bcstb], writes=[BPB[2]])
                    G.op(ST, "act", lambda e, cs_=cs_, sqb=sqb: e.activation(out=rst[:], in_=PB[2][:, :], func=AF.Sqrt, bias=EPS, scale=1.0 / 64), reads=[BPB[2]], writes=[Brst])
                    G.op(ST, "dve", lambda e, cs_=cs_, sqb=sqb: e.reciprocal(out=rst[:], in_=rst[:]), reads=[Brst], writes=[Brst])
                    if c < 16:
                        for par in range(2):
                            G.op(ST, "dve", lambda e, cs_=cs_, sqb=sqb, c=c, par=par: e.scalar_tensor_tensor(out=swq[:, 2 * (c - 12) + par, :], in0=cs_[:], scalar=prm[:, (P_QNW if par == 0 else P_QNW_HI):(P_QNW if par == 0 else P_QNW_HI) + 1], in1=rst[:],
                                                                                      op0=ALU.mult, op1=ALU.mult), reads=[Bcs, Brst, Bprm], writes=[Bswq[c - 12]])
                    else:
                        j = c - 16
                        G.op(ST, "pool", lambda e, cs_=cs_, sqb=sqb, j=j: e.tensor_copy(out=swk[:, j, 0:128], in_=swk[:, j, 512:640]), reads=[Bswk[j]], writes=[Bswk[j]])
                        G.op(ST, "dve", lambda e, cs_=cs_, sqb=sqb, j=j: e.scalar_tensor_tensor(out=swk[:, j, 128:640], in0=cs_[:], scalar=prm[:, P_KNW:P_KNW + 1], in1=rst[:],
                                                                         op0=ALU.mult, op1=ALU.mult), reads=[Bcs, Brst, Bprm], writes=[Bswk[j]])
            G.run()
            S.mark(f"p{phase}s{sti}b")
            S.op("pool", lambda e: e.tensor_copy(out=vv[:, 0, :, :], in_=vv[:, 4, :, :]), reads=[Bvv[4]], writes=[Bvv[0]])
            for tl in range(4):
                tsl = slice(tl * 128, (tl + 1) * 128)
                if own:
                    for k in range(8):
                        S.op("pe", lambda e, k=k, tsl=tsl: e.matmul(PB[3][:, :], lhsT=hT[:, k, tsl], rhs=wtm[:, k, 0:512], start=(k == 0), stop=(k == 7)),
                             reads=[BhT, Bwtm], writes=[BPB[3]])
                    S.op("act", lambda e, tl=tl: e.activation(out=siluz[:, tl, :], in_=PB[3][:, :], func=AF.Silu), reads=[BPB[3]], writes=[Bsz[tl]])
                for k in range(8):
                    S.op("pe", lambda e, k=k, tsl=tsl: e.matmul(PB[4][:, 0:136], lhsT=hT[:, k, tsl], rhs=wtm[:, k, 512:648], start=(k == 0), stop=(k == 7)),
                         reads=[BhT, Bwtm], writes=[BPB[4]])
                T = tmf[:, tl, :]
                Bt = Btmf[tl]
                S.op("dve", lambda e, T=T: e.tensor_tensor(out=T[:, 28:32], in0=PB[4][:, 0:4], in1=prm[:, P_DTB:P_DTB + 4], op=ALU.add), reads=[BPB[4], Bprm], writes=[Bt])
                S.op("act", lambda e, T=T: e.activation(out=T[:, 28:32], in_=T[:, 28:32], func=AF.Exp), reads=[Bt], writes=[Bt])
                S.op("act", lambda e, T=T: e.activation(out=T[:, 28:32], in_=T[:, 28:32], func=AF.Ln, bias=1.0, scale=1.0), reads=[Bt], writes=[Bt])
                S.op("dve", lambda e, T=T: e.tensor_tensor(out=T[:, 0:4], in0=T[:, 28:32], in1=nA[:], op=ALU.mult), reads=[Bt, Bder], writes=[Bt])
                S.op("act", lambda e, T=T: e.activation(out=T[:, 4:8], in_=PB[4][:, 4:8], func=AF.Sigmoid), reads=[BPB[4]], writes=[Bt])
                S.op("dve", lambda e, T=T: e.tensor_scalar(out=T[:, 24:28], in0=T[:, 4:8], scalar1=-1.0, scalar2=None, op0=ALU.mult), reads=[Bt], writes=[Bt])
                for j in range(2):
                    for d in range(2):
                        S.op("act" if d == 0 else "dve",
                             (lambda e, tl=tl, j=j, d=d: e.activation(out=vv[:, 1 + tl, j, d * 64:(d + 1) * 64], in_=PB[4][:, 8 + j * 64: 8 + (j + 1) * 64], func=AF.Identity))
                             if d == 0 else
                             (lambda e, tl=tl, j=j, d=d: e.tensor_copy(out=vv[:, 1 + tl, j, d * 64:(d + 1) * 64], in_=PB[4][:, 8 + j * 64: 8 + (j + 1) * 64])),
                             reads=[BPB[4]], writes=[Bvv[1 + tl]])
                S.op("pe", lambda e, T=T: e.matmul(PB[5][:, 0:4], lhsT=cs(C_UI), rhs=T[:, 0:4], start=True, stop=True), reads=[Bt, Bcst], writes=[BPB[5]])
                S.op("pe", lambda e, T=T: e.matmul(PB[5][:, 4:8], lhsT=cs(C_ONE), rhs=T[:, 0:4], start=True, stop=True), reads=[Bt, Bcst], writes=[BPB[5]])
                S.op("act", lambda e, T=T: e.activation(out=T[:, 8:12], in_=PB[5][:, 0:4], func=AF.Exp), reads=[BPB[5]], writes=[Bt])
                S.op("dve", lambda e, T=T: e.tensor_scalar(out=T[:, 12:16], in0=T[:, 8:12], scalar1=-1.0, scalar2=None, op0=ALU.mult), reads=[Bt], writes=[Bt])
                S.op("act", lambda e, T=T: e.activation(out=T[:, 28:32], in_=PB[5][:, 0:4], func=AF.Identity), reads=[BPB[5]], writes=[Bt])
                S.op("dve", lambda e, T=T: e.tensor_tensor(out=T[:, 28:32], in0=PB[5][:, 4:8], in1=T[:, 28:32], op=ALU.subtract), reads=[BPB[5], Bt], writes=[Bt])
                S.op("act", lambda e, T=T: e.activation(out=T[:, 16:20], in_=T[:, 28:32], func=AF.Exp), reads=[Bt], writes=[Bt])
                S.op("act", lambda e, T=T: e.activation(out=T[:, 20:24], in_=PB[5][:, 4:8], func=AF.Exp), reads=[BPB[5]], writes=[Bt])
            S.mark(f"p{phase}s{sti}b2")
            if dbg and phase == 0 and sti == 0:
                Bdbg = S.buf("dbg")
                S.dma("pool", lambda e: e.dma_start(out=dbg_d[:, 0:1536].rearrange("p (c t) -> p c t", c=12), in_=fm[:, :, 0:128]), reads=Bfm, writes=[Bdbg])
                S.dma("sp", lambda e: e.dma_start(out=dbg_d[:, 1536:1568], in_=tmf[:, 0, :]), reads=Btmf, writes=[Bdbg])
                S.dma("sp", lambda e: e.dma_start(out=dbg_d[:, 1600:1608], in_=A1[:, :]), reads=[BA1], writes=[Bdbg])
                S.dma("sp", lambda e: e.dma_start(out=dbg_d[:, 1608:1616], in_=B1[:, :]), reads=[BA1], writes=[Bdbg])
                S.dma("pool", lambda e: e.dma_start(out=dbg_d[:, 2048:3072].rearrange("p (c t) -> p c t", c=8), in_=hT[:, :, 0:128]), reads=[BhT], writes=[Bdbg])
            def dn_pre(tl):
                tsl = slice(tl * 128, (tl + 1) * 128)
                tb = tl % 2
                T = tmf[:, tl, :]
                Bt = Btmf[tl]
                for h in range(4):
                    Bd = Bdn[tb][h]
                    qT = fm[:, h, tsl]; kT = fm[:, 4 + h, tsl]; vT = fm[:, 8 + h, tsl]
                    Bq, Bk, Bv = Bfm[h], Bfm[4 + h], Bfm[8 + h]
                    G.op(0, "pe", lambda e, kT=kT, h=h: e.transpose(PT[:, h * 128:(h + 1) * 128], kT, ident_b), reads=[Bk, Bcstb], writes=[BPT[h]])
                    G.op(1, "act", lambda e, tl=tl, tb=tb, h=h, T=T: e.activation(out=Kdec[:, tb, h, :], in_=PT[:, h * 128:(h + 1) * 128], func=AF.Identity, scale=T[:, 16 + h:17 + h]),
                         reads=[BPT[h], Bt], writes=[Bd])
                    G.op(0, "pe", lambda e, vT=vT, h=h: e.transpose(PT[:, (4 + h) * 128:(5 + h) * 128], vT, ident_b), reads=[Bv, Bcstb], writes=[BPT[4 + h]])
                    G.op(1, "dve", lambda e, tl=tl, tb=tb, h=h: e.tensor_copy(out=Vtm[:, tb, h, :], in_=PT[:, (4 + h) * 128:(5 + h) * 128]), reads=[BPT[4 + h]], writes=[Bd])
                    pb = PB[h]; Bpb = BPB[h]
                    G.op(2, "pe", lambda e, pb=pb, kT=kT: e.matmul(pb[:, 0:128], lhsT=kT, rhs=kT, start=True, stop=True), reads=[Bk], writes=[Bpb])
                    if own:
                        G.op(2, "pe", lambda e, pb=pb, kT=kT, qT=qT: e.matmul(pb[:, 128:256], lhsT=kT, rhs=qT, start=True, stop=True), reads=[Bk, Bq], writes=[Bpb])
                    G.op(2, "pool", lambda e, h=h, T=T: e.tensor_scalar(out=lg[:, h, :], in0=cs(C_SL), scalar1=T[:, h:h + 1], scalar2=None, op0=ALU.mult),
                         reads=[Bcst, Bt], writes=[Blg[h]])
                    G.op(3, "pe", lambda e, pb=pb, h=h: e.matmul(pb[:, 256:384], lhsT=lg[:, h, :], rhs=cs(C_UI), start=True, stop=False), reads=[Blg[h], Bcst], writes=[Bpb])
                    G.op(3, "pe", lambda e, pb=pb: e.matmul(pb[:, 256:384], lhsT=cs(C_ID), rhs=cs(C_NEG), start=False, stop=True), reads=[Bcst], writes=[Bpb])
                    G.op(4, "act", lambda e, pb=pb, h=h: e.activation(out=DT[:, h, :], in_=pb[:, 256:384], func=AF.Exp), reads=[Bpb], writes=[BDT[h]])
                    if own:
                        G.op(5, "dve", lambda e, pb=pb, h=h, tl=tl, tb=tb: e.tensor_tensor(out=Aqk[:, tb, h, :], in0=pb[:, 128:256], in1=DT[:, h, :], op=ALU.mult),
                             reads=[Bpb, BDT[h]], writes=[Bd])
                    G.op(5, "dve", lambda e, pb=pb, h=h, T=T: e.scalar_tensor_tensor(out=lg[:, h, :], in0=pb[:, 0:128], scalar=T[:, 24 + h:25 + h], in1=DT[:, h, :],
                                                                                 op0=ALU.mult, op1=ALU.mult), reads=[Bpb, Bt, BDT[h], Blg[h]], writes=[Blg[h]])
                    G.op(6, "pool", lambda e, h=h: e.tensor_tensor(out=Bfull[:, h, 0, :], in0=lg[:, h, :], in1=cs(C_OFFD), op=ALU.mult), reads=[Blg[h], Bcst], writes=[BBf[h]])
                    G.op(7, "pe", lambda e, h=h: e.transpose(PT[:, h * 128:(h + 1) * 128], Bfull[:, h, 0, :], ident_b), reads=[BBf[h], Bcstb], writes=[BPT[h]])
                    G.op(8, "act", lambda e, h=h: e.activation(out=Bfull[:, h, 1, :], in_=PT[:, h * 128:(h + 1) * 128], func=AF.Identity), reads=[BPT[h]], writes=[BBf[h]])
                    G.op(9, "pool", lambda e, h=h: e.tensor_tensor(out=ZPR[:, h, 1, :], in0=Bfull[:, h, 0, :], in1=cstm[:, 0:128], op=ALU.mult), reads=[BBf[h], Bcstm], writes=[BZPR[h]])
                    G.op(9, "pool", lambda e, h=h: e.tensor_copy(out=ZPR[:, h, 2, :], in_=cs(C_ID)), reads=[Bcst], writes=[BZPR[h]])
                    G.op(9, "pool", lambda e, h=h: e.tensor_tensor(out=PTt[:, h, :], in0=Bfull[:, h, 1, :], in1=cstm[:, 0:128], op=ALU.mult), reads=[BBf[h], Bcstm], writes=[BPTt[h]])
                    G.op(9, "pool", lambda e, h=h: e.tensor_tensor(out=WT[:, h, 0, :], in0=Bfull[:, h, 1, :], in1=cstm[:, 128:256], op=ALU.mult), reads=[BBf[h], Bcstm], writes=[BWT[h]])
                    G.op(9, "pool", lambda e, h=h: e.tensor_tensor(out=WT[:, h, 1, :], in0=Bfull[:, h, 1, :], in1=cstm[:, 256:384], op=ALU.mult), reads=[BBf[h], Bcstm], writes=[BWT[h]])
                G.run()
                for lvl in range(5):
                    for h in range(4):
                        pb = PB[h]; Bpb = BPB[h]
                        if lvl < 4:
                            S.op("pe", lambda e, pb=pb, h=h: e.matmul(pb[:, 0:256], lhsT=PTt[:, h, :], rhs=ZPR[:, h, 1:3, :], start=True, stop=True),
                                 reads=[BPTt[h], BZPR[h]], writes=[Bpb])
                            S.op("pe", lambda e, pb=pb, h=h: e.matmul(pb[:, 256:384], lhsT=ZPR[:, h, 1, :], rhs=PTt[:, h, :], start=True, stop=True),
                                 reads=[BPTt[h], BZPR[h]], writes=[Bpb])
                            S.op("dve", lambda e, pb=pb, h=h: e.tensor_tensor(out=ZPR[:, h, 1:3, :], in0=pb[:, 0:256].rearrange("p (a b) -> p a b", a=2),
                                                                             in1=ZPR[:, h, 0:3:2, :], op=ALU.add), reads=[Bpb, BZPR[h]], writes=[BZPR[h]])
                            S.op("act", lambda e, pb=pb, h=h: e.activation(out=PTt[:, h, :], in_=pb[:, 256:384], func=AF.Identity), reads=[Bpb], writes=[BPTt[h]])
                        else:
                            S.op("pe", lambda e, pb=pb, h=h: e.matmul(pb[:, 0:128], lhsT=PTt[:, h, :], rhs=ZPR[:, h, 2, :], start=True, stop=True),
                                 reads=[BPTt[h], BZPR[h]], writes=[Bpb])
                            S.op("dve", lambda e, pb=pb, h=h: e.tensor_tensor(out=ZPR[:, h, 1, :], in0=pb[:, 0:128], in1=ZPR[:, h, 2, :], op=ALU.add),
                                 reads=[Bpb, BZPR[h]], writes=[BZPR[h]])
                for h in range(4):
                    S.op("pe", lambda e, h=h: e.transpose(PT[:, h * 128:(h + 1) * 128], ZPR[:, h, 1, :], ident_b), reads=[BZPR[h], Bcstb], writes=[BPT[h]])
                    S.op("act", lambda e, h=h: e.activation(out=PTt[:, h, :], in_=PT[:, h * 128:(h + 1) * 128], func=AF.Identity), reads=[BPT[h]], writes=[BPTt[h]])
                for h in range(4):
                    pb = PB[h]; Bpb = BPB[h]
                    S.op("pe", lambda e, pb=pb, h=h: e.matmul(pb[:, 0:128], lhsT=WT[:, h, 0, :], rhs=ZPR[:, h, 1, :], start=True, stop=True), reads=[BWT[h], BZPR[h]], writes=[Bpb])
                    S.op("act", lambda e, pb=pb, h=h: e.activation(out=T1s[:, h, :], in_=pb[:, 0:128], func=AF.Identity), reads=[Bpb], writes=[BT1[h]])
                for h in range(4):
                    pb = PB[h]; Bpb = BPB[h]
                    S.op("pe", lambda e, pb=pb, h=h: e.matmul(pb[:, 0:128], lhsT=PTt[:, h, :], rhs=T1s[:, h, :], start=True, stop=True), reads=[BPTt[h], BT1[h]], writes=[Bpb])
                    S.op("pe", lambda e, pb=pb, h=h: e.matmul(pb[:, 128:256], lhsT=T1s[:, h, :], rhs=PTt[:, h, :], start=True, stop=True), reads=[BPTt[h], BT1[h]], writes=[Bpb])
                    S.op("dve", lambda e, pb=pb, h=h: e.tensor_tensor(out=ZPR[:, h, 2, :], in0=pb[:, 0:128], in1=ZPR[:, h, 1, :], op=ALU.add), reads=[Bpb, BZPR[h]], writes=[BZPR[h]])
                    S.op("dve", lambda e, pb=pb, h=h: e.tensor_tensor(out=D2T[:, h, :], in0=pb[:, 128:256], in1=PTt[:, h, :], op=ALU.add), reads=[Bpb, BPTt[h]], writes=[BD2T[h]])
                for h in range(4):
                    pb = PB[h]; Bpb = BPB[h]
                    S.op("pe", lambda e, pb=pb, h=h: e.matmul(pb[:, 0:128], lhsT=WT[:, h, 1, :], rhs=ZPR[:, h, 2, :], start=True, stop=True), reads=[BWT[h], BZPR[h]], writes=[Bpb])
                    S.op("act", lambda e, pb=pb, h=h: e.activation(out=T1s[:, h, :], in_=pb[:, 0:128], func=AF.Identity), reads=[Bpb], writes=[BT1[h]])
                for h in range(4):
                    pb = PB[h]; Bpb = BPB[h]
                    S.op("pe", lambda e, pb=pb, h=h: e.matmul(pb[:, 0:128], lhsT=D2T[:, h, :], rhs=T1s[:, h, :], start=True, stop=True), reads=[BD2T[h], BT1[h]], writes=[Bpb])
                    S.op("dve", lambda e, pb=pb, h=h, tb=tb: e.tensor_tensor(out=Minv[:, tb, h, :], in0=pb[:, 0:128], in1=ZPR[:, h, 2, :], op=ALU.add),
                         reads=[Bpb, BZPR[h]], writes=[Bdn[tb][h]])
            def dn_scan(tl):
                tsl = slice(tl * 128, (tl + 1) * 128)
                tb = tl % 2
                T = tmf[:, tl, :]
                Bt = Btmf[tl]
                for h in range(4):
                    Bd = Bdn[tb][h]
                    pb = PB[h]; Bpb = BPB[h]
                    qT = fm[:, h, tsl]; kT = fm[:, 4 + h, tsl]
                    G.op(0, "pe", lambda e, pb=pb, kT=kT, h=h: e.matmul(pb[:, 0:128], lhsT=kT, rhs=Sbf[:, h, :], start=True, stop=True), reads=[Bfm[4 + h], BS[h]], writes=[Bpb])
                    if own:
                        G.op(0, "pe", lambda e, pb=pb, qT=qT, h=h: e.matmul(pb[:, 128:256], lhsT=qT, rhs=Sbf[:, h, :], start=True, stop=True), reads=[Bfm[h], BS[h]], writes=[Bpb])
                    G.op(1, "dve", lambda e, pb=pb, h=h, tl=tl, tb=tb, T=T: e.scalar_tensor_tensor(out=Rm[:, h, :], in0=pb[:, 0:128], scalar=T[:, 12 + h:13 + h], in1=Vtm[:, tb, h, :],
                                                                                        op0=ALU.mult, op1=ALU.add), reads=[Bpb, Bt, Bd], writes=[BRm[h]])
                    if own:
                        G.op(1, "act", lambda e, pb=pb, h=h, T=T: e.activation(out=QSs[:, h, :], in_=pb[:, 128:256], func=AF.Identity, scale=T[:, 8 + h:9 + h]),
                             reads=[Bpb, Bt], writes=[BQSs[h]])
                    G.op(2, "pe", lambda e, pb=pb, h=h, tl=tl, tb=tb: e.matmul(pb[:, 256:384], lhsT=Minv[:, tb, h, :], rhs=Rm[:, h, :], start=True, stop=True), reads=[Bd, BRm[h]], writes=[Bpb])
                    G.op(3, "act", lambda e, pb=pb, h=h, T=T: e.activation(out=vnew[:, h, :], in_=pb[:, 256:384], func=AF.Identity, scale=T[:, 4 + h:5 + h]),
                         reads=[Bpb, Bt], writes=[Bvn[h]])
                    p4 = PB[4][:, h * 128:(h + 1) * 128]
                    G.op(4, "pe", lambda e, p4=p4, h=h, tl=tl, tb=tb: e.matmul(p4, lhsT=Kdec[:, tb, h, :], rhs=vnew[:, h, :], start=True, stop=True), reads=[Bd, Bvn[h]], writes=[BP4[h]])
                    if own:
                        G.op(4, "pe", lambda e, pb=pb, h=h, tl=tl, tb=tb: e.matmul(pb[:, 384:512], lhsT=Aqk[:, tb, h, :], rhs=vnew[:, h, :], start=True, stop=True), reads=[Bd, Bvn[h]], writes=[Bpb])
                    G.op(5, "dve", lambda e, p4=p4, h=h, T=T: e.scalar_tensor_tensor(out=S32[:, h, :], in0=S32[:, h, :], scalar=T[:, 20 + h:21 + h], in1=p4, op0=ALU.mult, op1=ALU.add),
                         reads=[BP4[h], Bt, BS[h]], writes=[BS[h]])
                    G.op(6, "act", lambda e, h=h: e.activation(out=Sbf[:, h, :], in_=S32[:, h, :], func=AF.Identity), reads=[BS[h]], writes=[BS[h]])
                    if own:
                        G.op(5, "dve", lambda e, pb=pb, h=h: e.tensor_tensor(out=ot[:, h, :], in0=pb[:, 384:512], in1=QSs[:, h, :], op=ALU.add), reads=[Bpb, BQSs[h]], writes=[Bot[h]])
                        G.op(7, "act", lambda e, h=h: e.activation(out=QSs[:, h, :], in_=ot[:, h, :], func=AF.Square, accum_out=ost[:, h, 0:1]), reads=[Bot[h]], writes=[BQSs[h], Bost[h]])
                        G.op(8, "act", lambda e, h=h: e.activation(out=ost[:, h, 1:2], in_=ost[:, h, 0:1], func=AF.Sqrt, scale=1.0 / 128, bias=EPS), reads=[Bost[h]], writes=[Bost[h]])
                        G.op(9, "dve", lambda e, h=h: e.reciprocal(out=ost[:, h, 1:2], in_=ost[:, h, 1:2]), reads=[Bost[h]], writes=[Bost[h]])
                        G.op(10, "dve", lambda e, h=h: e.scalar_tensor_tensor(out=ot[:, h, :], in0=ot[:, h, :], scalar=ost[:, h, 1:2], in1=prm[:, P_DNW:P_DNW + 128], op0=ALU.mult, op1=ALU.mult),
                             reads=[Bot[h], Bost[h], Bprm], writes=[Bot[h]])
                        G.op(11, "pool", lambda e, h=h, tl=tl, tb=tb: e.tensor_tensor(out=om[:, h, :], in0=ot[:, h, :], in1=siluz[:, tl, h * 128:(h + 1) * 128], op=ALU.mult),
                             reads=[Bot[h], Bsz[tl]], writes=[Bom[h]])
                        G.op(12, "pe", lambda e, h=h: e.transpose(PT[:, h * 128:(h + 1) * 128], om[:, h, :], ident_b), reads=[Bom[h], Bcstb], writes=[BPT[h]])
                        G.op(13, "act", lambda e, h=h, tsl=tsl: e.activation(out=mixT[:, h, tsl], in_=PT[:, h * 128:(h + 1) * 128], func=AF.Identity), reads=[BPT[h]], writes=[Bmix[tl]])
                G.run()
            dn_pre(0)
            if dbg and phase == 0 and sti == 0:
                for i_, src in enumerate((Kdec, Vtm, Minv)):
                    S.dma("pool", lambda e, i_=i_, src=src: e.dma_start(out=dbg_d[:, 2048 + i_ * 512: 2048 + (i_ + 1) * 512].rearrange("p (h t) -> p h t", h=4), in_=src[:, 0, :, :]),
                          reads=Bdn[0] + [Bdbg], writes=[Bdbg])
            S.mark(f"p{phase}s{sti}c")
            dn_pre(1)
            dn_scan(0)
            dn_pre(2)
            dn_scan(1)
            dn_pre(3)
            dn_scan(2)
            dn_scan(3)
            S.mark(f"p{phase}s{sti}d")
            if not own:
                return
            for tl in range(4):
                gt = sti * 4 + tl
                for j in range(2):
                    for kb in range(2):
                        k0 = tl * 128 + kb * 128
                        pb = PB[kb]; Bpb = BPB[kb]
                        for g in range(4):
                            h = j * 4 + g
                            pr = (h % 2) * 64
                            S.op("pe", lambda e, pb=pb, j=j, k0=k0, pr=pr, h=h, g=g, tl=tl: e.matmul(
                                pb[:, g * 128:(g + 1) * 128], lhsT=swk[:, j, k0:k0 + 128], rhs=swq[:, h, tl * 128:(tl + 1) * 128],
                                start=True, stop=True), reads=[Bswk[j], Bswq[h // 2]], writes=[Bpb])
                        bofs = C_SWB + (kb * 8 + j * 4) * 128
                        S.op("dve", lambda e, pb=pb, kb=kb, bofs=bofs: e.scalar_tensor_tensor(out=sc[kb][:], in0=pb[:, :], scalar=0.125, in1=cst[:, bofs:bofs + 512],
                                                                                            op0=ALU.mult, op1=ALU.add), reads=[Bpb, Bcst], writes=[Bsc[kb]])
                        if kb == 0 and gt == 0:
                            S.op("act", lambda e, kb=kb: e.activation(out=pTt[:, kb, :], in_=sc[kb][:], func=AF.Exp, bias=prm[:, P_HALO:P_HALO + 1], scale=1.0),
                                 reads=[Bsc[kb], Bprm], writes=[BpT[kb]])
                        else:
                            S.op("act", lambda e, kb=kb: e.activation(out=pTt[:, kb, :], in_=sc[kb][:], func=AF.Exp), reads=[Bsc[kb]], writes=[BpT[kb]])
                    for kb in range(2):
                        S.op("pe", lambda e, kb=kb, tl=tl, j=j: e.matmul(PB[2][:, :], lhsT=vv[:, tl + kb, j, :], rhs=pTt[:, kb, :], start=(kb == 0), stop=(kb == 1)),
                             reads=[Bvv[tl + kb], BpT[kb]], writes=[BPB[2]])
                    for kb in range(2):
                        S.op("pe", lambda e, kb=kb: e.matmul(PB[3][:, :], lhsT=ones_b, rhs=pTt[:, kb, :], start=(kb == 0), stop=(kb == 1)),
                             reads=[Bcstb, BpT[kb]], writes=[BPB[3]])
                    for g in range(4):
                        h = j * 4 + g
                        S.op("dve", lambda e, g=g, h=h: e.tensor_scalar(out=den[:, g * 128:(g + 1) * 128], in0=PB[3][:, g * 128:(g + 1) * 128], scalar1=esink[:, h:h + 1],
                                                                       scalar2=None, op0=ALU.add), reads=[BPB[3], Bder], writes=[Bden])
                    S.op("dve", lambda e: e.reciprocal(out=den[:], in_=den[:]), reads=[Bden], writes=[Bden])
                    for g in range(4):
                        h = j * 4 + g
                        pr = (h % 2) * 64
                        S.op("dve", lambda e, g=g, h=h, pr=pr, tl=tl: e.tensor_tensor(out=mixT[pr:pr + 64, 4 + h // 2, tl * 128:(tl + 1) * 128],
                                                                                     in0=PB[2][pr:pr + 64, g * 128:(g + 1) * 128], in1=den[pr:pr + 64, g * 128:(g + 1) * 128], op=ALU.mult),
                             reads=[BPB[2], Bden], writes=[Bmix[tl]])
            S.mark(f"p{phase}s{sti}e")
            for tl in range(4):
                gt = sti * 4 + tl
                i2 = gt % 2
                for hh in range(2):
                    pb = PB[5 + hh]; Bpb = BPB[5 + hh]
                    for k in range(8):
                        S.op("pe", lambda e, pb=pb, k=k, tl=tl, hh=hh: e.matmul(pb[:, :], lhsT=mixT[:, k, tl * 128:(tl + 1) * 128], rhs=wout[:, k, hh * 512:(hh + 1) * 512],
                                                                               start=(k == 0), stop=(k == 7)), reads=[Bmix[tl], Bwout], writes=[Bpb])
                    S.op("dve", lambda e, pb=pb, i2=i2, hh=hh: e.tensor_tensor(out=x1t[i2][:, hh * 512:(hh + 1) * 512], in0=pb[:, :], in1=gate1[:, hh * 512:(hh + 1) * 512], op=ALU.mult),
                         reads=[Bpb, Bmodbc], writes=[Bx1t[i2]])
                    S.op("pool", lambda e, tl=tl, hh=hh, i2=i2: e.tensor_tensor(out=x1t[i2][:, hh * 512:(hh + 1) * 512], in0=x1t[i2][:, hh * 512:(hh + 1) * 512],
                                                                               in1=xt[tl][:, hh * 512:(hh + 1) * 512], op=ALU.add), reads=[Bx1t[i2], Bxt[tl]], writes=[Bx1t[i2]])
                S.dma("sp", lambda e, gt=gt, i2=i2: e.dma_start(out=out_d[gt * 128:(gt + 1) * 128, :], in_=x1t[i2][:]), reads=[Bx1t[i2]], writes=[Bout])

        for phase in range(2):
            for sti in range(4):
                supertile(phase, sti)
            if phase == 0:
                for h in range(4):
                    S.op("dve", lambda e, h=h: e.tensor_scalar(out=S32[:, h, :], in0=S32[:, h, :], scalar1=prm[:, P_FLAG:P_FLAG + 1], scalar2=None, op0=ALU.mult),
                         reads=[BS[h], Bprm], writes=[BS[h]])
                    S.op("act", lambda e, h=h: e.activation(out=Sbf[:, h, :], in_=S32[:, h, :], func=AF.Identity), reads=[BS[h]], writes=[BS[h]])
                for c in range(12):
                    S.op("dve", lambda e, c=c: e.tensor_scalar(out=halo[:, c, :], in0=halo[:, c, :], scalar1=prm[:, P_FLAG:P_FLAG + 1], scalar2=None, op0=ALU.mult),
                         reads=[Bhalo[c], Bprm], writes=[Bhalo[c]])


        conv_step(1000)
        S.mark("mixer_done")
        S.barrier()
        S.flush(); st.close()
        st = ExitStack()

        if stage >= 2 and SPARSE and not S.stopped:
            xs_d = nc.dram_tensor("xs", [NBLK * 128, 1024], BF16, kind="Internal").ap(); Bxs = S.buf("xs")
            ys_d = nc.dram_tensor("ysc", [NBLK * 128, 1024], F32, kind="Internal").ap(); Bys = S.buf("ys")
            h2s_d = nc.dram_tensor("h2s", [2048, 1024], BF16, kind="Internal").ap(); Bh2s = S.buf("h2s")
            sh2 = sb("sh2", [128, 1024]); w2s = sb("w2s", [128, 1024]); g2 = sb("g2", [128, 1024]); Bmb = S.buf("mb")
            for i_, dst in enumerate((sh2, w2s, g2)):
                S.dma("sp", lambda e, i_=i_, dst=dst: e.dma_start(out=dst[:], in_=scr_d[i_:i_ + 1, :].to_broadcast([128, 1024])), reads=[Bscr], writes=[Bmb])
            bdn = sb("bdn", [32, 1024]); Bbdn = S.buf("bdn")
            S.dma("sp", lambda e: e.dma_start(out=bdn[:], in_=bdn_d), writes=[Bbdn])
            prm2 = sb("prm2", [128, 32]); Bprm2 = S.buf("prm2")
            S.dma("sp", lambda e: e.dma_start(out=prm2[:], in_=prm2_d[:, 0:32]), writes=[Bprm2])
            prm3 = sb("prm3", [128, 160]); Bprm3 = S.buf("prm3")
            S.dma("sp", lambda e: e.dma_start(out=prm3[:], in_=prm3_d), writes=[Bprm3])
            Rk = sb("Rk", [128, 16, 32]); I4 = sb("I4", [128, 16, 4]); GK = sb("GK", [128, 16, 4]); Gt = sb("Gt", [128, 16, 32])
            DIf = sb("DIf", [128, 16, 4]); DI = sb("DI", [128, 64], I32)
            Brt = [S.buf(f"rt{t}") for t in range(16)]; BDI = [S.buf(f"DI{t}") for t in range(16)]
            cntbc = sb("cntbc", [128, 32]); Bcnt = S.buf("cnt")
            rtg = sb("rtg", [128, 4, 32]); Brtg = S.buf("rtg")
            ebf = sb("ebf", [128, 96]); sam = sb("sam", [128, 96]); idf = sb("idf", [128, 2, 96]); idxW = sb("idxW", [128, 192], I32); Beb = S.buf("eb")
            GT = sb("GT", [32, 128]); BGT = S.buf("GT")
            S.op("pool", lambda e: e.memset(cntbc[:], 0.0), writes=[Bcnt])
            st_moe = st
            st = ExitStack()
            wr = sb("wr", [128, 8, 32]); Bwr = S.buf("wr")
            S.dma("sp", lambda e: e.dma_start(out=wr[:], in_=wr_d.rearrange("(k p) n -> p k n", p=128)), writes=[Bwr])
            xb = sb("xb", [128, 1024]); Bxb = S.buf("xb")
            h2f = sb("h2f", [128, 1024]); Bh2f = S.buf("h2f")
            h2b = sb("h2b", [128, 1024], BF16); Bh2b = S.buf("h2b")
            h2Tf = sb("h2Tf", [128, 8, 128]); Bh2Tf = S.buf("h2Tf")
            SUb = sb("SUb", [128, 128], BF16); BSUb = S.buf("SUb")
            Mb = sb("Mb", [128, 32], BF16); BMb = S.buf("Mb")
            sm = sb("sm", [128, 64]); Bsm = S.buf("sm")
            i8 = sb("i8", [128, 8], U32); Bi8 = S.buf("i8")
            ex = sb("ex", [128, 32]); Bex = S.buf("ex")
            dtm = sb("dtm", [128, 2, 32]); Bdtm = S.buf("dtm")
            S.op("dve", lambda e: e.tensor_tensor(out=SUb[:], in0=cs(C_UI), in1=cs(C_OFFD), op=ALU.mult), reads=[Bcst], writes=[BSUb])
            for t in range(16):
                S.dma("sp", lambda e, t=t: e.dma_start(out=xb[:], in_=out_d[t * 128:(t + 1) * 128, :]), reads=[Bout], writes=[Bxb])
                S.op("act", lambda e: e.activation(out=h2f[:], in_=xb[:], func=AF.Square, accum_out=sm[:, 11:12]), reads=[Bxb], writes=[Bh2f, Bsm])
                S.op("act", lambda e: e.activation(out=sm[:, 12:13], in_=sm[:, 11:12], func=AF.Sqrt, scale=1.0 / 1024, bias=EPS), reads=[Bsm], writes=[Bsm])
                S.op("dve", lambda e: e.reciprocal(out=sm[:, 12:13], in_=sm[:, 12:13]), reads=[Bsm], writes=[Bsm])
                S.op("dve", lambda e: e.scalar_tensor_tensor(out=h2f[:], in0=xb[:], scalar=sm[:, 12:13], in1=w2s[:], op0=ALU.mult, op1=ALU.mult), reads=[Bxb, Bsm, Bmb, Bh2f], writes=[Bh2f])
                S.op("dve", lambda e: e.tensor_tensor(out=h2f[:], in0=h2f[:], in1=sh2[:], op=ALU.add), reads=[Bh2f, Bmb], writes=[Bh2f])
                S.op("act", lambda e: e.activation(out=h2b[:], in_=h2f[:], func=AF.Identity), reads=[Bh2f], writes=[Bh2b])
                S.dma("sp", lambda e, t=t: e.dma_start(out=h2s_d[t * 128:(t + 1) * 128, :], in_=h2b[:]), reads=[Bh2b], writes=[Bh2s])
                for b2 in range(2):
                    for kk in range(4):
                        k = b2 * 4 + kk
                        S.op("pe", lambda e, b2=b2, kk=kk, k=k: e.transpose(PB[b2][:, kk * 128:(kk + 1) * 128], h2f[:, k * 128:(k + 1) * 128], cs(C_ID)), reads=[Bh2f, Bcst], writes=[BPB[b2]])
                    S.op("dve", lambda e, b2=b2: e.tensor_copy(out=h2Tf[:, b2 * 4:(b2 + 1) * 4, :], in_=PB[b2][:, :].rearrange("p (a b) -> p a b", a=4)), reads=[BPB[b2]], writes=[Bh2Tf])
                for k in range(8):
                    S.op("pe", lambda e, k=k: e.matmul(PB[6][:, 0:32], lhsT=h2Tf[:, k, :], rhs=wr[:, k, :], start=(k == 0), stop=(k == 7)), reads=[Bh2Tf, Bwr], writes=[BPB[6]])
                S.op("dve", lambda e: e.tensor_tensor(out=sm[:, 16:48], in0=PB[6][:, 0:32], in1=prm2[:, 0:32], op=ALU.add), reads=[BPB[6], Bprm2, Bsm], writes=[Bsm])
                S.op("dve", lambda e: e.max(out=sm[:, 0:8], in_=sm[:, 16:48]), reads=[Bsm], writes=[Bsm])
                S.op("dve", lambda e: e.max_index(out=i8[:], in_max=sm[:, 0:8], in_values=sm[:, 16:48]), reads=[Bsm], writes=[Bi8])
                S.op("dve", lambda e, t=t: e.tensor_copy(out=I4[:, t, :], in_=i8[:, 0:4]), reads=[Bi8], writes=[Brt[t]])
                S.op("dve", lambda e: e.tensor_scalar(out=sm[:, 8:9], in0=sm[:, 0:1], scalar1=-1.0, scalar2=None, op0=ALU.mult), reads=[Bsm], writes=[Bsm])
                S.op("act", lambda e: e.activation(out=ex[:], in_=sm[:, 16:48], func=AF.Exp, bias=sm[:, 8:9], scale=1.0), reads=[Bsm], writes=[Bex])
                S.op("act", lambda e: e.activation(out=sm[:, 48:52], in_=sm[:, 0:4], func=AF.Exp, bias=sm[:, 8:9], scale=1.0), reads=[Bsm], writes=[Bsm])
                S.op("dve", lambda e: e.tensor_scalar(out=Mb[:], in0=sm[:, 16:48], scalar1=sm[:, 3:4], scalar2=None, op0=ALU.is_ge), reads=[Bsm], writes=[BMb])
                S.op("dve", lambda e: e.scalar_tensor_tensor(out=ex[:], in0=sm[:, 16:48], scalar=sm[:, 3:4], in1=ex[:], op0=ALU.is_ge, op1=ALU.mult), reads=[Bsm, Bex], writes=[Bex])
                S.op("dve", lambda e: e.reduce_sum(out=sm[:, 9:10], in_=ex[:], axis=AX.X), reads=[Bex, Bsm], writes=[Bsm])
                S.op("dve", lambda e: e.reciprocal(out=sm[:, 10:11], in_=sm[:, 9:10]), reads=[Bsm], writes=[Bsm])
                S.op("dve", lambda e, t=t: e.tensor_scalar(out=Gt[:, t, :], in0=ex[:], scalar1=sm[:, 10:11], scalar2=None, op0=ALU.mult), reads=[Bex, Bsm], writes=[Brt[t]])
                S.op("dve", lambda e, t=t: e.tensor_scalar(out=GK[:, t, :], in0=sm[:, 48:52], scalar1=sm[:, 10:11], scalar2=None, op0=ALU.mult), reads=[Bsm], writes=[Brt[t]])
                S.op("pe", lambda e: e.matmul(PB[6][:, 32:64], lhsT=SUb[:], rhs=Mb[:], start=True, stop=True), reads=[BSUb, BMb], writes=[BPB[6]])
                S.op("pe", lambda e: e.matmul(PB[6][:, 64:96], lhsT=ones_b, rhs=Mb[:], start=True, stop=True), reads=[Bcstb, BMb], writes=[BPB[6]])
                S.op("dve", lambda e, t=t: e.tensor_tensor(out=Rk[:, t, :], in0=PB[6][:, 32:64], in1=cntbc[:], op=ALU.add), reads=[BPB[6], Bcnt], writes=[Brt[t]])
                S.op("dve", lambda e: e.tensor_tensor(out=cntbc[:], in0=PB[6][:, 64:96], in1=cntbc[:], op=ALU.add), reads=[BPB[6], Bcnt], writes=[Bcnt])
                S.mark(f"m_fe{t}")
            S.op("dve", lambda e: e.tensor_scalar(out=rtg[:, 0, :], in0=cntbc[:], scalar1=0.0, scalar2=None, op0=ALU.is_gt), reads=[Bcnt], writes=[Brtg])
            for j in range(1, 16):
                S.op("dve", lambda e, j=j: e.scalar_tensor_tensor(out=rtg[:, 0, :], in0=cntbc[:], scalar=128.0 * j, in1=rtg[:, 0, :], op0=ALU.is_gt, op1=ALU.add), reads=[Bcnt, Brtg], writes=[Brtg])
            S.op("dve", lambda e: e.tensor_copy(out=rtg[:, 1, :], in_=rtg[:, 0, :]), reads=[Brtg], writes=[Brtg])
            a_, b_ = 1, 2
            for sh in (1, 2, 4, 8, 16):
                S.op("dve", lambda e, a_=a_, b_=b_, sh=sh: e.tensor_tensor(out=rtg[:, b_, sh:32], in0=rtg[:, a_, sh:32], in1=rtg[:, a_, 0:32 - sh], op=ALU.add), reads=[Brtg], writes=[Brtg])
                S.op("dve", lambda e, a_=a_, b_=b_, sh=sh: e.tensor_copy(out=rtg[:, b_, 0:sh], in_=rtg[:, a_, 0:sh]), reads=[Brtg], writes=[Brtg])
                a_, b_ = b_, a_
            incl = a_
            S.op("dve", lambda e, incl=incl: e.tensor_tensor(out=rtg[:, 3, :], in0=rtg[:, incl, :], in1=rtg[:, 0, :], op=ALU.subtract), reads=[Brtg], writes=[Brtg])
            S.op("dve", lambda e: e.tensor_scalar(out=rtg[:, 3, :], in0=rtg[:, 3, :], scalar1=128.0, scalar2=None, op0=ALU.mult), reads=[Brtg], writes=[Brtg])
            S.op("dve", lambda e, incl=incl: e.tensor_scalar(out=ebf[:], in0=prm3[:, 32:128], scalar1=rtg[:, incl, 0:1], scalar2=None, op0=ALU.is_ge), reads=[Brtg, Bprm3], writes=[Beb])
            for e_ in range(1, 32):
                S.op("dve", lambda e, e_=e_, incl=incl: e.scalar_tensor_tensor(out=ebf[:], in0=prm3[:, 32:128], scalar=rtg[:, incl, e_:e_ + 1], in1=ebf[:], op0=ALU.is_ge, op1=ALU.add),
                     reads=[Brtg, Bprm3, Beb], writes=[Beb])
            S.op("dve", lambda e: e.tensor_scalar(out=ebf[:], in0=ebf[:], scalar1=31.0, scalar2=None, op0=ALU.min), reads=[Beb], writes=[Beb])
            S.op("dve", lambda e: e.memset(sam[:], 0.0), writes=[Beb])
            S.op("dve", lambda e: e.tensor_tensor(out=sam[:, 2:96], in0=ebf[:, 2:96], in1=ebf[:, 0:94], op=ALU.is_equal), reads=[Beb], writes=[Beb])
            S.op("dve", lambda e: e.tensor_scalar(out=idf[:, 0, :], in0=ebf[:], scalar1=128.0, scalar2=prm3[:, 128:129], op0=ALU.mult, op1=ALU.add), reads=[Beb, Bprm3], writes=[Beb])
            S.op("dve", lambda e: e.scalar_tensor_tensor(out=idf[:, 1, :], in0=sam[:], scalar=1.0e6, in1=idf[:, 0, :], op0=ALU.mult, op1=ALU.add), reads=[Beb], writes=[Beb])
            S.op("dve", lambda e: e.tensor_copy(out=idxW[:], in_=idf[:, :, :].rearrange("p a b -> p (a b)")), reads=[Beb], writes=[Beb])
            S.mark("m_rt")
            for t in range(16):
                S.op("dve", lambda e, t=t: e.tensor_tensor(out=dtm[:, 0, :], in0=Rk[:, t, :], in1=rtg[:, 3, :], op=ALU.add), reads=[Brt[t], Brtg, Bdtm], writes=[Bdtm])
                for k in range(4):
                    S.op("dve", lambda e, t=t, k=k: e.scalar_tensor_tensor(out=dtm[:, 1, :], in0=prm3[:, 0:32], scalar=I4[:, t, k:k + 1], in1=dtm[:, 0, :], op0=ALU.is_equal, op1=ALU.mult),
                         reads=[Brt[t], Bprm3, Bdtm], writes=[Bdtm])
                    S.op("dve", lambda e, t=t, k=k: e.reduce_sum(out=DIf[:, t, k:k + 1], in_=dtm[:, 1, :], axis=AX.X), reads=[Bdtm], writes=[BDI[t]])
                S.op("dve", lambda e, t=t: e.tensor_copy(out=DI[:, t * 4:t * 4 + 4], in_=DIf[:, t, :]), reads=[BDI[t]], writes=[BDI[t]])
                S.dma("sp", lambda e, t=t: e.dma_start(out=h2b[:], in_=h2s_d[t * 128:(t + 1) * 128, :]), reads=[Bh2s], writes=[Bh2b])
                for k in range(4):
                    S.dma("pool", lambda e, t=t, k=k: e.indirect_dma_start(out=xs_d[:, :], out_offset=bass.IndirectOffsetOnAxis(ap=DI[:, t * 4 + k:t * 4 + k + 1], axis=0), in_=h2b[:, :], in_offset=None), reads=[Bh2b, BDI[t]], writes=[Bxs])
                S.mark(f"m_d{t}")
            S.barrier()
            S.flush(); st.close()
            st = ExitStack()
            wu = [sb(f"wu{i}", [128, 8, 2048], BF16) for i in range(2)]; Bwu = [S.buf(f"wu{i}") for i in range(2)]
            wd = [sb(f"wd{i}", [128, 8, 1024], BF16) for i in range(2)]; Bwd = [S.buf(f"wd{i}") for i in range(2)]
            bup = [sb(f"bup{i}", [128, 16]) for i in range(2)]; Bbup = [S.buf(f"bup{i}") for i in range(2)]
            xblk = [sb(f"xblk{i}", [128, 1024], BF16) for i in range(2)]; Bxblk = [S.buf(f"xblk{i}") for i in range(2)]
            xT = [sb(f"xT{i}", [128, 8, 128], BF16) for i in range(2)]; BxT = [S.buf(f"xT{i}") for i in range(2)]
            gq = sb("gq", [128, 1024]); lq = sb("lq", [128, 1024]); sgm = sb("sgm", [128, 1024]); Bgq = S.buf("gq"); Blq = S.buf("lq"); Bsgm = S.buf("sgm")
            actT = [sb(f"actT{i}", [128, 8, 128], BF16) for i in range(2)]; BactT = [S.buf(f"actT{i}") for i in range(2)]
            yblk = [sb(f"yblk{i}", [128, 1024]) for i in range(2)]; Byblk = [S.buf(f"yblk{i}") for i in range(2)]

            regs = {}

            def bnd(e):
                if "bnd" not in regs:
                    regs["bnd"] = e.alloc_register("bnd4095")
                    e.reg_mov(regs["bnd"], 4095)
                return regs["bnd"]

            def load_wu(b):
                i = b % 2
                S.dma("pool", lambda e, b=b, i=i: e.indirect_dma_start(out=wu[i][:, :, :].rearrange("p k n -> p (k n)"), out_offset=None, in_=wub_d[:, :],
                                                                    in_offset=bass.IndirectOffsetOnAxis(ap=idxW[:, 96 + b:96 + b + 1], axis=0), bounds_check=bnd(e), oob_is_err=False),
                      reads=[Beb, Bwcv], writes=[Bwu[i]])
                S.dma("pool", lambda e, b=b, i=i: e.indirect_dma_start(out=bup[i][:, :], out_offset=None, in_=bupg_d[:, :],
                                                                    in_offset=bass.IndirectOffsetOnAxis(ap=idxW[:, b:b + 1], axis=0)),
                      reads=[Beb], writes=[Bbup[i]])

            def load_wd(b):
                i = b % 2
                S.dma("pool", lambda e, b=b, i=i: e.indirect_dma_start(out=wd[i][:, :, :].rearrange("p k n -> p (k n)"), out_offset=None, in_=wdb_d[:, :],
                                                                    in_offset=bass.IndirectOffsetOnAxis(ap=idxW[:, 96 + b:96 + b + 1], axis=0), bounds_check=bnd(e), oob_is_err=False),
                      reads=[Beb, Bwcv], writes=[Bwd[i]])

            def up_blk(b):
                i = b % 2
                S.dma("sp", lambda e, b=b, i=i: e.dma_start(out=xblk[i][:], in_=xs_d[b * 128:(b + 1) * 128, :]), reads=[Bxs], writes=[Bxblk[i]])
                for k in range(8):
                    S.op("pe", lambda e, i=i, k=k: e.transpose(PT[:, k * 128:(k + 1) * 128], xblk[i][:, k * 128:(k + 1) * 128], ident_b), reads=[Bxblk[i], Bcstb], writes=[BPTb])
                S.op("act", lambda e, i=i: e.activation(out=xT[i][:, :, :], in_=PT[:, :].rearrange("p (a b) -> p a b", a=8), func=AF.Identity), reads=[BPTb], writes=[BxT[i]])
                for j in range(16):
                    pb = PB[j // 4]; Bpb = BPB[j // 4]
                    for k in range(8):
                        S.op("pe", lambda e, pb=pb, i=i, j=j, k=k: e.matmul(pb[:, (j % 4) * 128:(j % 4 + 1) * 128], lhsT=wu[i][:, k, j * 128:(j + 1) * 128], rhs=xT[i][:, k, :],
                                                                           start=(k == 0), stop=(k == 7)), reads=[Bwu[i], BxT[i]], writes=[Bpb])
                for j in range(16):
                    pb = PB[j // 4]; Bpb = BPB[j // 4]
                    dst = gq if j < 8 else lq
                    Bdst = Bgq if j < 8 else Blq
                    jj = j % 8
                    S.op("dve", lambda e, pb=pb, i=i, j=j, jj=jj, dst=dst: e.tensor_scalar(out=dst[:, jj * 128:(jj + 1) * 128], in0=pb[:, (j % 4) * 128:(j % 4 + 1) * 128], scalar1=bup[i][:, j:j + 1],
                                                                                       scalar2=7.0, op0=ALU.add, op1=ALU.min), reads=[Bpb, Bbup[i]], writes=[Bdst])
                S.op("act", lambda e: e.activation(out=sgm[:], in_=gq[:], func=AF.Sigmoid, scale=1.702), reads=[Bgq], writes=[Bsgm])
                S.op("dve", lambda e: e.tensor_scalar(out=lq[:], in0=lq[:], scalar1=-7.0, scalar2=1.0, op0=ALU.max, op1=ALU.add), reads=[Blq], writes=[Blq])
                S.op("dve", lambda e: e.tensor_tensor(out=gq[:], in0=gq[:], in1=sgm[:], op=ALU.mult), reads=[Bgq, Bsgm], writes=[Bgq])
                S.op("dve", lambda e, i=i: e.tensor_tensor(out=actT[i][:, :, :].rearrange("p a b -> p (a b)"), in0=gq[:], in1=lq[:], op=ALU.mult), reads=[Bgq, Blq], writes=[BactT[i]])

            def down_blk(b):
                i = b % 2
                for hh in range(2):
                    pb = PB[4 + hh]; Bpb = BPB[4 + hh]
                    for jd in range(8):
                        S.op("pe", lambda e, pb=pb, i=i, jd=jd, hh=hh: e.matmul(pb[:, :], lhsT=actT[i][:, jd, :], rhs=wd[i][:, jd, hh * 512:(hh + 1) * 512], start=(jd == 0), stop=(jd == 7)),
                             reads=[BactT[i], Bwd[i]], writes=[Bpb])
                    S.op("act", lambda e, pb=pb, i=i, hh=hh: e.activation(out=yblk[i][:, hh * 512:(hh + 1) * 512], in_=pb[:, :], func=AF.Identity), reads=[Bpb], writes=[Byblk[i]])
                S.dma("sp", lambda e, b=b, i=i: e.dma_start(out=ys_d[b * 128:(b + 1) * 128, :], in_=yblk[i][:]), reads=[Byblk[i]], writes=[Bys])

            load_wu(0); load_wd(0); load_wu(1); load_wd(1)
            S.mark("m_ld")
            up_blk(0)
            S.mark("m_b0")
            for b in range(1, NBLK):
                up_blk(b)
                if b + 1 < NBLK:
                    load_wu(b + 1)
                down_blk(b - 1)
                if b + 1 < NBLK:
                    load_wd(b + 1)
                S.mark(f"m_b{b}")
            down_blk(NBLK - 1)
            S.mark("m_blk")
            S.barrier()
            S.flush(); st.close()
            st = ExitStack()
            Yg_ = [sb(f"Yg{i}", [128, 4, 1024]) for i in range(2)]
            Bo2.extend(S.buf(f"o2_{t}") for t in range(16)); BYg_ = [S.buf(f"Yg{i}") for i in range(2)]
            xb_ = [sb(f"xbc{i}", [128, 1024]) for i in range(2)]; Bxb_ = [S.buf(f"xbc{i}") for i in range(2)]
            acc_ = [sb(f"accc{i}", [128, 1024]) for i in range(2)]; Bacc_ = [S.buf(f"accc{i}") for i in range(2)]
            for t in range(16):
                Yg = Yg_[t % 2]; BYg = BYg_[t % 2]; xb = xb_[t % 2]; Bxb = Bxb_[t % 2]; acc = acc_[t % 2]; Bacc = Bacc_[t % 2]
                for k in range(4):
                    S.dma("pool", lambda e, t=t, k=k, Yg=Yg, xb=xb, acc=acc: e.indirect_dma_start(out=Yg[:, k, :], out_offset=None, in_=ys_d[:, :],
                                                                       in_offset=bass.IndirectOffsetOnAxis(ap=DI[:, t * 4 + k:t * 4 + k + 1], axis=0)),
                          reads=[BDI[t], Bys], writes=[BYg])
                S.dma("sp", lambda e, t=t, Yg=Yg, xb=xb, acc=acc: e.dma_start(out=xb[:], in_=out_d[t * 128:(t + 1) * 128, :]), reads=[Bo2[t]], writes=[Bxb])
                S.op("pe", lambda e, t=t, Yg=Yg, xb=xb, acc=acc: e.transpose(PB[6][0:32, 128:256], Gt[:, t, :], cs(C_ID)), reads=[Brt[t], Bcst], writes=[BPB[6]])
                S.op("act", lambda e, Yg=Yg, xb=xb, acc=acc: e.activation(out=GT[:, :], in_=PB[6][0:32, 128:256], func=AF.Identity), reads=[BPB[6]], writes=[BGT])
                S.op("dve", lambda e, t=t, Yg=Yg, xb=xb, acc=acc: e.tensor_scalar(out=acc[:], in0=Yg[:, 0, :], scalar1=GK[:, t, 0:1], scalar2=None, op0=ALU.mult), reads=[BYg, Brt[t]], writes=[Bacc])
                for k in range(1, 4):
                    S.op("dve", lambda e, t=t, k=k, Yg=Yg, xb=xb, acc=acc: e.scalar_tensor_tensor(out=acc[:], in0=Yg[:, k, :], scalar=GK[:, t, k:k + 1], in1=acc[:], op0=ALU.mult, op1=ALU.add),
                         reads=[BYg, Brt[t], Bacc], writes=[Bacc])
                for hh in range(2):
                    pb = PB[4 + hh]; Bpb = BPB[4 + hh]
                    S.op("pe", lambda e, pb=pb, hh=hh, Yg=Yg, xb=xb, acc=acc: e.matmul(pb[:, :], lhsT=GT[0:32, :], rhs=bdn[0:32, hh * 512:(hh + 1) * 512], start=True, stop=True), reads=[BGT, Bbdn], writes=[Bpb])
                    S.op("dve", lambda e, pb=pb, hh=hh, Yg=Yg, xb=xb, acc=acc: e.tensor_tensor(out=acc[:, hh * 512:(hh + 1) * 512], in0=pb[:, :], in1=acc[:, hh * 512:(hh + 1) * 512], op=ALU.add),
                         reads=[Bpb, Bacc], writes=[Bacc])
                S.op("dve", lambda e, Yg=Yg, xb=xb, acc=acc: e.tensor_tensor(out=acc[:], in0=acc[:], in1=g2[:], op=ALU.mult), reads=[Bacc, Bmb], writes=[Bacc])
                S.op("dve", lambda e, Yg=Yg, xb=xb, acc=acc: e.tensor_tensor(out=acc[:], in0=acc[:], in1=xb[:], op=ALU.add), reads=[Bacc, Bxb], writes=[Bacc])
                S.dma("sp", lambda e, t=t, Yg=Yg, xb=xb, acc=acc: e.dma_start(out=out_d[t * 128:(t + 1) * 128, :], in_=acc[:]), reads=[Bacc], writes=[Bo2[t]])
            S.flush(); st.close()
            st = st_moe
        if stage >= 2 and not SPARSE and not S.stopped:
            sh2 = sb("sh2", [128, 1024]); w2s = sb("w2s", [128, 1024]); g2 = sb("g2", [128, 1024]); Bmb = S.buf("mb")
            for i_, dst in enumerate((sh2, w2s, g2)):
                S.dma("sp", lambda e, i_=i_, dst=dst: e.dma_start(out=dst[:], in_=scr_d[i_:i_ + 1, :].to_broadcast([128, 1024])), reads=[Bscr], writes=[Bmb])
            wr = sb("wr", [128, 8, 32]); Bwr = S.buf("wr")
            S.dma("sp", lambda e: e.dma_start(out=wr[:], in_=wr_d.rearrange("(k p) n -> p k n", p=128)), writes=[Bwr])
            prm2 = sb("prm2", [128, 544]); Bprm2 = S.buf("prm2")
            S.dma("sp", lambda e: e.dma_start(out=prm2[:], in_=prm2_d), writes=[Bprm2])
            bdn = sb("bdn", [32, 1024]); Bbdn = S.buf("bdn")
            S.dma("sp", lambda e: e.dma_start(out=bdn[:], in_=bdn_d), writes=[Bbdn])
            h2T = sb("h2T", [128, 8, 1024], BF16); Bh2T = [S.buf(f"h2T{t}") for t in range(8)]
            xb = sb("xb", [128, 1024]); Bxb = S.buf("xb")
            h2f = sb("h2f", [128, 1024]); Bh2f = S.buf("h2f")
            h2Tf = sb("h2Tf", [128, 8, 128]); Bh2Tf = S.buf("h2Tf")
            Gt = sb("Gt", [128, 8, 32]); BG = [S.buf(f"G{t}") for t in range(8)]
            GT = sb("GT", [32, 128]); BGT = S.buf("GT")
            acc = sb("acc", [128, 8, 1024]); Bacc = [S.buf(f"acc{t}") for t in range(8)]
            wu = [sb(f"wu{i}", [128, 8, 2048], BF16) for i in range(2)]; Bwu = [S.buf(f"wu{i}") for i in range(2)]
            wd = sb("wd", [128, 8, 1024], BF16); Bwd = S.buf("wd")
            actT = [sb(f"actT{i}", [128, 8, 512], BF16) for i in range(2)]; BactT = [S.buf(f"actT{i}") for i in range(2)]
            gq = [sb(f"gq{i}", [128, 512]) for i in range(2)]; sg = [sb(f"sg{i}", [128, 512]) for i in range(1)] * 2; lq = [sb(f"lq{i}", [128, 512]) for i in range(1)] * 2
            Bgq = [S.buf(f"gq{i}") for i in range(2)]; Bsg = [S.buf(f"sg{i}") for i in range(1)] * 2; Blq = [S.buf(f"lq{i}") for i in range(1)] * 2
            sm = sb("sm", [128, 64]); Bsm = S.buf("sm")
            ex = sb("ex", [128, 32]); Bex = S.buf("ex")
            wupv = wup_d.rearrange("(e k p) n -> e p k n", e=32, p=128)
            wdnv = wdn_d.rearrange("(e k p) n -> e p k n", e=32, p=128)

            def load_wu(e_):
                for hh in range(2):
                    S.dma("pool", lambda e, e_=e_, hh=hh: e.dma_start(out=wu[e_ % 2][:, :, hh * 1024:(hh + 1) * 1024], in_=wupv[e_, :, :, hh * 1024:(hh + 1) * 1024]), writes=[Bwu[e_ % 2]])

            def load_wd(e_):
                S.dma("pool", lambda e, e_=e_: e.dma_start(out=wd[:], in_=wdnv[e_]), writes=[Bwd])

            rot = {"up": 0, "dn": 0, "q": 0}

            def up_item(e_, tg, ai):
                for jj in range(8):
                    banks = []
                    for j in (jj, jj + 8):
                        bi = rot["up"] % 4
                        rot["up"] += 1
                        pb = PB[bi]; Bpb = BPB[bi]
                        for k in range(8):
                            S.op("pe", lambda e, pb=pb, k=k, j=j, e_=e_, tg=tg: e.matmul(pb[:, :], lhsT=wu[e_ % 2][:, k, j * 128:(j + 1) * 128], rhs=h2T[:, k, tg * 512:(tg + 1) * 512],
                                                                                       start=(k == 0), stop=(k == 7)), reads=[Bwu[e_ % 2]] + Bh2T[tg * 4:(tg + 1) * 4], writes=[Bpb])
                        banks.append((pb, Bpb))
                    qi = rot["q"] % 2
                    rot["q"] += 1
                    (pa, Bpa), (pbb, Bpbb) = banks
                    bg = 32 + e_ * 16 + jj
                    bl = 32 + e_ * 16 + jj + 8
                    S.op("dve", lambda e, pa=pa, qi=qi, bg=bg: e.tensor_scalar(out=gq[qi][:], in0=pa[:, :], scalar1=prm2[:, bg:bg + 1], scalar2=7.0, op0=ALU.add, op1=ALU.min),
                         reads=[Bpa, Bprm2], writes=[Bgq[qi]])
                    S.op("act", lambda e, qi=qi: e.activation(out=sg[qi][:], in_=gq[qi][:], func=AF.Sigmoid, scale=1.702), reads=[Bgq[qi]], writes=[Bsg[qi]])
                    S.op("dve", lambda e, pbb=pbb, qi=qi, bl=bl: e.tensor_scalar(out=lq[qi][:], in0=pbb[:, :], scalar1=prm2[:, bl:bl + 1], scalar2=7.0, op0=ALU.add, op1=ALU.min),
                         reads=[Bpbb, Bprm2], writes=[Blq[qi]])
                    S.op("dve", lambda e, qi=qi: e.tensor_scalar(out=lq[qi][:], in0=lq[qi][:], scalar1=-7.0, scalar2=1.0, op0=ALU.max, op1=ALU.add), reads=[Blq[qi]], writes=[Blq[qi]])
                    S.op("dve", lambda e, qi=qi: e.tensor_tensor(out=gq[qi][:], in0=gq[qi][:], in1=sg[qi][:], op=ALU.mult), reads=[Bgq[qi], Bsg[qi]], writes=[Bgq[qi]])
                    S.op("dve", lambda e, qi=qi, ai=ai, jj=jj: e.tensor_tensor(out=actT[ai][:, jj, :], in0=gq[qi][:], in1=lq[qi][:], op=ALU.mult), reads=[Bgq[qi], Blq[qi]], writes=[BactT[ai]])

            def down_item(e_, tg, ai):
                for tt in range(4):
                    t = tg * 4 + tt
                    for hh in range(2):
                        bi = 4 + rot["dn"] % 2
                        rot["dn"] += 1
                        pb = PB[bi]; Bpb = BPB[bi]
                        for jd in range(8):
                            S.op("pe", lambda e, pb=pb, jd=jd, tt=tt, hh=hh, ai=ai: e.matmul(pb[:, :], lhsT=actT[ai][:, jd, tt * 128:(tt + 1) * 128], rhs=wd[:, jd, hh * 512:(hh + 1) * 512],
                                                                                         start=(jd == 0), stop=(jd == 7)), reads=[BactT[ai], Bwd], writes=[Bpb])
                        if e_ == 0:
                            S.op("dve", lambda e, pb=pb, t=t, hh=hh, e_=e_: e.tensor_scalar(out=acc[:, t, hh * 512:(hh + 1) * 512], in0=pb[:, :], scalar1=Gt[:, t, e_:e_ + 1], scalar2=None, op0=ALU.mult),
                                 reads=[Bpb, BG[t]], writes=[Bacc[t]])
                        else:
                            S.op("dve", lambda e, pb=pb, t=t, hh=hh, e_=e_: e.scalar_tensor_tensor(out=acc[:, t, hh * 512:(hh + 1) * 512], in0=pb[:, :], scalar=Gt[:, t, e_:e_ + 1],
                                                                                                in1=acc[:, t, hh * 512:(hh + 1) * 512], op0=ALU.mult, op1=ALU.add),
                                 reads=[Bpb, BG[t], Bacc[t]], writes=[Bacc[t]])

            for pss in range(2):
                for t in range(8):
                    gt = pss * 8 + t
                    S.dma("sp", lambda e, gt=gt: e.dma_start(out=xb[:], in_=out_d[gt * 128:(gt + 1) * 128, :]), reads=[Bout], writes=[Bxb])
                    S.op("act", lambda e: e.activation(out=h2f[:], in_=xb[:], func=AF.Square, accum_out=sm[:, 11:12]), reads=[Bxb], writes=[Bh2f, Bsm])
                    S.op("act", lambda e: e.activation(out=sm[:, 12:13], in_=sm[:, 11:12], func=AF.Sqrt, scale=1.0 / 1024, bias=EPS), reads=[Bsm], writes=[Bsm])
                    S.op("dve", lambda e: e.reciprocal(out=sm[:, 12:13], in_=sm[:, 12:13]), reads=[Bsm], writes=[Bsm])
                    S.op("dve", lambda e: e.scalar_tensor_tensor(out=h2f[:], in0=xb[:], scalar=sm[:, 12:13], in1=w2s[:], op0=ALU.mult, op1=ALU.mult), reads=[Bxb, Bsm, Bmb, Bh2f], writes=[Bh2f])
                    S.op("dve", lambda e: e.tensor_tensor(out=h2f[:], in0=h2f[:], in1=sh2[:], op=ALU.add), reads=[Bh2f, Bmb], writes=[Bh2f])
                    for b2 in range(2):
                        for kk in range(4):
                            k = b2 * 4 + kk
                            S.op("pe", lambda e, b2=b2, kk=kk, k=k: e.transpose(PB[b2][:, kk * 128:(kk + 1) * 128], h2f[:, k * 128:(k + 1) * 128], cs(C_ID)), reads=[Bh2f, Bcst], writes=[BPB[b2]])
                        S.op("act", lambda e, b2=b2, t=t: e.activation(out=h2T[:, b2 * 4:(b2 + 1) * 4, t * 128:(t + 1) * 128], in_=PB[b2][:, :].rearrange("p (a b) -> p a b", a=4), func=AF.Identity),
                             reads=[BPB[b2]], writes=[Bh2T[t]])
                        S.op("dve", lambda e, b2=b2: e.tensor_copy(out=h2Tf[:, b2 * 4:(b2 + 1) * 4, :], in_=PB[b2][:, :].rearrange("p (a b) -> p a b", a=4)), reads=[BPB[b2]], writes=[Bh2Tf])
                    for k in range(8):
                        S.op("pe", lambda e, k=k: e.matmul(PB[6][:, 0:32], lhsT=h2Tf[:, k, :], rhs=wr[:, k, :], start=(k == 0), stop=(k == 7)), reads=[Bh2Tf, Bwr], writes=[BPB[6]])
                    S.op("dve", lambda e: e.tensor_tensor(out=sm[:, 16:48], in0=PB[6][:, 0:32], in1=prm2[:, 0:32], op=ALU.add), reads=[BPB[6], Bprm2, Bsm], writes=[Bsm])
                    S.op("dve", lambda e: e.max(out=sm[:, 0:8], in_=sm[:, 16:48]), reads=[Bsm], writes=[Bsm])
                    S.op("dve", lambda e: e.tensor_scalar(out=sm[:, 8:9], in0=sm[:, 0:1], scalar1=-1.0, scalar2=None, op0=ALU.mult), reads=[Bsm], writes=[Bsm])
                    S.op("act", lambda e: e.activation(out=ex[:], in_=sm[:, 16:48], func=AF.Exp, bias=sm[:, 8:9], scale=1.0), reads=[Bsm], writes=[Bex])
                    S.op("dve", lambda e: e.scalar_tensor_tensor(out=ex[:], in0=sm[:, 16:48], scalar=sm[:, 3:4], in1=ex[:], op0=ALU.is_ge, op1=ALU.mult), reads=[Bsm, Bex], writes=[Bex])
                    S.op("dve", lambda e: e.reduce_sum(out=sm[:, 9:10], in_=ex[:], axis=AX.X), reads=[Bex, Bsm], writes=[Bsm])
                    S.op("dve", lambda e: e.reciprocal(out=sm[:, 10:11], in_=sm[:, 9:10]), reads=[Bsm], writes=[Bsm])
                    S.op("dve", lambda e, t=t: e.tensor_scalar(out=Gt[:, t, :], in0=ex[:], scalar1=sm[:, 10:11], scalar2=None, op0=ALU.mult), reads=[Bex, Bsm], writes=[BG[t]])
                items = [(e_, tg) for e_ in range(32) for tg in range(2)]
                load_wu(0)
                load_wd(0)
                load_wu(1)
                up_item(0, 0, 0)
                for i in range(1, len(items)):
                    e_, tg = items[i]
                    pe_, ptg = items[i - 1]
                    if tg == 0 and e_ + 1 < 32:
                        load_wu(e_ + 1)
                    up_item(e_, tg, i % 2)
                    down_item(pe_, ptg, (i - 1) % 2)
                    if ptg == 1 and pe_ + 1 < 32:
                        load_wd(pe_ + 1)
                down_item(31, 1, (len(items) - 1) % 2)
                for t in range(8):
                    gt = pss * 8 + t
                    S.op("pe", lambda e, t=t: e.transpose(PB[6][0:32, 128:256], Gt[:, t, :], cs(C_ID)), reads=[BG[t], Bcst], writes=[BPB[6]])
                    S.op("act", lambda e: e.activation(out=GT[:, :], in_=PB[6][0:32, 128:256], func=AF.Identity), reads=[BPB[6]], writes=[BGT])
                    S.dma("sp", lambda e, gt=gt: e.dma_start(out=xb[:], in_=out_d[gt * 128:(gt + 1) * 128, :]), reads=[Bout], writes=[Bxb])
                    for hh in range(2):
                        pb = PB[4 + hh]; Bpb = BPB[4 + hh]
                        S.op("pe", lambda e, pb=pb, hh=hh: e.matmul(pb[:, :], lhsT=GT[0:32, :], rhs=bdn[0:32, hh * 512:(hh + 1) * 512], start=True, stop=True), reads=[BGT, Bbdn], writes=[Bpb])
                        S.op("dve", lambda e, pb=pb, t=t, hh=hh: e.tensor_tensor(out=acc[:, t, hh * 512:(hh + 1) * 512], in0=pb[:, :], in1=acc[:, t, hh * 512:(hh + 1) * 512], op=ALU.add),
                             reads=[Bpb, Bacc[t]], writes=[Bacc[t]])
                    S.op("dve", lambda e, t=t: e.tensor_tensor(out=acc[:, t, :], in0=acc[:, t, :], in1=g2[:], op=ALU.mult), reads=[Bacc[t], Bmb], writes=[Bacc[t]])
                    S.op("dve", lambda e, t=t: e.tensor_tensor(out=acc[:, t, :], in0=acc[:, t, :], in1=xb[:], op=ALU.add), reads=[Bacc[t], Bxb], writes=[Bacc[t]])
                    S.dma("sp", lambda e, gt=gt, t=t: e.dma_start(out=out_d[gt * 128:(gt + 1) * 128, :], in_=acc[:, t, :]), reads=[Bacc[t]], writes=[Bout])

        S.flush(); st.close()
        st = st_root
        if S.stopped:
            S.dma("sp", lambda e: e.dma_start(out=out_d[0:128, 0:NPRM], in_=prm[:, :]), reads=[Bprm], writes=[Bout], force=True)
        S.final_wait("sp", [Bout] + Bo2)
        S.flush()
    return nc


def prep_inputs(inp):
    f = lambda a: np.ascontiguousarray(np.asarray(a, dtype=np.float32))
    x = f(inp["x"]); c = f(inp["c"])
    w_in = f(inp["w_in"][0])
    q_, k_, v_ = w_in[:, 0:512], w_in[:, 512:1024], w_in[:, 1024:1536]
    z_, a_, b_ = w_in[:, 1536:2048], w_in[:, 2048:2052], w_in[:, 2052:2056]
    sq_, sk_, sv_ = w_in[:, 2056:2568], w_in[:, 2568:2696], w_in[:, 2696:2824]
    w_fm = f(np.concatenate([q_, k_, v_, sq_, sk_[:, 0:64], sk_[:, 0:64], sk_[:, 64:128], sk_[:, 64:128]], axis=1))
    w_tm = f(np.concatenate([z_, a_, b_, sv_], axis=1))
    cst = make_consts()
    ii = np.arange(128)
    bd32 = (ii[:, None] // 32 == ii[None, :] // 32)
    m1 = ((ii[:, None] // 32) % 2 == 0) & (ii[None, :] // 32 == ii[:, None] // 32 + 1)
    m2 = (ii[:, None] < 64) & (ii[None, :] >= 64)
    cstm = np.concatenate([bd32, m1.T, m2.T], axis=1).astype(np.float32).astype(ml_dtypes.bfloat16)
    b_ada = f(inp["b_ada"][0])
    n2bc = f(np.broadcast_to(f(inp["norm2_w"][0])[None, :], (128, 1024)))
    shared = {"cst": cst, "w_ada": f(inp["w_ada"][0]), "b_ada": b_ada[None, :], "w_fm": w_fm, "w_tm": w_tm,
              "w_out": f(inp["w_out"][0]), "n2bc": n2bc, "cstm": cstm,
              "w_router": f(inp["w_router"][0]), "b_down": f(inp["b_down"][0]),
              "w_up": f(inp["w_up"][0]).reshape(32 * 1024, 2048), "w_down": f(inp["w_down"][0]).reshape(32 * 1024, 1024)}
    prm2 = np.zeros((128, 544), np.float32)
    prm2[:, 0:32] = f(inp["b_router"][0])[None, :]
    prm2[:, 32:544] = f(inp["b_up"][0]).reshape(32, 16, 128).transpose(2, 0, 1).reshape(128, 512)
    shared["prm2"] = prm2
    if SPARSE:
        prm3 = np.zeros((128, 160), np.float32)
        prm3[:, 0:32] = np.arange(32, dtype=np.float32)[None, :]
        prm3[:, 32:128] = np.arange(96, dtype=np.float32)[None, :]
        prm3[:, 128] = np.arange(128, dtype=np.float32)
        shared["prm3"] = prm3
        shared["b_upg"] = f(f(inp["b_up"][0]).reshape(32, 16, 128).transpose(0, 2, 1).reshape(4096, 16))
        shared["w_upg"] = f(f(inp["w_up"][0]).reshape(32, 8, 128, 2048).transpose(0, 2, 1, 3).reshape(4096, 8, 2048))
        shared["w_dng"] = f(f(inp["w_down"][0]).reshape(32, 8, 128, 1024).transpose(0, 2, 1, 3).reshape(4096, 8, 1024))
        del shared["w_up"], shared["w_down"]
    maps = []
    for core in range(8):
        b, hf = core // 2, core % 2
        prm = np.zeros((128, NPRM), np.float32)
        prm[:, P_FLAG] = float(hf)
        prm[:, P_HALO] = (float(hf) - 1.0) * 30000.0
        prm[:, P_C:P_C + 8] = c[b].reshape(8, 128).T
        prm[:, P_ALOG:P_ALOG + 4] = f(inp["a_log"][0])[None, :]
        prm[:, P_DTB:P_DTB + 4] = f(inp["dt_bias"][0])[None, :]
        prm[:, P_SINK:P_SINK + 8] = f(inp["sinks"][0])[None, :]
        prm[:, P_DNW:P_DNW + 128] = f(inp["dn_norm_w"][0])[None, :]
        prm[0:64, P_QNW] = f(inp["q_norm_w"][0])
        prm[64:128, P_QNW_HI] = f(inp["q_norm_w"][0])
        prm[:, P_KNW] = np.tile(f(inp["k_norm_w"][0]), 2)
        prm[:, P_N1:P_N1 + 8] = f(inp["norm1_w"][0]).reshape(8, 128).T
        prm[:, P_BADA:P_BADA + 48] = b_ada.reshape(48, 128).T
        prm[:, P_CONV:P_CONV + 48] = f(inp["conv_w"][0]).T.reshape(12, 128, 4).transpose(1, 0, 2).reshape(128, 48)
        m = dict(shared)
        m["prm"] = prm
        m["xp"] = f(x[b, 0:2048])
        m["xo"] = f(x[b, hf * 2048:(hf + 1) * 2048])
        maps.append(m)
    return maps


_NC_CACHE = {}


def kernel(**inputs):
    maps = prep_inputs(inputs)
    if "nc" not in _NC_CACHE:
        _NC_CACHE["nc"] = build()
    res = run_bass_kernel_spmd(_NC_CACHE["nc"], maps, core_ids=list(range(8)))
    out = np.zeros((4, 4096, 1024), np.float32)
    for core in range(8):
        b, hf = core // 2, core % 2
        out[b, hf * 2048:(hf + 1) * 2048] = res.results[core]["out"]
    return out
```
